# Optimizing a Trainium2 kernel written in Bass

```python
import jax, jax.numpy as jnp
from jax import lax
import numpy as np

D_MODEL = 1024
BATCH = 16
SEQ = 4096
DEPTH = 4

MLA_HEADS = 8
QK_NOPE_DIM = 64
QK_ROPE_DIM = 32
V_HEAD_DIM = 64
Q_LORA_RANK = 256
KV_LORA_RANK = 128
ROPE_THETA = 10000.0
Q_BLOCK = 128

MLSTM_HEADS = 4
MLSTM_QK_DIM = 64
MLSTM_V_DIM = 128
CONV_K = 4
CHUNK = 64

D_FF = 2816
EPS = 1e-6

MLA_WIDTH = MLA_HEADS * V_HEAD_DIM
MLSTM_WIDTH = MLSTM_HEADS * MLSTM_V_DIM
MIX_WIDTH = MLA_WIDTH + MLSTM_WIDTH
MLSTM_QK_WIDTH = MLSTM_HEADS * MLSTM_QK_DIM
Q_UP_WIDTH = MLA_HEADS * (QK_NOPE_DIM + QK_ROPE_DIM)
KV_UP_WIDTH = MLA_HEADS * (QK_NOPE_DIM + V_HEAD_DIM)
IN_SPLITS = (Q_LORA_RANK, KV_LORA_RANK, QK_ROPE_DIM,
             MLSTM_QK_WIDTH, MLSTM_QK_WIDTH, MLSTM_WIDTH, MLSTM_WIDTH,
             MLSTM_HEADS, MLSTM_HEADS)
D_IN = sum(IN_SPLITS)

kernel_name = "hymba_mla_mlstm_macaron"


def rmsnorm(x, g):
    xf = x.astype(jnp.float32)
    y = xf * lax.rsqrt(jnp.mean(xf * xf, axis=-1, keepdims=True) + EPS)
    return (y * g.astype(jnp.float32)).astype(x.dtype)


def head_rmsnorm(x, g):
    B, S, H, d = x.shape
    xf = x.astype(jnp.float32)
    y = xf * lax.rsqrt(jnp.mean(xf * xf, axis=-1, keepdims=True) + EPS)
    return (y.reshape(B, S, H * d) * g.astype(jnp.float32)).astype(x.dtype)


def half_swiglu(x, g, w_gate, w_up, w_down):
    h = rmsnorm(x, g)
    return x + 0.5 * ((jax.nn.silu(h @ w_gate) * (h @ w_up)) @ w_down)


def rope_tables(positions):
    inv = ROPE_THETA ** (-jnp.arange(0, QK_ROPE_DIM, 2, dtype=jnp.float32) / QK_ROPE_DIM)
    ang = positions.astype(jnp.float32)[..., None] * inv
    cos, sin = jnp.cos(ang), jnp.sin(ang)
    return jnp.concatenate([cos, cos], -1), jnp.concatenate([sin, sin], -1)


def apply_rope(x, cos, sin):
    xf = x.astype(jnp.float32)
    x1, x2 = jnp.split(xf, 2, axis=-1)
    rot = jnp.concatenate([-x2, x1], axis=-1)
    return (xf * cos + rot * sin).astype(x.dtype)


def mla_attention(c_q, c_kv, k_rope_raw, cos, sin, q_norm_g, w_uq, kv_norm_g, w_ukv):
    B, S = c_q.shape[:2]
    q = (rmsnorm(c_q, q_norm_g) @ w_uq).reshape(B, S, MLA_HEADS, QK_NOPE_DIM + QK_ROPE_DIM)
    q_nope, q_rope = q[..., :QK_NOPE_DIM], q[..., QK_NOPE_DIM:]
    q_rope = apply_rope(q_rope, cos[:, :, None, :], sin[:, :, None, :])
    kv = (rmsnorm(c_kv, kv_norm_g) @ w_ukv).reshape(B, S, MLA_HEADS, QK_NOPE_DIM + V_HEAD_DIM)
    k_nope, v = kv[..., :QK_NOPE_DIM], kv[..., QK_NOPE_DIM:]
    k_rope = apply_rope(k_rope_raw, cos, sin)
    nb = S // Q_BLOCK
    qn_b = q_nope.reshape(B, nb, Q_BLOCK, MLA_HEADS, QK_NOPE_DIM).swapaxes(0, 1)
    qr_b = q_rope.reshape(B, nb, Q_BLOCK, MLA_HEADS, QK_ROPE_DIM).swapaxes(0, 1)
    key_pos = jnp.arange(S)
    scale = (QK_NOPE_DIM + QK_ROPE_DIM) ** -0.5

    def one_block(args):
        qn, qr, blk = args
        s = (jnp.einsum('bqhd,bkhd->bhqk', qn, k_nope)
             + jnp.einsum('bqhr,bkr->bhqk', qr, k_rope))
        s = s.astype(jnp.float32) * scale
        q_pos = blk * Q_BLOCK + jnp.arange(Q_BLOCK)
        s = jnp.where(key_pos[None, :] <= q_pos[:, None], s, -jnp.inf)
        p = jax.nn.softmax(s, axis=-1).astype(v.dtype)
        return jnp.einsum('bhqk,bkhd->bqhd', p, v)

    o = lax.map(one_block, (qn_b, qr_b, jnp.arange(nb)))
    return o.swapaxes(0, 1).reshape(B, S, MLA_HEADS, V_HEAD_DIM)


def causal_depthwise_conv(u, w, b):
    C = u.shape[-1]
    y = lax.conv_general_dilated(u, w[:, None, :].astype(u.dtype), window_strides=(1,),
                                 padding=[(CONV_K - 1, 0)],
                                 dimension_numbers=('NWC', 'WIO', 'NWC'),
                                 feature_group_count=C)
    return y + b


def mlstm_chunkwise(q, k, v, i_pre, log_f):
    B, S, H, dk = q.shape
    dv = v.shape[-1]
    nc, L = S // CHUNK, CHUNK
    f32 = jnp.float32

    def chunks(t):
        return t.astype(f32).reshape(B, nc, L, H, -1).transpose(0, 3, 1, 2, 4)

    qc = chunks(q) * (dk ** -0.5)
    kc, vc = chunks(k), chunks(v)
    ig = i_pre.astype(f32).reshape(B, nc, L, H).transpose(0, 3, 1, 2)
    lf = log_f.astype(f32).reshape(B, nc, L, H).transpose(0, 3, 1, 2)
    b = jnp.cumsum(lf, axis=-1)
    b_end = b[..., -1]

    w_end = b_end[..., None] - b + ig
    g = jnp.max(w_end, axis=-1)
    e_end = jnp.exp(w_end - g[..., None])
    kv_chunk = jnp.einsum('bhcl,bhcld,bhcle->bhcde', e_end, kc, vc)
    n_chunk = jnp.einsum('bhcl,bhcld->bhcd', e_end, kc)

    def step(carry, xs):
        C, n, m = carry
        a, gc, kvc, nck = xs
        m_new = jnp.maximum(a + m, gc)
        d_old = jnp.exp(a + m - m_new)
        d_new = jnp.exp(gc - m_new)
        C_new = d_old[..., None, None] * C + d_new[..., None, None] * kvc
        n_new = d_old[..., None] * n + d_new[..., None] * nck
        return (C_new, n_new, m_new), (C, n, m)

    init = (jnp.zeros((B, H, dk, dv), f32), jnp.zeros((B, H, dk), f32), jnp.zeros((B, H), f32))
    xs = (jnp.moveaxis(b_end, 2, 0), jnp.moveaxis(g, 2, 0),
          jnp.moveaxis(kv_chunk, 2, 0), jnp.moveaxis(n_chunk, 2, 0))
    _, (C_prev, n_prev, m_prev) = lax.scan(step, init, xs)
    C_prev = jnp.moveaxis(C_prev, 0, 2)
    n_prev = jnp.moveaxis(n_prev, 0, 2)
    m_prev = jnp.moveaxis(m_prev, 0, 2)

    causal = jnp.tril(jnp.ones((L, L), dtype=bool))
    log_d = jnp.where(causal, b[..., :, None] - b[..., None, :] + ig[..., None, :], -jnp.inf)
    inter = b + m_prev[..., None]
    m_t = jnp.maximum(inter, jnp.max(log_d, axis=-1))
    p = jnp.exp(log_d - m_t[..., None]) * jnp.einsum('bhcld,bhcsd->bhcls', qc, kc)
    e_inter = jnp.exp(inter - m_t)
    num = (e_inter[..., None] * jnp.einsum('bhcld,bhcde->bhcle', qc, C_prev)
           + jnp.einsum('bhcls,bhcse->bhcle', p, vc))
    den = e_inter * jnp.einsum('bhcld,bhcd->bhcl', qc, n_prev) + jnp.sum(p, axis=-1)
    h = num / jnp.maximum(jnp.abs(den), jnp.exp(-m_t))[..., None]
    return h.transpose(0, 2, 3, 1, 4).reshape(B, S, H, dv).astype(v.dtype)


def setup_inputs(seed: int = 0) -> dict:
    key = jax.random.key(seed)
    ks = jax.random.split(key, 32)
    f32 = jnp.float32

    def w(k, shape, fan_in):
        return jax.random.normal(k, shape, f32) * (fan_in ** -0.5)

    def gain(k, shape):
        return 1.0 + 0.02 * jax.random.normal(k, shape, f32)

    x = jax.random.normal(ks[0], (BATCH, SEQ, D_MODEL), f32)
    offsets = jax.random.randint(ks[1], (BATCH, 1), 0, 4096, dtype=jnp.int32)
    positions = (offsets + jnp.arange(SEQ, dtype=jnp.int32)[None, :]).astype(jnp.int32)
    return {
        "x": x,
        "positions": positions,
        "ffn1_norm": gain(ks[2], (DEPTH, D_MODEL)),
        "ffn1_w_gate": w(ks[3], (DEPTH, D_MODEL, D_FF), D_MODEL),
        "ffn1_w_up": w(ks[4], (DEPTH, D_MODEL, D_FF), D_MODEL),
        "ffn1_w_down": w(ks[5], (DEPTH, D_FF, D_MODEL), D_FF),
        "mix_norm": gain(ks[6], (DEPTH, D_MODEL)),
        "w_in": w(ks[7], (DEPTH, D_MODEL, D_IN), D_MODEL),
        "q_latent_norm": gain(ks[8], (DEPTH, Q_LORA_RANK)),
        "w_uq": w(ks[9], (DEPTH, Q_LORA_RANK, Q_UP_WIDTH), Q_LORA_RANK),
        "kv_latent_norm": gain(ks[10], (DEPTH, KV_LORA_RANK)),
        "w_ukv": w(ks[11], (DEPTH, KV_LORA_RANK, KV_UP_WIDTH), KV_LORA_RANK),
        "conv_w": w(ks[12], (DEPTH, CONV_K, 2 * MLSTM_QK_WIDTH), CONV_K),
        "conv_b": 0.01 * jax.random.normal(ks[13], (DEPTH, 2 * MLSTM_QK_WIDTH), f32),
        "b_igate": 0.1 * jax.random.normal(ks[14], (DEPTH, MLSTM_HEADS), f32),
        "b_fgate": (jnp.linspace(3.0, 6.0, MLSTM_HEADS, dtype=f32)[None, :]
                    + 0.1 * jax.random.normal(ks[15], (DEPTH, MLSTM_HEADS), f32)),
        "attn_head_norm": gain(ks[16], (DEPTH, MLA_WIDTH)),
        "mlstm_head_norm": gain(ks[17], (DEPTH, MLSTM_WIDTH)),
        "w_out": w(ks[18], (DEPTH, MIX_WIDTH, D_MODEL), MIX_WIDTH),
        "ffn2_norm": gain(ks[19], (DEPTH, D_MODEL)),
        "ffn2_w_gate": w(ks[20], (DEPTH, D_MODEL, D_FF), D_MODEL),
        "ffn2_w_up": w(ks[21], (DEPTH, D_MODEL, D_FF), D_MODEL),
        "ffn2_w_down": w(ks[22], (DEPTH, D_FF, D_MODEL), D_FF),
        "final_norm": gain(ks[23], (D_MODEL,)),
    }


def reference(x, positions, ffn1_norm, ffn1_w_gate, ffn1_w_up, ffn1_w_down,
              mix_norm, w_in, q_latent_norm, w_uq, kv_latent_norm, w_ukv,
              conv_w, conv_b, b_igate, b_fgate, attn_head_norm, mlstm_head_norm,
              w_out, ffn2_norm, ffn2_w_gate, ffn2_w_up, ffn2_w_down, final_norm):
    B, S, _ = x.shape
    cos, sin = rope_tables(positions)
    split_idx = [int(v) for v in np.cumsum(IN_SPLITS)[:-1]]

    for l in range(DEPTH):
        x = half_swiglu(x, ffn1_norm[l], ffn1_w_gate[l], ffn1_w_up[l], ffn1_w_down[l])

        h = rmsnorm(x, mix_norm[l])
        z = h @ w_in[l]
        c_q, c_kv, k_r, q_m, k_m, v_m, o_m, i_m, f_m = jnp.split(z, split_idx, axis=-1)

        y_att = mla_attention(c_q, c_kv, k_r, cos, sin, q_latent_norm[l], w_uq[l],
                              kv_latent_norm[l], w_ukv[l])
        y_att = head_rmsnorm(y_att, attn_head_norm[l])

        qk = jax.nn.silu(causal_depthwise_conv(jnp.concatenate([q_m, k_m], axis=-1),
                                               conv_w[l], conv_b[l]))
        q_c, k_c = jnp.split(qk, 2, axis=-1)
        i_pre = (i_m + b_igate[l]).astype(jnp.float32)
        log_f = jax.nn.log_sigmoid((f_m + b_fgate[l]).astype(jnp.float32))
        y_mem = mlstm_chunkwise(q_c.reshape(B, S, MLSTM_HEADS, MLSTM_QK_DIM),
                                k_c.reshape(B, S, MLSTM_HEADS, MLSTM_QK_DIM),
                                v_m.reshape(B, S, MLSTM_HEADS, MLSTM_V_DIM),
                                i_pre, log_f)
        y_mem = jax.nn.sigmoid(o_m) * head_rmsnorm(y_mem, mlstm_head_norm[l])

        x = x + jnp.concatenate([y_att, y_mem], axis=-1) @ w_out[l]

        x = half_swiglu(x, ffn2_norm[l], ffn2_w_gate[l], ffn2_w_up[l], ffn2_w_down[l])

    return rmsnorm(x, final_norm)
```

```python
import contextlib
import numpy as np
import concourse.bass as bass
import concourse.mybir as mybir
from concourse.bass_utils import run_bass_kernel_spmd

F32 = mybir.dt.float32
BF16 = mybir.dt.bfloat16
I32 = mybir.dt.int32
AF = mybir.ActivationFunctionType
ALU = mybir.AluOpType

D = 1024
DFF = 2816
NFC = 8
NC_FF = 22
TB = 512
EPS = 1e-6
ENGS = ("pe", "act", "dve", "pool", "sp")
NSLOT = 8

G_FFN1, G_MIX, G_FFN2, G_QL, G_KVL, G_CW, G_CB, G_AH, G_MH = 0, 8, 16, 24, 26, 27, 43, 47, 55
NGC = 59


class Res:
    __slots__ = ("w", "r", "excl")

    def __init__(self, excl=False):
        self.w = None
        self.r = []
        self.excl = excl


class Op:
    __slots__ = ("eng", "fn", "deps", "dma", "sig", "sem", "val", "slot")

    def __init__(self, eng, fn, dma):
        self.eng = eng
        self.fn = fn
        self.dma = dma
        self.deps = []
        self.sig = False
        self.sem = None
        self.val = 0
        self.slot = -1


class Prog:
    def __init__(self, nc):
        self.nc = nc
        self.ops = {e: [] for e in ENGS}
        self.dma_count = {e: 0 for e in ENGS}
        self.last_dma_in_slot = {}

    def op(self, eng, fn, reads=(), writes=(), dma=False):
        o = Op(eng, fn, dma)
        deps = []
        ex = [r for r in reads if r.excl]
        if ex:
            reads = [r for r in reads if not r.excl]
            writes = list(writes) + ex
        for r in reads:
            if r.w is not None:
                deps.append(r.w)
        for r in writes:
            if r.w is not None:
                deps.append(r.w)
            deps.extend(r.r)
        for d in deps:
            if d.eng == eng and not d.dma and not dma and eng == "pe":
                continue
            if d not in o.deps:
                o.deps.append(d)
                d.sig = True
        if dma:
            k = self.dma_count[eng]
            self.dma_count[eng] += 1
            o.slot = k % NSLOT
            prev = self.last_dma_in_slot.get((eng, o.slot))
            if prev is not None and prev not in o.deps:
                o.deps.append(prev)
            self.last_dma_in_slot[(eng, o.slot)] = o
            o.sig = True
        for r in reads:
            r.r.append(o)
        for r in writes:
            r.w = o
            r.r = []
        self.ops[eng].append(o)
        return o

    def emit(self, final_waits=()):
        nc = self.nc
        with contextlib.ExitStack() as st:
            sems = {e: st.enter_context(nc.semaphore("s_" + e)) for e in ENGS}
            dsems = {(e, s): st.enter_context(nc.semaphore(f"d_{e}_{s}"))
                     for e in ENGS if self.dma_count[e] > 0 for s in range(NSLOT)}
            cnt = {e: 0 for e in ENGS}
            dcnt = {}
            for e in ENGS:
                for o in self.ops[e]:
                    if o.dma:
                        key = (e, o.slot)
                        dcnt[key] = dcnt.get(key, 0) + 16
                        o.sem = dsems[key]
                        o.val = dcnt[key]
                    elif o.sig:
                        cnt[e] += 1
                        o.sem = sems[e]
                        o.val = cnt[e]
            block = st.enter_context(nc.Block())
            engobj = {"pe": nc.tensor, "act": nc.scalar, "dve": nc.vector,
                      "pool": nc.gpsimd, "sp": nc.sync}
            finals = list(final_waits)

            def run(e):
                eo = engobj[e]
                waited = {}
                for o in self.ops[e]:
                    for d in o.deps:
                        key = id(d.sem)
                        if waited.get(key, 0) >= d.val:
                            continue
                        eo.wait_ge(d.sem, d.val)
                        waited[key] = d.val
                    ins = o.fn(eo)
                    if o.sig:
                        ins.then_inc(o.sem, 16 if o.dma else 1)
                if e == "sp":
                    for d in finals:
                        if waited.get(id(d.sem), 0) >= d.val:
                            continue
                        eo.wait_ge(d.sem, d.val)
                        waited[id(d.sem)] = d.val

            @block.tensor
            def _(eng):
                run("pe")

            @block.scalar
            def _(eng):
                run("act")

            @block.vector
            def _(eng):
                run("dve")

            @block.gpsimd
            def _(eng):
                run("pool")

            @block.sync
            def _(eng):
                run("sp")


class Ring:
    def __init__(self, tiles):
        self.tiles = tiles
        self.res = [Res() for _ in tiles]
        self.i = 0

    def next(self):
        k = self.i % len(self.tiles)
        self.i += 1
        return self.tiles[k], self.res[k]


NWT = 18
NWS = NWT // 2
PI = 3.14159265358979
TWO_PI = 6.28318530717959
ATT_SCALE = 96.0 ** -0.5
MSTAGE = 9


def build(S, NSEQ, NL, do_mixer=True, do_mlstm=True, do_attn=True, final=True):
    nc = bass.Bass("TRN2", target_bir_lowering=False)
    NT = S * NSEQ
    NB = S // TB
    NCH = S // 128

    def din(name, shape, dt=F32):
        return nc.dram_tensor(name, list(shape), dt, kind="ExternalInput").ap()

    def dscr(name, shape, dt):
        return nc.dram_tensor(name, list(shape), dt, kind="Internal").ap()

    xT = din("xT", [D, NT])
    pos = din("pos", [1, NT], I32)
    invf = din("invf", [96, 2])
    wgu = din("wgu", [NL, 2, NC_FF, 128, 2048])
    wd = din("wd", [NL, 2, NFC, 128, DFF])
    win = din("win", [NL, NWS, 128, 2048])
    wout = din("wout", [NL, NFC, 128, 1536])
    uq = din("uq", [NL, 128, 3072])
    ukv = din("ukv", [NL, 128, 1024])
    gcols = din("gcols", [NL, 128, NGC])
    gfin = din("gfin", [128, NFC])
    gbias = din("gbias", [NL, 8])
    outT = nc.dram_tensor("outT", [D, NT], F32, kind="ExternalOutput").ap()

    wgu_b = dscr("wgu_b", [NL, 2, NC_FF, 128, 2048], BF16)
    wd_b = dscr("wd_b", [NL, 2, NFC, 128, DFF], BF16)
    win_b = dscr("win_b", [NL, NWS, 128, 2048], BF16)
    wout_b = dscr("wout_b", [NL, NFC, 128, 1536], BF16)
    xs = [dscr("xs0", [D, NT], F32), dscr("xs1", [D, NT], F32)]
    rope_d = dscr("rope_d", [2, 96, NT], F32)
    kc_d = dscr("kc_d", [NSEQ, 8, 96, S], BF16)
    vc_d = dscr("vc_d", [NSEQ, 8, 128, NCH, 65], BF16)

    P = Prog(nc)
    st = contextlib.ExitStack()
    with st:
        def sb(name, shape, dt=F32):
            return st.enter_context(nc.sbuf_tensor(name, list(shape), dt))

        x_sb = sb("x_sb", [128, NFC, TB]); r_x = [Res() for _ in range(NFC)]
        hT = sb("hT", [128, NFC, TB], BF16); r_h = [Res() for _ in range(NFC)]
        aT = sb("aT", [128, NC_FF, TB], BF16); r_a = [Res() for _ in range(NC_FF)]
        rstd = sb("rstd", [128, TB]); r_rstd = Res()
        sq_ring = Ring([sb(f"sq{i}", [128, TB], BF16) for i in range(2)])
        e_ring = Ring([sb(f"e{i}", [128, TB]) for i in range(2)])
        t_ring = Ring([sb(f"t{i}", [128, TB]) for i in range(2)])
        big_ring = Ring([sb(f"wb{i}", [128, 2048], BF16) for i in range(2)])
        small_ring = Ring([sb(f"ws{i}", [128, DFF], BF16) for i in range(2)])
        onesB = sb("onesB", [128, 128], BF16); r_const = Res()
        gc = sb("gc", [128, NL, NGC]); gf = sb("gf", [128, NFC])
        ostage = Ring([sb(f"ost{i}", [128, TB]) for i in range(2)])

        ps = [st.enter_context(nc.psum_tensor(f"ps{i}", [128, TB], F32)) for i in range(8)]
        r_ps = [[Res(excl=True)] * 4 for _ in range(8)]

        P.op("pool", lambda e: e.memset(onesB[:], 1.0), writes=[r_const])
        P.op("sp", lambda e: e.dma_start(out=gc[:], in_=gcols.rearrange("l p c -> p l c")), writes=[r_const], dma=True)
        P.op("sp", lambda e: e.dma_start(out=gf[:], in_=gfin), writes=[r_const], dma=True)

        if do_mixer:
            cqn = sb("cqn", [128, 2, TB], BF16); r_cqn = Res()
            ckvn = sb("ckvn", [128, TB], BF16); r_ckvn = Res()
            cs = sb("cs", [96, 2, TB]); r_cs = Res()
            KT = sb("KT", [96, 8, TB], BF16); r_KT = [Res() for _ in range(8)]
            VT = sb("VT", [128, 8, 4, 65], BF16); r_VT = Res()
            QT = sb("QT", [96, 8, TB], BF16); r_QT = [Res() for _ in range(8)]
            KH = Ring([sb(f"KH{i}", [96, TB], BF16) for i in range(3)])
            VH = Ring([sb(f"VH{i}", [128, 4, 65], BF16) for i in range(3)])
            PT = Ring([sb(f"PT{i}", [128, TB], BF16) for i in range(3)])
            sq65 = sb("sq65", [65, TB], BF16); r_sq65 = Res()
            NormW = sb("NormW", [65, 64], BF16)
            yA = sb("yA", [64, 8, TB], BF16); r_yA = [Res() for _ in range(8)]
            yM = sb("yM", [128, 4, TB], BF16); r_yM = [Res() for _ in range(4)]
            uq_sb = sb("uq_sb", [128, 2, 8, 192], BF16); r_uq = Res()
            ukv_sb = sb("ukv_sb", [128, 8, 128], BF16); r_ukv = Res()
            gb = sb("gb", [128, NL, 8])
            invf_sb = sb("invf_sb", [96, 2])
            TriB = sb("TriB", [128, 128], BF16)
            U = sb("U", [128, 4, TB + 3]); r_U = [Res() for _ in range(4)]
            qcT = sb("qcT", [128, 2, TB], BF16); r_qcT = [Res() for _ in range(2)]
            kTm = sb("kTm", [128, 2, TB], BF16); r_kTm = [Res() for _ in range(2)]
            so = sb("so", [128, 4, TB], BF16); r_so = [Res() for _ in range(4)]
            hTm = sb("hTm", [128, 4, TB]); r_hTm = [Res() for _ in range(4)]
            Vm = sb("Vm", [128, 4, 4, 129], BF16); r_Vm = [Res() for _ in range(4)]
            gsb = sb("gsb", [128, 4, 8]); r_gsb = Res()
            TriF = sb("TriF", [128, 128]); OnesF = sb("OnesF", [128, 128]); IdentF = sb("IdentF", [128, 128])
            IdentB = sb("IdentB", [128, 128], BF16); MaskNeg = sb("MaskNeg", [128, 128])
            nlf = Ring([sb(f"nlf{i}", [128, 4]) for i in range(2)])
            bias_s = Ring([sb(f"bias_s{i}", [128, 4]) for i in range(2)])
            wl = Ring([sb(f"wl{i}", [128, 4]) for i in range(2)])
            ebend = Ring([sb(f"ebend{i}", [128, 4]) for i in range(2)])
            ktok = Ring([sb(f"ktok{i}", [128, 256], BF16) for i in range(2)])
            nrep = Ring([sb(f"nrep{i}", [128, 128]) for i in range(2)])
            DT = Ring([sb(f"DT{i}", [128, 128]) for i in range(2)])
            EE = Ring([sb(f"EE{i}", [128, 128]) for i in range(2)])
            pTm = Ring([sb(f"pTm{i}", [128, 128], BF16) for i in range(2)])
            qb = Ring([sb(f"qb{i}", [128, 128], BF16) for i in range(2)])
            dn = Ring([sb(f"dn{i}", [128, 128]) for i in range(2)])
            kw = Ring([sb(f"kw{i}", [128, 64], BF16) for i in range(2)])
            Cst = sb("Cst", [128, 2, 129]); r_Cst = [Res() for _ in range(4)]
            Cb = sb("Cb", [128, 2, 128], BF16); nrepB = sb("nrepB", [128, 2, 128], BF16)
            r_Cb = [Res() for _ in range(4)]

            pl = lambda fn, **k: P.op("pool", fn, **k)
            pl(lambda e: e.memset(NormW[0:64, :], 1.0 / 64), writes=[r_const])
            pl(lambda e: e.memset(NormW[64:65, :], EPS), writes=[r_const])
            pl(lambda e: e.memset(OnesF[:], 1.0), writes=[r_const])
            pl(lambda e: e.memset(MaskNeg[:], 0.0), writes=[r_const])
            pl(lambda e: e.memset(VT[:], 1.0), writes=[r_VT])
            pl(lambda e: e.memset(Vm[:], 1.0), writes=r_Vm)
            pl(lambda e: e.memset(yA[:], 0.0), writes=r_yA)
            pl(lambda e: e.memset(yM[:], 0.0), writes=r_yM)
            pl(lambda e: e.affine_select(out=TriF[:], in_=OnesF[:], pattern=[[1, 128]], compare_op=ALU.is_ge,
                                         fill=0.0, base=0, channel_multiplier=-1), reads=[r_const], writes=[r_const])
            pl(lambda e: e.affine_select(out=MaskNeg[:], in_=MaskNeg[:], pattern=[[1, 128]], compare_op=ALU.is_ge,
                                         fill=-30000.0, base=0, channel_multiplier=-1), reads=[r_const], writes=[r_const])
            pl(lambda e: e.affine_select(out=IdentF[:], in_=OnesF[:], pattern=[[1, 128]], compare_op=ALU.is_equal,
                                         fill=0.0, base=0, channel_multiplier=-1), reads=[r_const], writes=[r_const])
            pl(lambda e: e.tensor_copy(out=TriB[:], in_=TriF[:]), reads=[r_const], writes=[r_const])
            pl(lambda e: e.tensor_copy(out=IdentB[:], in_=IdentF[:]), reads=[r_const], writes=[r_const])
            P.op("sp", lambda e: e.dma_start(out=invf_sb[:], in_=invf), writes=[r_const], dma=True)
            for l in range(NL):
                P.op("sp", lambda e, l=l: e.dma_start(out=gb[:, l, :], in_=gbias[l].partition_broadcast(128)),
                     writes=[r_const], dma=True)

            r_rope = Res()
            a, bq, cq_ = e_ring.tiles[0][0:96, :], e_ring.tiles[1][0:96, :], t_ring.tiles[0][0:96, :]
            rpi = t_ring.tiles[1][0:96, :].bitcast(I32)
            r_a_, r_b_, r_c_, r_i_ = e_ring.res[0], e_ring.res[1], t_ring.res[0], t_ring.res[1]
            for ci in range(NT // TB):
                c0 = ci * TB
                P.op("sp", lambda e, c0=c0: e.dma_start(out=rpi, in_=pos[0, c0:c0 + TB].partition_broadcast(96)),
                     writes=[r_i_], dma=True)
                dv = lambda fn, **k: P.op("dve", fn, **k)
                dv(lambda e: e.tensor_copy(out=a, in_=rpi), reads=[r_i_], writes=[r_a_])
                dv(lambda e: e.tensor_scalar(out=a, in0=a, scalar1=invf_sb[:, 0:1], scalar2=None, op0=ALU.mult),
                   reads=[r_a_, r_const], writes=[r_a_])
                for which in range(2):
                    sh = PI / 2 if which == 0 else 0.0
                    dv(lambda e, sh=sh: e.tensor_scalar(out=bq, in0=a, scalar1=sh, scalar2=1.0 / TWO_PI, op0=ALU.add, op1=ALU.mult),
                       reads=[r_a_], writes=[r_b_])
                    dv(lambda e: e.tensor_copy(out=rpi, in_=bq), reads=[r_b_], writes=[r_i_])
                    dv(lambda e: e.tensor_copy(out=bq, in_=rpi), reads=[r_i_], writes=[r_b_])
                    dv(lambda e: e.scalar_tensor_tensor(out=bq, in0=bq, scalar=-TWO_PI, in1=a, op0=ALU.mult, op1=ALU.add),
                       reads=[r_b_, r_a_], writes=[r_b_])
                    if which == 0:
                        dv(lambda e, sh=sh: e.tensor_scalar(out=bq, in0=bq, scalar1=sh, scalar2=None, op0=ALU.add),
                           reads=[r_b_], writes=[r_b_])
                    dv(lambda e: e.tensor_scalar(out=cq_, in0=bq, scalar1=PI, scalar2=-TWO_PI, op0=ALU.is_gt, op1=ALU.mult),
                       reads=[r_b_], writes=[r_c_])
                    dv(lambda e: e.tensor_tensor(out=bq, in0=bq, in1=cq_, op=ALU.add), reads=[r_b_, r_c_], writes=[r_b_])
                    dv(lambda e: e.tensor_scalar(out=cq_, in0=bq, scalar1=-PI, scalar2=TWO_PI, op0=ALU.is_lt, op1=ALU.mult),
                       reads=[r_b_], writes=[r_c_])
                    dv(lambda e: e.tensor_tensor(out=bq, in0=bq, in1=cq_, op=ALU.add), reads=[r_b_, r_c_], writes=[r_b_])
                    dv(lambda e: e.tensor_scalar(out=bq, in0=bq, scalar1=-3.14159, scalar2=3.14159, op0=ALU.max, op1=ALU.min),
                       reads=[r_b_], writes=[r_b_])
                    if which == 0:
                        P.op("act", lambda e: e.activation(out=cq_, in_=bq, func=AF.Sin), reads=[r_b_], writes=[r_c_])
                    else:
                        P.op("act", lambda e: e.activation(out=cq_, in_=bq, func=AF.Sin, scale=invf_sb[:, 1:2]),
                             reads=[r_b_, r_const], writes=[r_c_])
                    P.op("sp", lambda e, which=which, c0=c0: e.dma_start(out=rope_d[which, :, c0:c0 + TB], in_=cq_),
                         reads=[r_c_], writes=[r_rope], dma=True)

        r_w = {}

        def conv_ops(l):
            ops = []

            def add(key, dst, src, n):
                r_w[key] = Res()
                for h0 in range(0, n, 2048):
                    h1 = min(n, h0 + 2048)
                    ops.append((key, dst[:, h0:h1], src[:, h0:h1]))
            for f in range(2):
                for c in range(NC_FF):
                    add(("gu", l, f, c), wgu_b[l, f, c], wgu[l, f, c], 2048)
                for fc in range(NFC):
                    add(("d", l, f, fc), wd_b[l, f, fc], wd[l, f, fc], DFF)
                if f == 0 and do_mixer:
                    for j in range(NWS):
                        add(("in", l, j), win_b[l, j], win[l, j], 2048)
                    for fc in range(NFC):
                        add(("out", l, fc), wout_b[l, fc], wout[l, fc], 1536)
            return ops

        def emit_conv(items):
            for key, dst, src in items:
                P.op("pool", lambda e, dst=dst, src=src: e.dma_start(out=dst, in_=src),
                     writes=[r_w[key]], dma=True)

        def bc_rstd(psrc, r_src, n, dst, r_dst):
            P.op("act", lambda e: e.activation(out=dst, in_=psrc, func=AF.Ln, bias=EPS, scale=1.0 / n),
                 reads=r_src, writes=[r_dst])
            P.op("act", lambda e: e.activation(out=dst, in_=dst, func=AF.Exp, scale=-0.5),
                 reads=[r_dst], writes=[r_dst])

        def sigmoid_chain(dst, r_dst, src, r_src, final_out=None, r_final=None):
            P.op("act", lambda e: e.activation(out=dst, in_=src, func=AF.Exp, scale=-1.0), reads=r_src, writes=[r_dst])
            P.op("act", lambda e: e.activation(out=dst, in_=dst, func=AF.Ln, bias=1.0, scale=1.0), reads=[r_dst], writes=[r_dst])
            if final_out is None:
                P.op("act", lambda e: e.activation(out=dst, in_=dst, func=AF.Exp, scale=-1.0), reads=[r_dst], writes=[r_dst])
            else:
                P.op("act", lambda e: e.activation(out=final_out, in_=dst, func=AF.Exp, scale=-1.0), reads=[r_dst], writes=r_final)

        def norm_stats():
            for fc in range(NFC):
                sq, r_sq = sq_ring.next()
                P.op("act", lambda e, sq=sq, fc=fc: e.activation(out=sq[:], in_=x_sb[:, fc, :], func=AF.Square),
                     reads=[r_x[fc]], writes=[r_sq])
                P.op("pe", lambda e, sq=sq, fc=fc: e.matmul(ps[6][:], lhsT=onesB[:], rhs=sq[:], start=(fc == 0), stop=(fc == NFC - 1)),
                     reads=[r_sq, r_const], writes=r_ps[6])
            bc_rstd(ps[6][:], r_ps[6], D, rstd[:], r_rstd)

        def norm_to_hT(l, gbase):
            norm_stats()
            for fc in range(NFC):
                P.op("dve", lambda e, fc=fc: e.scalar_tensor_tensor(
                    out=hT[:, fc, :], in0=x_sb[:, fc, :], scalar=gc[:, l, gbase + fc:gbase + fc + 1],
                    in1=rstd[:], op0=ALU.mult, op1=ALU.mult),
                    reads=[r_x[fc], r_rstd, r_const], writes=[r_h[fc]])

        gu_alt = [0]

        def ffn(l, f):
            norm_to_hT(l, G_FFN1 if f == 0 else G_FFN2)
            for c in range(NC_FF):
                slab, r_slab = big_ring.next()
                P.op("sp", lambda e, slab=slab, c=c: e.dma_start(out=slab[:], in_=wgu_b[l, f, c]),
                     reads=[r_w[("gu", l, f, c)]], writes=[r_slab], dma=True)
                k = gu_alt[0] % 2
                gu_alt[0] += 1
                G, U_ = ps[2 * k], ps[2 * k + 1]
                rG, rU = r_ps[2 * k], r_ps[2 * k + 1]
                for kc in range(NFC):
                    P.op("pe", lambda e, slab=slab, kc=kc, G=G: e.matmul(
                        G[:], lhsT=slab[:, kc * 128:(kc + 1) * 128], rhs=hT[:, kc, :], start=(kc == 0), stop=(kc == NFC - 1)),
                        reads=[r_slab, r_h[kc]], writes=rG)
                for kc in range(NFC):
                    P.op("pe", lambda e, slab=slab, kc=kc, U_=U_: e.matmul(
                        U_[:], lhsT=slab[:, (8 + kc) * 128:(9 + kc) * 128], rhs=hT[:, kc, :], start=(kc == 0), stop=(kc == NFC - 1)),
                        reads=[r_slab, r_h[kc]], writes=rU)
                e1, r_e1 = e_ring.next()
                t1, r_t1 = t_ring.next()
                sigmoid_chain(e1[:], r_e1, G[:], rG)
                P.op("dve", lambda e, e1=e1, t1=t1, G=G: e.tensor_tensor(out=t1[:], in0=G[:], in1=e1[:], op=ALU.mult),
                     reads=rG + [r_e1], writes=[r_t1])
                P.op("dve", lambda e, t1=t1, U_=U_, c=c: e.tensor_tensor(out=aT[:, c, :], in0=U_[:], in1=t1[:], op=ALU.mult),
                     reads=rU + [r_t1], writes=[r_a[c]])
            for fc in range(NFC):
                slab, r_slab = small_ring.next()
                P.op("sp", lambda e, slab=slab, fc=fc: e.dma_start(out=slab[:], in_=wd_b[l, f, fc]),
                     reads=[r_w[("d", l, f, fc)]], writes=[r_slab], dma=True)
                Y, rY = ps[4 + fc % 2], r_ps[4 + fc % 2]
                for c in range(NC_FF):
                    P.op("pe", lambda e, slab=slab, c=c, Y=Y: e.matmul(
                        Y[:], lhsT=slab[:, c * 128:(c + 1) * 128], rhs=aT[:, c, :], start=(c == 0), stop=(c == NC_FF - 1)),
                        reads=[r_slab, r_a[c]], writes=rY)
                P.op("dve", lambda e, Y=Y, fc=fc: e.scalar_tensor_tensor(
                    out=x_sb[:, fc, :], in0=Y[:], scalar=0.5, in1=x_sb[:, fc, :], op0=ALU.mult, op1=ALU.add),
                    reads=rY + [r_x[fc]], writes=[r_x[fc]])

        r_kc = [[Res() for _ in range(NB)] for _ in range(NSEQ)]
        r_vc = [[Res() for _ in range(NB)] for _ in range(NSEQ)]

        def mixer(l, s, b):
            t0 = s * S + b * TB
            dv = lambda fn, **k: P.op("dve", fn, **k)
            ac = lambda fn, **k: P.op("act", fn, **k)
            pe = lambda fn, **k: P.op("pe", fn, **k)
            pl = lambda fn, **k: P.op("pool", fn, **k)
            if s == 0 and b == 0:
                pl(lambda e: e.dma_start(out=uq_sb[:], in_=uq[l]), writes=[r_uq], dma=True)
                pl(lambda e: e.dma_start(out=ukv_sb[:], in_=ukv[l]), writes=[r_ukv], dma=True)
            norm_to_hT(l, G_MIX)
            P.op("sp", lambda e: e.dma_start(out=cs[:], in_=rope_d[:, :, t0:t0 + TB].rearrange("w p t -> p w t")),
                 reads=[r_rope], writes=[r_cs], dma=True)
            if b == 0:
                dv(lambda e: e.memset(U[:, :, 0:3], 0.0), writes=r_U)
                dv(lambda e: e.memset(Cst[:], 0.0), writes=r_Cst)
                dv(lambda e: e.memset(Cb[:], 0.0), writes=r_Cb)
                dv(lambda e: e.memset(nrepB[:], 0.0), writes=r_Cb)

            def proj(slab, off, out_ap, r_out, M=128):
                for kc in range(NFC):
                    pe(lambda e, kc=kc: e.matmul(out_ap, lhsT=slab[:, off + kc * 128: off + kc * 128 + M], rhs=hT[:, kc, :],
                                                 start=(kc == 0), stop=(kc == NFC - 1)),
                       reads=[cur_r_slab[0], r_h[kc]], writes=r_out)

            cur_r_slab = [None]
            for j in range(NWS):
                slab, r_slab = big_ring.next()
                cur_r_slab[0] = r_slab
                P.op("sp", lambda e, slab=slab, j=j: e.dma_start(out=slab[:], in_=win_b[l, j]),
                     reads=[r_w[("in", l, j)]], writes=[r_slab], dma=True)
                for tt in (2 * j, 2 * j + 1):
                    off = (tt % 2) * 1024
                    if tt in (0, 1):
                        proj(slab, off, ps[tt][:], r_ps[tt])
                        if tt == 1:
                            for q in range(2):
                                sq, r_sq = sq_ring.next()
                                ac(lambda e, sq=sq, q=q: e.activation(out=sq[:], in_=ps[q][:], func=AF.Square), reads=r_ps[q], writes=[r_sq])
                                pe(lambda e, sq=sq, q=q: e.matmul(ps[6][:], lhsT=onesB[:], rhs=sq[:], start=(q == 0), stop=(q == 1)),
                                   reads=[r_sq, r_const], writes=r_ps[6])
                            bc_rstd(ps[6][:], r_ps[6], 256, rstd[:], r_rstd)
                            for q in range(2):
                                dv(lambda e, q=q: e.scalar_tensor_tensor(out=cqn[:, q, :], in0=ps[q][:], scalar=gc[:, l, G_QL + q:G_QL + q + 1],
                                                                      in1=rstd[:], op0=ALU.mult, op1=ALU.mult),
                                   reads=r_ps[q] + [r_rstd, r_const], writes=[r_cqn])
                    elif tt == 2:
                        proj(slab, off, ps[2][:], r_ps[2])
                        sq, r_sq = sq_ring.next()
                        ac(lambda e, sq=sq: e.activation(out=sq[:], in_=ps[2][:], func=AF.Square), reads=r_ps[2], writes=[r_sq])
                        pe(lambda e, sq=sq: e.matmul(ps[7][:], lhsT=onesB[:], rhs=sq[:], start=True, stop=True),
                           reads=[r_sq, r_const], writes=r_ps[7])
                        e1, r_e1 = e_ring.next()
                        bc_rstd(ps[7][:], r_ps[7], 128, e1[:], r_e1)
                        dv(lambda e, e1=e1: e.scalar_tensor_tensor(out=ckvn[:], in0=ps[2][:], scalar=gc[:, l, G_KVL:G_KVL + 1],
                                                                in1=e1[:], op0=ALU.mult, op1=ALU.mult),
                           reads=r_ps[2] + [r_e1, r_const], writes=[r_ckvn])
                    elif tt in (3, 4):
                        proj(slab, off, ps[tt][0:96, :], r_ps[tt], M=96)
                        if tt == 4:
                            ta, r_ta = t_ring.next()
                            tb_, r_tb = t_ring.next()
                            dv(lambda e, ta=ta: e.tensor_tensor(out=ta[64:96, :], in0=ps[3][64:96, :], in1=cs[64:96, 0, :], op=ALU.mult),
                               reads=r_ps[3] + [r_cs], writes=[r_ta])
                            dv(lambda e, tb_=tb_: e.tensor_tensor(out=tb_[64:96, :], in0=ps[4][64:96, :], in1=cs[64:96, 1, :], op=ALU.mult),
                               reads=r_ps[4] + [r_cs], writes=[r_tb])
                            dv(lambda e, ta=ta, tb_=tb_: e.tensor_tensor(out=KT[64:96, 0, :], in0=ta[64:96, :], in1=tb_[64:96, :], op=ALU.add),
                               reads=[r_ta, r_tb], writes=[r_KT[0]])
                            for h in range(1, 8):
                                pl(lambda e, h=h: e.tensor_copy(out=KT[64:96, h, :], in_=KT[64:96, 0, :]), reads=[r_KT[0]], writes=[r_KT[h]])
                    elif 5 <= tt <= 8:
                        i = tt - 5
                        pk = ps[i % 4]
                        proj(slab, off, pk[:], r_ps[i % 4])
                        ac(lambda e, i=i, pk=pk: e.activation(out=U[:, i, 3:TB + 3], in_=pk[:], func=AF.Copy), reads=r_ps[i % 4], writes=[r_U[i]])
                    elif 9 <= tt <= 12:
                        h = tt - 9
                        pk, rk = ps[4 + h % 2], r_ps[4 + h % 2]
                        proj(slab, off, pk[:], rk)
                        e1, r_e1 = e_ring.next()
                        sigmoid_chain(e1[:], r_e1, pk[:], rk, final_out=so[:, h, :], r_final=[r_so[h]])
                    elif tt == 13:
                        for jj in range(4):
                            for kc in range(NFC):
                                pe(lambda e, kc=kc, jj=jj, slab=slab, off=off: e.matmul(
                                    ps[6][:, jj * 8:(jj + 1) * 8], lhsT=hT[:, kc, jj * 128:(jj + 1) * 128],
                                    rhs=slab[:, off + kc * 128: off + kc * 128 + 8], start=(kc == 0), stop=(kc == NFC - 1)),
                                   reads=[r_slab, r_h[kc]], writes=[r_ps[6][0]])
                        for jj in range(4):
                            dv(lambda e, jj=jj: e.tensor_tensor(out=gsb[:, jj, :], in0=ps[6][:, jj * 8:(jj + 1) * 8], in1=gb[:, l, :], op=ALU.add),
                               reads=[r_ps[6][0], r_const], writes=[r_gsb])
                    else:
                        hh = tt - 14
                        pk, rk = ps[hh % 2], r_ps[hh % 2]
                        for jj in range(4):
                            for kc in range(NFC):
                                pe(lambda e, kc=kc, jj=jj, slab=slab, off=off, pk=pk: e.matmul(
                                    pk[:, jj * 128:(jj + 1) * 128], lhsT=hT[:, kc, jj * 128:(jj + 1) * 128],
                                    rhs=slab[:, off + kc * 128: off + (kc + 1) * 128], start=(kc == 0), stop=(kc == NFC - 1)),
                                   reads=[r_slab, r_h[kc]], writes=rk)
                        ac(lambda e, hh=hh, pk=pk: e.activation(out=Vm[:, :, hh, 0:128], in_=pk[:].rearrange("p (j d) -> p j d", d=128), func=AF.Copy),
                           reads=rk, writes=[r_Vm[hh]])

            for h in range(8):
                pk, rk = ps[2 + h % 2], r_ps[2 + h % 2]
                pe(lambda e, h=h, pk=pk: e.matmul(pk[0:64, :], lhsT=ukv_sb[:, h, 0:64], rhs=ckvn[:], start=True, stop=True),
                   reads=[r_ukv, r_ckvn], writes=rk)
                ac(lambda e, h=h, pk=pk: e.activation(out=KT[0:64, h, :], in_=pk[0:64, :], func=AF.Copy), reads=rk, writes=[r_KT[h]])
            for jj in range(4):
                pk, rk = ps[4 + jj % 2], r_ps[4 + jj % 2]
                pe(lambda e, jj=jj, pk=pk: e.matmul(pk[:].rearrange("p (h d) -> p h d", d=64), lhsT=ckvn[:, jj * 128:(jj + 1) * 128],
                                                   rhs=ukv_sb[:, :, 64:128], start=True, stop=True),
                   reads=[r_ukv, r_ckvn], writes=rk)
                dv(lambda e, jj=jj, pk=pk: e.tensor_copy(out=VT[:, :, jj, 0:64], in_=pk[:].rearrange("p (h d) -> p h d", d=64)),
                   reads=rk, writes=[r_VT])
            P.op("sp", lambda e: e.dma_start(out=kc_d[s, :, :, t0 - s * S:t0 - s * S + TB].rearrange("h p t -> p h t"), in_=KT[:]),
                 reads=r_KT, writes=[r_kc[s][b]], dma=True)
            P.op("sp", lambda e: e.dma_start(out=vc_d[s, :, :, 4 * b:4 * b + 4, :].rearrange("h p c e -> p h c e"), in_=VT[:]),
                 reads=[r_VT], writes=[r_vc[s][b]], dma=True)

            for h in range(8):
                pq, rq = ps[2 * (h % 2)], r_ps[2 * (h % 2)]
                pr_, rr = ps[2 * (h % 2) + 1], r_ps[2 * (h % 2) + 1]
                for kc in range(2):
                    pe(lambda e, h=h, kc=kc, pq=pq: e.matmul(pq[0:96, :], lhsT=uq_sb[:, kc, h, 0:96], rhs=cqn[:, kc, :], start=(kc == 0), stop=(kc == 1)),
                       reads=[r_uq, r_cqn], writes=rq)
                for kc in range(2):
                    pe(lambda e, h=h, kc=kc, pr_=pr_: e.matmul(pr_[0:96, :], lhsT=uq_sb[:, kc, h, 96:192], rhs=cqn[:, kc, :], start=(kc == 0), stop=(kc == 1)),
                       reads=[r_uq, r_cqn], writes=rr)
                ac(lambda e, h=h, pq=pq: e.activation(out=QT[0:64, h, :], in_=pq[0:64, :], func=AF.Copy), reads=rq, writes=[r_QT[h]])
                ta, r_ta = t_ring.next()
                tb_, r_tb = t_ring.next()
                dv(lambda e, ta=ta, pq=pq: e.tensor_tensor(out=ta[64:96, :], in0=pq[64:96, :], in1=cs[64:96, 0, :], op=ALU.mult),
                   reads=rq + [r_cs], writes=[r_ta])
                dv(lambda e, tb_=tb_, pr_=pr_: e.tensor_tensor(out=tb_[64:96, :], in0=pr_[64:96, :], in1=cs[64:96, 1, :], op=ALU.mult),
                   reads=rr + [r_cs], writes=[r_tb])
                dv(lambda e, ta=ta, tb_=tb_, h=h: e.tensor_tensor(out=QT[64:96, h, :], in0=ta[64:96, :], in1=tb_[64:96, :], op=ALU.add),
                   reads=[r_ta, r_tb], writes=[r_QT[h]])

            if do_attn:
                s_alt = 0
                for h in range(8):
                    pO, rO = ps[3 + h % 2], r_ps[3 + h % 2]
                    nk = 4 * (b + 1)
                    for kb in range(b + 1):
                        kh, r_kh = KH.next()
                        vh, r_vh = VH.next()
                        P.op("sp", lambda e, kh=kh, kb=kb, h=h: e.dma_start(out=kh[:], in_=kc_d[s, h, :, kb * TB:(kb + 1) * TB]),
                             reads=[r_kc[s][kb]], writes=[r_kh], dma=True)
                        P.op("sp", lambda e, vh=vh, kb=kb, h=h: e.dma_start(out=vh[:], in_=vc_d[s, h, :, 4 * kb:4 * kb + 4, :]),
                             reads=[r_vc[s][kb]], writes=[r_vh], dma=True)
                        for kk in range(4):
                            kc = kb * 4 + kk
                            d = kc - 4 * b
                            col0 = 128 * d if d > 0 else 0
                            pS, rS = ps[s_alt % 3], r_ps[s_alt % 3]
                            s_alt += 1
                            pt, r_pt = PT.next()
                            pe(lambda e, kh=kh, kk=kk, h=h, col0=col0, pS=pS: e.matmul(
                                pS[:, col0:TB], lhsT=kh[:, kk * 128:(kk + 1) * 128], rhs=QT[:, h, col0:TB], start=True, stop=True),
                               reads=[r_kh, r_QT[h]], writes=rS)
                            ac(lambda e, pt=pt, pS=pS, col0=col0: e.activation(out=pt[:, col0:TB], in_=pS[:, col0:TB], func=AF.Exp, scale=ATT_SCALE),
                               reads=rS, writes=[r_pt])
                            if d >= 0:
                                dv(lambda e, pt=pt, col0=col0: e.tensor_tensor(out=pt[:, col0:col0 + 128], in0=pt[:, col0:col0 + 128], in1=TriB[:], op=ALU.mult),
                                   reads=[r_pt, r_const], writes=[r_pt])
                            pe(lambda e, vh=vh, kk=kk, pt=pt, col0=col0, kc=kc, nk=nk, pO=pO: e.matmul(
                                pO[0:65, col0:TB], lhsT=vh[:, kk, :], rhs=pt[:, col0:TB], start=(kc == 0), stop=(kc == nk - 1)),
                               reads=[r_vh, r_pt], writes=rO)
                    ac(lambda e, pO=pO: e.activation(out=sq65[:], in_=pO[0:65, :], func=AF.Square), reads=rO, writes=[r_sq65])
                    pe(lambda e: e.matmul(ps[5][0:64, :], lhsT=NormW[:], rhs=sq65[:], start=True, stop=True),
                       reads=[r_sq65, r_const], writes=r_ps[5])
                    e1, r_e1 = e_ring.next()
                    P.op("act", lambda e, e1=e1: e.activation(out=e1[0:64, :], in_=ps[5][0:64, :], func=AF.Ln), reads=r_ps[5], writes=[r_e1])
                    P.op("act", lambda e, e1=e1: e.activation(out=e1[0:64, :], in_=e1[0:64, :], func=AF.Exp, scale=-0.5), reads=[r_e1], writes=[r_e1])
                    dv(lambda e, e1=e1, h=h, pO=pO: e.scalar_tensor_tensor(out=yA[:, h, :], in0=pO[0:64, :], scalar=gc[0:64, l, G_AH + h:G_AH + h + 1],
                                                                    in1=e1[0:64, :], op0=ALU.mult, op1=ALU.mult),
                       reads=rO + [r_e1, r_const], writes=[r_yA[h]])

            if do_mlstm:
                for i in range(4):
                    acc, r_acc = t_ring.next()
                    cw = lambda jj, i=i: gc[:, l, G_CW + i * 4 + jj:G_CW + i * 4 + jj + 1]
                    dv(lambda e, acc=acc, i=i, cw=cw: e.tensor_scalar(out=acc[:], in0=U[:, i, 0:TB], scalar1=cw(0),
                                                                      scalar2=gc[:, l, G_CB + i:G_CB + i + 1], op0=ALU.mult, op1=ALU.add),
                       reads=[r_U[i], r_const], writes=[r_acc])
                    for jj in range(1, 4):
                        dv(lambda e, acc=acc, i=i, jj=jj, cw=cw: e.scalar_tensor_tensor(out=acc[:], in0=U[:, i, jj:jj + TB], scalar=cw(jj),
                                                                                  in1=acc[:], op0=ALU.mult, op1=ALU.add),
                           reads=[r_U[i], r_acc, r_const], writes=[r_acc])
                    e1, r_e1 = e_ring.next()
                    sigmoid_chain(e1[:], r_e1, acc[:], [r_acc])
                    if i < 2:
                        dv(lambda e, acc=acc, e1=e1, i=i: e.scalar_tensor_tensor(out=qcT[:, i, :], in0=acc[:], scalar=0.125, in1=e1[:], op0=ALU.mult, op1=ALU.mult),
                           reads=[r_acc, r_e1], writes=[r_qcT[i]])
                    else:
                        dv(lambda e, acc=acc, e1=e1, i=i: e.tensor_tensor(out=kTm[:, i - 2, :], in0=acc[:], in1=e1[:], op=ALU.mult),
                           reads=[r_acc, r_e1], writes=[r_kTm[i - 2]])
                    dv(lambda e, i=i: e.tensor_copy(out=U[:, i, 0:3], in_=U[:, i, TB:TB + 3]), reads=[r_U[i]], writes=[r_U[i]])

                for jj in range(4 if MSTAGE >= 2 else 0):
                    cols = slice(jj * 128, (jj + 1) * 128)
                    nl_, r_nl = nlf.next()
                    bs_, r_bs = bias_s.next()
                    wl_, r_wl = wl.next()
                    eb_, r_eb = ebend.next()
                    kt_, r_kt = ktok.next()
                    ac(lambda e, nl_=nl_, jj=jj: e.activation(out=nl_[:], in_=gsb[:, jj, 4:8], func=AF.Exp, scale=-1.0), reads=[r_gsb], writes=[r_nl])
                    ac(lambda e, nl_=nl_: e.activation(out=nl_[:], in_=nl_[:], func=AF.Ln, bias=1.0, scale=1.0), reads=[r_nl], writes=[r_nl])
                    rg0 = [r_ps[6][0]]
                    pe(lambda e, nl_=nl_: e.matmul(ps[6][:, 0:4], lhsT=TriF[:], rhs=nl_[:], start=True, stop=True), reads=[r_nl, r_const], writes=rg0)
                    pe(lambda e, nl_=nl_: e.matmul(ps[6][:, 4:8], lhsT=OnesF[:], rhs=nl_[:], start=True, stop=True), reads=[r_nl, r_const], writes=rg0)
                    dv(lambda e, bs_=bs_, jj=jj: e.tensor_tensor(out=bs_[:], in0=ps[6][:, 0:4], in1=gsb[:, jj, 0:4], op=ALU.add),
                       reads=rg0 + [r_gsb], writes=[r_bs])
                    dv(lambda e, bs_=bs_, wl_=wl_: e.tensor_tensor(out=wl_[:], in0=bs_[:], in1=ps[6][:, 4:8], op=ALU.subtract),
                       reads=rg0 + [r_bs], writes=[r_wl])
                    ac(lambda e, wl_=wl_: e.activation(out=wl_[:], in_=wl_[:], func=AF.Exp), reads=[r_wl], writes=[r_wl])
                    ac(lambda e, eb_=eb_: e.activation(out=eb_[:], in_=ps[6][:, 4:8], func=AF.Exp, scale=-1.0), reads=rg0, writes=[r_eb])
                    rg1 = r_ps[7]
                    ktv = ps[7][:, 0:256]
                    for i in range(2 if MSTAGE >= 3 else 0):
                        pe(lambda e, i=i, cols=cols, ktv=ktv: e.matmul(ktv[:, i * 128:(i + 1) * 128], lhsT=kTm[:, i, cols], rhs=IdentB[:], start=True, stop=True),
                           reads=[r_kTm[i], r_const], writes=rg1)
                    dv(lambda e, kt_=kt_, ktv=ktv: e.tensor_copy(out=kt_[:], in_=ktv), reads=rg1, writes=[r_kt])
                    for h in range(4 if MSTAGE >= 4 else 0):
                        po = (h % 2) * 64
                        prs = slice(po, po + 64)
                        ti = h // 2
                        pC, rC = ps[5], r_ps[5]
                        nr_, r_nr = nrep.next()
                        dt_, r_dt = DT.next()
                        ee_, r_ee = EE.next()
                        pt_, r_ptm = pTm.next()
                        qb_, r_qb = qb.next()
                        dn_, r_dn = dn.next()
                        kw_, r_kw = kw.next()
                        dv(lambda e, nr_=nr_, nl_=nl_, h=h: e.tensor_scalar(out=nr_[:], in0=OnesF[:], scalar1=nl_[:, h:h + 1], scalar2=-1.0, op0=ALU.mult, op1=ALU.mult),
                           reads=[r_nl, r_const], writes=[r_nr])
                        pe(lambda e, nr_=nr_: e.matmul(ps[1][:, 0:128], lhsT=nr_[:], rhs=TriF[:], start=True, stop=True), reads=[r_nr, r_const], writes=r_ps[1])
                        pe(lambda e, nr_=nr_: e.matmul(ps[0][:, 0:128], lhsT=nr_[:], rhs=TriF[:], start=True, stop=False), reads=[r_nr, r_const], writes=r_ps[0])
                        pe(lambda e: e.matmul(ps[0][:, 0:128], lhsT=IdentF[:], rhs=MaskNeg[:], start=False, stop=True), reads=[r_const], writes=r_ps[0])
                        pe(lambda e, prs=prs, ti=ti, cols=cols: e.matmul(ps[2][:, 0:128], lhsT=kTm[prs, ti, cols], rhs=qcT[prs, ti, cols], start=True, stop=True),
                           reads=[r_kTm[ti], r_qcT[ti]], writes=r_ps[2])
                        ac(lambda e, dt_=dt_, bs_=bs_, h=h: e.activation(out=dt_[:], in_=ps[0][:, 0:128], func=AF.Exp, bias=bs_[:, h:h + 1], scale=1.0),
                           reads=r_ps[0] + [r_bs], writes=[r_dt])
                        ac(lambda e, ee_=ee_: e.activation(out=ee_[:], in_=ps[1][:, 0:128], func=AF.Exp), reads=r_ps[1], writes=[r_ee])
                        dv(lambda e, pt_=pt_, dt_=dt_: e.tensor_tensor(out=pt_[:], in0=ps[2][:, 0:128], in1=dt_[:], op=ALU.mult),
                           reads=r_ps[2] + [r_dt], writes=[r_ptm])
                        dv(lambda e, qb_=qb_, ee_=ee_, prs=prs, ti=ti, cols=cols: e.tensor_tensor(out=qb_[prs, :], in0=qcT[prs, ti, cols], in1=ee_[prs, :], op=ALU.mult),
                           reads=[r_qcT[ti], r_ee], writes=[r_qb])
                        pe(lambda e, jj=jj, h=h, pt_=pt_: e.matmul(ps[3][:, 0:128], lhsT=Vm[:, jj, h, 0:128], rhs=pt_[:], start=True, stop=False),
                           reads=[r_Vm[h], r_ptm], writes=r_ps[3])
                        pe(lambda e, prs=prs, ti=ti, qb_=qb_: e.matmul(ps[3][:, 0:128], lhsT=Cb[prs, ti, :], rhs=qb_[prs, :], start=False, stop=True),
                           reads=[r_Cb[h], r_qb], writes=r_ps[3])
                        pe(lambda e, pt_=pt_: e.matmul(ps[4][:, 0:128], lhsT=onesB[:], rhs=pt_[:], start=True, stop=False),
                           reads=[r_const, r_ptm], writes=r_ps[4])
                        pe(lambda e, prs=prs, ti=ti, qb_=qb_: e.matmul(ps[4][:, 0:128], lhsT=nrepB[prs, ti, :], rhs=qb_[prs, :], start=False, stop=True),
                           reads=[r_Cb[h], r_qb], writes=r_ps[4])
                        ac(lambda e, dn_=dn_: e.activation(out=dn_[:], in_=ps[4][:, 0:128], func=AF.Abs), reads=r_ps[4], writes=[r_dn])
                        dv(lambda e, dn_=dn_: e.tensor_scalar(out=dn_[:], in0=dn_[:], scalar1=1.0, scalar2=None, op0=ALU.max),
                           reads=[r_dn], writes=[r_dn])
                        dv(lambda e, dn_=dn_: e.reciprocal(out=dn_[:], in_=dn_[:]), reads=[r_dn], writes=[r_dn])
                        dv(lambda e, dn_=dn_, h=h, cols=cols: e.tensor_tensor(out=hTm[:, h, cols], in0=ps[3][:, 0:128], in1=dn_[:], op=ALU.mult),
                           reads=r_ps[3] + [r_dn], writes=[r_hTm[h]])
                        if MSTAGE < 5:
                            continue
                        dv(lambda e, kw_=kw_, kt_=kt_, wl_=wl_, h=h: e.tensor_scalar(out=kw_[:], in0=kt_[:, h * 64:(h + 1) * 64], scalar1=wl_[:, h:h + 1], scalar2=None, op0=ALU.mult),
                           reads=[r_kt, r_wl], writes=[r_kw])
                        pe(lambda e, pC=pC, prs=prs, kw_=kw_, jj=jj, h=h: e.matmul(pC[prs, 0:129], lhsT=kw_[:], rhs=Vm[:, jj, h, :], start=True, stop=True),
                           reads=[r_kw, r_Vm[h]], writes=rC)
                        dv(lambda e, pC=pC, prs=prs, ti=ti, eb_=eb_, h=h: e.scalar_tensor_tensor(out=Cst[prs, ti, :], in0=Cst[prs, ti, :], scalar=eb_[prs, h:h + 1],
                                                                                     in1=pC[prs, 0:129], op0=ALU.mult, op1=ALU.add),
                           reads=rC + [r_eb, r_Cst[h]], writes=[r_Cst[h]])
                        dv(lambda e, prs=prs, ti=ti: e.tensor_copy(out=Cb[prs, ti, :], in_=Cst[prs, ti, 0:128]), reads=[r_Cst[h]], writes=[r_Cb[h]])
                        dv(lambda e, prs=prs, ti=ti: e.tensor_scalar(out=nrepB[prs, ti, :], in0=OnesF[prs, :], scalar1=Cst[prs, ti, 128:129], scalar2=None, op0=ALU.mult),
                           reads=[r_Cst[h], r_const], writes=[r_Cb[h]])
                for h in range(4):
                    sq, r_sq = sq_ring.next()
                    ac(lambda e, sq=sq, h=h: e.activation(out=sq[:], in_=hTm[:, h, :], func=AF.Square), reads=[r_hTm[h]], writes=[r_sq])
                    pe(lambda e, sq=sq: e.matmul(ps[7][:], lhsT=onesB[:], rhs=sq[:], start=True, stop=True), reads=[r_sq, r_const], writes=r_ps[7])
                    e1, r_e1 = e_ring.next()
                    bc_rstd(ps[7][:], r_ps[7], 128, e1[:], r_e1)
                    t1, r_t1 = t_ring.next()
                    dv(lambda e, t1=t1, e1=e1, h=h: e.scalar_tensor_tensor(out=t1[:], in0=hTm[:, h, :], scalar=gc[:, l, G_MH + h:G_MH + h + 1], in1=e1[:],
                                                                     op0=ALU.mult, op1=ALU.mult),
                       reads=[r_hTm[h], r_e1, r_const], writes=[r_t1])
                    dv(lambda e, t1=t1, h=h: e.tensor_tensor(out=yM[:, h, :], in0=t1[:], in1=so[:, h, :], op=ALU.mult),
                       reads=[r_t1, r_so[h]], writes=[r_yM[h]])

            for fc in range(NFC):
                slab, r_slab = small_ring.next()
                P.op("sp", lambda e, slab=slab, fc=fc: e.dma_start(out=slab[:, 0:1536], in_=wout_b[l, fc]),
                     reads=[r_w[("out", l, fc)]], writes=[r_slab], dma=True)
                Y, rY = ps[6 + fc % 2], r_ps[6 + fc % 2]
                for h in range(8):
                    pe(lambda e, slab=slab, h=h, Y=Y: e.matmul(Y[:], lhsT=slab[0:64, h * 128:(h + 1) * 128], rhs=yA[:, h, :], start=(h == 0), stop=False),
                       reads=[r_slab, r_yA[h]], writes=rY)
                for h in range(4):
                    pe(lambda e, slab=slab, h=h, Y=Y: e.matmul(Y[:], lhsT=slab[:, 1024 + h * 128:1024 + (h + 1) * 128], rhs=yM[:, h, :], start=False, stop=(h == 3)),
                       reads=[r_slab, r_yM[h]], writes=rY)
                dv(lambda e, Y=Y, fc=fc: e.tensor_tensor(out=x_sb[:, fc, :], in0=Y[:], in1=x_sb[:, fc, :], op=ALU.add),
                   reads=rY + [r_x[fc]], writes=[r_x[fc]])

        conv_all = [conv_ops(l) for l in range(NL)]
        emit_conv(conv_all[0])
        r_xs = [[[Res() for _ in range(NB)] for _ in range(NSEQ)] for _ in range(2)]
        finals = []
        for l in range(NL):
            nxt = conv_all[l + 1] if l + 1 < NL else []
            npass = NSEQ * NB
            per = (len(nxt) + npass - 1) // npass if nxt else 0
            ip = 0
            for s in range(NSEQ):
                for b in range(NB):
                    t0 = s * S + b * TB
                    src = xT if l == 0 else xs[(l - 1) % 2]
                    srcv = src.rearrange("(fc p) t -> p fc t", p=128)[:, :, t0:t0 + TB]
                    rd = [] if l == 0 else [r_xs[(l - 1) % 2][s][b]]
                    P.op("sp", lambda e, srcv=srcv: e.dma_start(out=x_sb[:], in_=srcv), reads=rd, writes=r_x, dma=True)
                    ffn(l, 0)
                    if do_mixer:
                        mixer(l, s, b)
                    ffn(l, 1)
                    if l == NL - 1 and final:
                        norm_stats()
                        for fc in range(NFC):
                            o_t, r_o = ostage.next()
                            P.op("dve", lambda e, fc=fc, o_t=o_t: e.scalar_tensor_tensor(
                                out=o_t[:], in0=x_sb[:, fc, :], scalar=gf[:, fc:fc + 1], in1=rstd[:], op0=ALU.mult, op1=ALU.mult),
                                reads=[r_x[fc], r_rstd, r_const], writes=[r_o])
                            finals.append(P.op("sp", lambda e, fc=fc, o_t=o_t, t0=t0: e.dma_start(
                                out=outT[fc * 128:(fc + 1) * 128, t0:t0 + TB], in_=o_t[:]), reads=[r_o], writes=[Res()], dma=True))
                    elif l == NL - 1:
                        dst = outT.rearrange("(fc p) t -> p fc t", p=128)[:, :, t0:t0 + TB]
                        finals.append(P.op("sp", lambda e, dst=dst: e.dma_start(out=dst, in_=x_sb[:]), reads=r_x, writes=[Res()], dma=True))
                    else:
                        dst = xs[l % 2].rearrange("(fc p) t -> p fc t", p=128)[:, :, t0:t0 + TB]
                        P.op("sp", lambda e, dst=dst: e.dma_start(out=dst, in_=x_sb[:]), reads=r_x, writes=[r_xs[l % 2][s][b]], dma=True)
                    if nxt:
                        emit_conv(nxt[ip * per:(ip + 1) * per])
                        ip += 1
        P.emit(final_waits=finals)
    return nc


def prep_weights(inp, L0, NL):
    f32 = np.float32
    w = {}
    inp = {k: (np.asarray(v)[L0:L0 + NL] if k not in ('x', 'positions', 'final_norm') else v) for k, v in inp.items()}
    wgu = np.zeros((NL, 2, NC_FF, 128, 2, NFC, 128), f32)
    wd = np.zeros((NL, 2, NFC, 128, NC_FF, 128), f32)
    for f, pre in enumerate(("ffn1", "ffn2")):
        for j, nm in enumerate(("w_gate", "w_up")):
            a = np.asarray(inp[f"{pre}_{nm}"], f32)[:NL]
            a = a.reshape(NL, NFC, 128, NC_FF, 128)
            wgu[:, f, :, :, j, :, :] = a.transpose(0, 3, 2, 1, 4)
        a = np.asarray(inp[f"{pre}_w_down"], f32)[:NL]
        a = a.reshape(NL, NC_FF, 128, NFC, 128)
        wd[:, f] = a.transpose(0, 3, 2, 1, 4)
    w["wgu"] = wgu.reshape(NL, 2, NC_FF, 128, 2048)
    w["wd"] = wd.reshape(NL, 2, NFC, 128, DFF)
    Win = np.asarray(inp["w_in"], f32)[:NL].reshape(NL, NFC, 128, 1960)
    tiles = np.zeros((NL, NWT, 128, NFC, 128), f32)

    def put(t, c0, c1, src_cols):
        tiles[:, t, :, :, c0:c1] = Win[:, :, :, src_cols].transpose(0, 2, 1, 3)
    put(0, 0, 128, np.arange(0, 128)); put(1, 0, 128, np.arange(128, 256)); put(2, 0, 128, np.arange(256, 384))
    put(3, 64, 96, np.arange(384, 416))
    put(4, 64, 96, 384 + (np.arange(32) + 16) % 32)
    put(5, 0, 128, np.arange(416, 544)); put(6, 0, 128, np.arange(544, 672))
    put(7, 0, 128, np.arange(672, 800)); put(8, 0, 128, np.arange(800, 928))
    for h in range(4):
        put(9 + h, 0, 128, np.arange(1440 + 128 * h, 1568 + 128 * h))
        put(14 + h, 0, 128, np.arange(928 + 128 * h, 1056 + 128 * h))
    put(13, 0, 8, np.arange(1952, 1960))
    w["win"] = np.ascontiguousarray(tiles.reshape(NL, NWS, 2, 128, NFC * 128).transpose(0, 1, 3, 2, 4)).reshape(NL, NWS, 128, 2048)
    Wo = np.asarray(inp["w_out"], f32)[:NL]
    wout = np.zeros((NL, NFC, 128, 1536), f32)
    att = Wo[:, 0:512].reshape(NL, 8, 64, NFC, 128)
    wout[:, :, 0:64, 0:1024] = att.transpose(0, 3, 2, 1, 4).reshape(NL, NFC, 64, 1024)
    mem = Wo[:, 512:1024].reshape(NL, 4, 128, NFC, 128)
    wout[:, :, :, 1024:1536] = mem.transpose(0, 3, 2, 1, 4).reshape(NL, NFC, 128, 512)
    w["wout"] = wout
    Wq = np.asarray(inp["w_uq"], f32)[:NL].reshape(NL, 2, 128, 8, 96)
    uq = np.zeros((NL, 128, 2, 8, 192), f32)
    uq[:, :, :, :, 0:96] = Wq.transpose(0, 2, 1, 3, 4)
    uq[:, :, :, :, 160:192] = Wq.transpose(0, 2, 1, 3, 4)[..., 64 + (np.arange(32) + 16) % 32]
    w["uq"] = uq.reshape(NL, 128, 3072)
    w["ukv"] = np.ascontiguousarray(np.asarray(inp["w_ukv"], f32)[:NL])
    gc = np.zeros((NL, 128, NGC), f32)
    for base, nm in ((G_FFN1, "ffn1_norm"), (G_MIX, "mix_norm"), (G_FFN2, "ffn2_norm")):
        gc[:, :, base:base + 8] = np.asarray(inp[nm], f32)[:NL].reshape(NL, 8, 128).transpose(0, 2, 1)
    gc[:, :, G_QL:G_QL + 2] = np.asarray(inp["q_latent_norm"], f32)[:NL].reshape(NL, 2, 128).transpose(0, 2, 1)
    gc[:, :, G_KVL] = np.asarray(inp["kv_latent_norm"], f32)[:NL]
    cw = np.asarray(inp["conv_w"], f32)[:NL].reshape(NL, 4, 4, 128)
    gc[:, :, G_CW:G_CW + 16] = cw.transpose(0, 3, 2, 1).reshape(NL, 128, 16)
    gc[:, :, G_CB:G_CB + 4] = np.asarray(inp["conv_b"], f32)[:NL].reshape(NL, 4, 128).transpose(0, 2, 1)
    gc[:, 0:64, G_AH:G_AH + 8] = np.asarray(inp["attn_head_norm"], f32)[:NL].reshape(NL, 8, 64).transpose(0, 2, 1)
    gc[:, :, G_MH:G_MH + 4] = np.asarray(inp["mlstm_head_norm"], f32)[:NL].reshape(NL, 4, 128).transpose(0, 2, 1)
    w["gcols"] = gc
    w["gfin"] = np.ascontiguousarray(np.asarray(inp["final_norm"], f32).reshape(8, 128).T)
    w["gbias"] = np.concatenate([np.asarray(inp["b_igate"], f32)[:NL], np.asarray(inp["b_fgate"], f32)[:NL]], axis=1)
    inv = (10000.0 ** (-np.arange(0, 32, 2, dtype=np.float32) / 32)).astype(f32)
    invf = np.ones((96, 2), f32)
    invf[:, 0] = 0.0
    invf[64:80, 0] = inv
    invf[80:96, 0] = inv
    invf[64:80, 1] = -1.0
    w["invf"] = invf
    return w


_CACHE = {}


def launch(inp, xT_list, S, B, L0, NL, ncores, final, **kw):
    NSEQ = B // ncores
    key = (S, NSEQ, NL, final, tuple(sorted(kw.items())))
    if key not in _CACHE:
        _CACHE[key] = build(S, NSEQ, NL, final=final, **kw)
    nc = _CACHE[key]
    w = prep_weights(inp, L0, NL)
    posn = np.asarray(inp["positions"], np.int32)
    in_maps = []
    for c in range(ncores):
        m = dict(w)
        m["xT"] = xT_list[c]
        m["pos"] = np.ascontiguousarray(posn[c * NSEQ:(c + 1) * NSEQ].reshape(1, NSEQ * S))
        in_maps.append(m)
    res = run_bass_kernel_spmd(nc, in_maps, core_ids=list(range(ncores)))
    return [res.results[c]["outT"] for c in range(ncores)]


def run(inp, S, B, NL, ncores, per_launch=None, **kw):
    NSEQ = B // ncores
    x = np.asarray(inp["x"], np.float32)
    xT_list = [np.ascontiguousarray(x[c * NSEQ:(c + 1) * NSEQ].reshape(NSEQ * S, D).T) for c in range(ncores)]
    per = per_launch or NL
    for L0 in range(0, NL, per):
        n = min(per, NL - L0)
        xT_list = launch(inp, xT_list, S, B, L0, n, ncores, final=(L0 + n == NL), **kw)
    out = np.empty((B, S, D), np.float32)
    for c in range(ncores):
        out[c * NSEQ:(c + 1) * NSEQ] = xT_list[c].T.reshape(NSEQ, S, D)
    return out


def kernel(**inputs):
    return run(inputs, 4096, 16, 4, 8, per_launch=1)
```

```python
import contextlib
import numpy as np
import concourse.bass as bass
import concourse.mybir as mybir
from concourse.bass_utils import run_bass_kernel_spmd

F32 = mybir.dt.float32
BF16 = mybir.dt.bfloat16
I32 = mybir.dt.int32
AF = mybir.ActivationFunctionType
ALU = mybir.AluOpType

D = 1024
DFF = 2816
NFC = 8
NC_FF = 22
TB = 512
EPS = 1e-6
ENGS = ("pe", "act", "dve", "pool", "sp")
NSLOT = 8

G_FFN1, G_MIX, G_FFN2, G_QL, G_KVL, G_CW, G_CB, G_AH, G_MH = 0, 8, 16, 24, 26, 27, 43, 47, 55
NGC = 59


class Res:
    __slots__ = ("w", "r", "excl")

    def __init__(self, excl=False):
        self.w = None
        self.r = []
        self.excl = excl


class Op:
    __slots__ = ("eng", "fn", "deps", "dma", "sig", "sem", "val", "slot")

    def __init__(self, eng, fn, dma):
        self.eng = eng
        self.fn = fn
        self.dma = dma
        self.deps = []
        self.sig = False
        self.sem = None
        self.val = 0
        self.slot = -1


class Prog:
    def __init__(self, nc):
        self.nc = nc
        self.ops = {e: [] for e in ENGS}
        self.dma_count = {e: 0 for e in ENGS}
        self.last_dma_in_slot = {}

    def op(self, eng, fn, reads=(), writes=(), dma=False):
        o = Op(eng, fn, dma)
        deps = []
        ex = [r for r in reads if r.excl]
        if ex:
            reads = [r for r in reads if not r.excl]
            writes = list(writes) + ex
        for r in reads:
            if r.w is not None:
                deps.append(r.w)
        for r in writes:
            if r.w is not None:
                deps.append(r.w)
            deps.extend(r.r)
        for d in deps:
            if d.eng == eng and not d.dma and not dma and eng == "pe":
                continue
            if d not in o.deps:
                o.deps.append(d)
                d.sig = True
        if dma:
            k = self.dma_count[eng]
            self.dma_count[eng] += 1
            o.slot = k % NSLOT
            prev = self.last_dma_in_slot.get((eng, o.slot))
            if prev is not None and prev not in o.deps:
                o.deps.append(prev)
            self.last_dma_in_slot[(eng, o.slot)] = o
            o.sig = True
        for r in reads:
            r.r.append(o)
        for r in writes:
            r.w = o
            r.r = []
        self.ops[eng].append(o)
        return o

    def emit(self, final_waits=()):
        nc = self.nc
        with contextlib.ExitStack() as st:
            sems = {e: st.enter_context(nc.semaphore("s_" + e)) for e in ENGS}
            dsems = {(e, s): st.enter_context(nc.semaphore(f"d_{e}_{s}"))
                     for e in ENGS if self.dma_count[e] > 0 for s in range(NSLOT)}
            cnt = {e: 0 for e in ENGS}
            dcnt = {}
            for e in ENGS:
                for o in self.ops[e]:
                    if o.dma:
                        key = (e, o.slot)
                        dcnt[key] = dcnt.get(key, 0) + 16
                        o.sem = dsems[key]
                        o.val = dcnt[key]
                    elif o.sig:
                        cnt[e] += 1
                        o.sem = sems[e]
                        o.val = cnt[e]
            block = st.enter_context(nc.Block())
            engobj = {"pe": nc.tensor, "act": nc.scalar, "dve": nc.vector,
                      "pool": nc.gpsimd, "sp": nc.sync}
            finals = list(final_waits)

            def run(e):
                eo = engobj[e]
                waited = {}
                for o in self.ops[e]:
                    need = {}
                    for d in o.deps:
                        key = id(d.sem)
                        if d.val > need.get(key, (0, None))[0]:
                            need[key] = (d.val, d.sem)
                    for key, (val, sem) in need.items():
                        if waited.get(key, 0) >= val:
                            continue
                        eo.wait_ge(sem, val)
                        waited[key] = val
                    ins = o.fn(eo)
                    if o.sig:
                        ins.then_inc(o.sem, 16 if o.dma else 1)
                if e == "sp":
                    for d in finals:
                        if waited.get(id(d.sem), 0) >= d.val:
                            continue
                        eo.wait_ge(d.sem, d.val)
                        waited[id(d.sem)] = d.val

            @block.tensor
            def _(eng):
                run("pe")

            @block.scalar
            def _(eng):
                run("act")

            @block.vector
            def _(eng):
                run("dve")

            @block.gpsimd
            def _(eng):
                run("pool")

            @block.sync
            def _(eng):
                run("sp")


class Ring:
    def __init__(self, tiles):
        self.tiles = tiles
        self.res = [Res() for _ in tiles]
        self.i = 0

    def next(self):
        k = self.i % len(self.tiles)
        self.i += 1
        return self.tiles[k], self.res[k]


NWT = 18
NWS = NWT // 2
PI = 3.14159265358979
TWO_PI = 6.28318530717959
ATT_SCALE = 96.0 ** -0.5
MSTAGE = 9


def build(S, NSEQ, NL, do_mixer=True, do_mlstm=True, do_attn=True, final=True):
    nc = bass.Bass("TRN2", target_bir_lowering=False)
    NT = S * NSEQ
    NB = S // TB
    NCH = S // 128

    def din(name, shape, dt=F32):
        return nc.dram_tensor(name, list(shape), dt, kind="ExternalInput").ap()

    def dscr(name, shape, dt):
        return nc.dram_tensor(name, list(shape), dt, kind="Internal").ap()

    xT = din("xT", [D, NT])
    pos = din("pos", [1, NT], I32)
    invf = din("invf", [96, 2])
    wgu = din("wgu", [NL, 2, NC_FF, 128, 2048])
    wd = din("wd", [NL, 2, NFC, 128, DFF])
    win = din("win", [NL, NWS, 128, 2048])
    wout = din("wout", [NL, NFC, 128, 1536])
    uq = din("uq", [NL, 128, 3072])
    ukv = din("ukv", [NL, 128, 1024])
    gcols = din("gcols", [NL, 128, NGC])
    gfin = din("gfin", [128, NFC])
    gbias = din("gbias", [NL, 8])
    outT = nc.dram_tensor("outT", [D, NT], F32, kind="ExternalOutput").ap()

    wgu_b = dscr("wgu_b", [NL, 2, NC_FF, 128, 2048], BF16)
    wd_b = dscr("wd_b", [NL, 2, NFC, 128, DFF], BF16)
    win_b = dscr("win_b", [NL, NWS, 128, 2048], BF16)
    wout_b = dscr("wout_b", [NL, NFC, 128, 1536], BF16)
    xs = [dscr("xs0", [D, NT], F32), dscr("xs1", [D, NT], F32)]
    rope_d = dscr("rope_d", [2, 96, NT], F32)
    kc_d = dscr("kc_d", [NSEQ, 8, 96, S], BF16)
    vc_d = dscr("vc_d", [NSEQ, 8, 128, NCH, 65], BF16)

    P = Prog(nc)
    st = contextlib.ExitStack()
    with st:
        def sb(name, shape, dt=F32):
            return st.enter_context(nc.sbuf_tensor(name, list(shape), dt))

        x_sb = sb("x_sb", [128, NFC, TB]); r_x = [Res() for _ in range(NFC)]
        hT = sb("hT", [128, NFC, TB], BF16); r_h = [Res() for _ in range(NFC)]
        aT = sb("aT", [128, NC_FF, TB], BF16); r_a = [Res() for _ in range(NC_FF)]
        rstd = sb("rstd", [128, TB]); r_rstd = Res()
        sq_ring = Ring([sb(f"sq{i}", [128, TB], BF16) for i in range(2)])
        e_ring = Ring([sb(f"e{i}", [128, TB]) for i in range(2)])
        t_ring = Ring([sb(f"t{i}", [128, TB]) for i in range(2)])
        big_ring = Ring([sb(f"wb{i}", [128, 2048], BF16) for i in range(2)])
        small_ring = Ring([sb(f"ws{i}", [128, DFF], BF16) for i in range(2)])
        onesB = sb("onesB", [128, 128], BF16); r_const = Res()
        gc = sb("gc", [128, NL, NGC]); gf = sb("gf", [128, NFC])
        ostage = Ring([sb(f"ost{i}", [128, TB]) for i in range(2)])

        ps = [st.enter_context(nc.psum_tensor(f"ps{i}", [128, TB], F32)) for i in range(8)]
        r_ps = [[Res(excl=True)] * 4 for _ in range(8)]

        P.op("pool", lambda e: e.memset(onesB[:], 1.0), writes=[r_const])
        P.op("sp", lambda e: e.dma_start(out=gc[:], in_=gcols.rearrange("l p c -> p l c")), writes=[r_const], dma=True)
        P.op("sp", lambda e: e.dma_start(out=gf[:], in_=gfin), writes=[r_const], dma=True)

        if do_mixer:
            cqn = sb("cqn", [128, 2, TB], BF16); r_cqn = Res()
            ckvn = sb("ckvn", [128, TB], BF16); r_ckvn = Res()
            cs = sb("cs", [96, 2, TB]); r_cs = Res()
            KT = sb("KT", [96, 8, TB], BF16); r_KT = [Res() for _ in range(8)]
            VT = sb("VT", [128, 8, 4, 65], BF16); r_VT = Res()
            QT = sb("QT", [96, 8, TB], BF16); r_QT = [Res() for _ in range(8)]
            KH = Ring([sb(f"KH{i}", [96, TB], BF16) for i in range(3)])
            VH = Ring([sb(f"VH{i}", [128, 4, 65], BF16) for i in range(3)])
            PT = Ring([sb(f"PT{i}", [128, TB], BF16) for i in range(3)])
            sq65 = sb("sq65", [65, TB], BF16); r_sq65 = Res()
            NormW = sb("NormW", [65, 64], BF16)
            yA = sb("yA", [64, 8, TB], BF16); r_yA = [Res() for _ in range(8)]
            yM = sb("yM", [128, 4, TB], BF16); r_yM = [Res() for _ in range(4)]
            uq_sb = sb("uq_sb", [128, 2, 8, 192], BF16); r_uq = Res()
            ukv_sb = sb("ukv_sb", [128, 8, 128], BF16); r_ukv = Res()
            gb = sb("gb", [128, NL, 8])
            invf_sb = sb("invf_sb", [96, 2])
            TriB = sb("TriB", [128, 128], BF16)
            U = sb("U", [128, 4, TB + 3]); r_U = [Res() for _ in range(4)]
            qcT = sb("qcT", [128, 2, TB], BF16); r_qcT = [Res() for _ in range(2)]
            kTm = sb("kTm", [128, 2, TB], BF16); r_kTm = [Res() for _ in range(2)]
            so = sb("so", [128, 4, TB], BF16); r_so = [Res() for _ in range(4)]
            hTm = sb("hTm", [128, 4, TB]); r_hTm = [Res() for _ in range(4)]
            Vm = sb("Vm", [128, 4, 4, 129], BF16); r_Vm = [Res() for _ in range(4)]
            gsb = sb("gsb", [128, 4, 8]); r_gsb = Res()
            TriF = sb("TriF", [128, 128]); OnesF = sb("OnesF", [128, 128]); IdentF = sb("IdentF", [128, 128])
            IdentB = sb("IdentB", [128, 128], BF16); MaskNeg = sb("MaskNeg", [128, 128])
            nlf = Ring([sb(f"nlf{i}", [128, 4]) for i in range(2)])
            bias_s = Ring([sb(f"bias_s{i}", [128, 4]) for i in range(2)])
            wl = Ring([sb(f"wl{i}", [128, 4]) for i in range(2)])
            ebend = Ring([sb(f"ebend{i}", [128, 4]) for i in range(2)])
            ktok = Ring([sb(f"ktok{i}", [128, 256], BF16) for i in range(2)])
            nrep = Ring([sb(f"nrep{i}", [128, 128]) for i in range(2)])
            DT = Ring([sb(f"DT{i}", [128, 128]) for i in range(2)])
            EE = Ring([sb(f"EE{i}", [128, 128]) for i in range(2)])
            pTm = Ring([sb(f"pTm{i}", [128, 128], BF16) for i in range(2)])
            qb = Ring([sb(f"qb{i}", [128, 128], BF16) for i in range(2)])
            dn = Ring([sb(f"dn{i}", [128, 128]) for i in range(2)])
            kw = Ring([sb(f"kw{i}", [128, 64], BF16) for i in range(2)])
            Cst = sb("Cst", [128, 2, 129]); r_Cst = [Res() for _ in range(4)]
            Cb = sb("Cb", [128, 2, 128], BF16); nrepB = sb("nrepB", [128, 2, 128], BF16)
            r_Cb = [Res() for _ in range(4)]

            pl = lambda fn, **k: P.op("pool", fn, **k)
            pl(lambda e: e.memset(NormW[0:64, :], 1.0 / 64), writes=[r_const])
            pl(lambda e: e.memset(NormW[64:65, :], EPS), writes=[r_const])
            pl(lambda e: e.memset(OnesF[:], 1.0), writes=[r_const])
            pl(lambda e: e.memset(MaskNeg[:], 0.0), writes=[r_const])
            pl(lambda e: e.memset(VT[:], 1.0), writes=[r_VT])
            pl(lambda e: e.memset(Vm[:], 1.0), writes=r_Vm)
            pl(lambda e: e.memset(yA[:], 0.0), writes=r_yA)
            pl(lambda e: e.memset(yM[:], 0.0), writes=r_yM)
            pl(lambda e: e.affine_select(out=TriF[:], in_=OnesF[:], pattern=[[1, 128]], compare_op=ALU.is_ge,
                                         fill=0.0, base=0, channel_multiplier=-1), reads=[r_const], writes=[r_const])
            pl(lambda e: e.affine_select(out=MaskNeg[:], in_=MaskNeg[:], pattern=[[1, 128]], compare_op=ALU.is_ge,
                                         fill=-30000.0, base=0, channel_multiplier=-1), reads=[r_const], writes=[r_const])
            pl(lambda e: e.affine_select(out=IdentF[:], in_=OnesF[:], pattern=[[1, 128]], compare_op=ALU.is_equal,
                                         fill=0.0, base=0, channel_multiplier=-1), reads=[r_const], writes=[r_const])
            pl(lambda e: e.tensor_copy(out=TriB[:], in_=TriF[:]), reads=[r_const], writes=[r_const])
            pl(lambda e: e.tensor_copy(out=IdentB[:], in_=IdentF[:]), reads=[r_const], writes=[r_const])
            P.op("sp", lambda e: e.dma_start(out=invf_sb[:], in_=invf), writes=[r_const], dma=True)
            for l in range(NL):
                P.op("sp", lambda e, l=l: e.dma_start(out=gb[:, l, :], in_=gbias[l].partition_broadcast(128)),
                     writes=[r_const], dma=True)

            r_rope = Res()
            a, bq, cq_ = e_ring.tiles[0][0:96, :], e_ring.tiles[1][0:96, :], t_ring.tiles[0][0:96, :]
            rpi = t_ring.tiles[1][0:96, :].bitcast(I32)
            r_a_, r_b_, r_c_, r_i_ = e_ring.res[0], e_ring.res[1], t_ring.res[0], t_ring.res[1]
            for ci in range(NT // TB):
                c0 = ci * TB
                P.op("sp", lambda e, c0=c0: e.dma_start(out=rpi, in_=pos[0, c0:c0 + TB].partition_broadcast(96)),
                     writes=[r_i_], dma=True)
                dv = lambda fn, **k: P.op("dve", fn, **k)
                dv(lambda e: e.tensor_copy(out=a, in_=rpi), reads=[r_i_], writes=[r_a_])
                dv(lambda e: e.tensor_scalar(out=a, in0=a, scalar1=invf_sb[:, 0:1], scalar2=None, op0=ALU.mult),
                   reads=[r_a_, r_const], writes=[r_a_])
                for which in range(2):
                    sh = PI / 2 if which == 0 else 0.0
                    dv(lambda e, sh=sh: e.tensor_scalar(out=bq, in0=a, scalar1=sh, scalar2=1.0 / TWO_PI, op0=ALU.add, op1=ALU.mult),
                       reads=[r_a_], writes=[r_b_])
                    dv(lambda e: e.tensor_copy(out=rpi, in_=bq), reads=[r_b_], writes=[r_i_])
                    dv(lambda e: e.tensor_copy(out=bq, in_=rpi), reads=[r_i_], writes=[r_b_])
                    dv(lambda e: e.scalar_tensor_tensor(out=bq, in0=bq, scalar=-TWO_PI, in1=a, op0=ALU.mult, op1=ALU.add),
                       reads=[r_b_, r_a_], writes=[r_b_])
                    if which == 0:
                        dv(lambda e, sh=sh: e.tensor_scalar(out=bq, in0=bq, scalar1=sh, scalar2=None, op0=ALU.add),
                           reads=[r_b_], writes=[r_b_])
                    dv(lambda e: e.tensor_scalar(out=cq_, in0=bq, scalar1=PI, scalar2=-TWO_PI, op0=ALU.is_gt, op1=ALU.mult),
                       reads=[r_b_], writes=[r_c_])
                    dv(lambda e: e.tensor_tensor(out=bq, in0=bq, in1=cq_, op=ALU.add), reads=[r_b_, r_c_], writes=[r_b_])
                    dv(lambda e: e.tensor_scalar(out=cq_, in0=bq, scalar1=-PI, scalar2=TWO_PI, op0=ALU.is_lt, op1=ALU.mult),
                       reads=[r_b_], writes=[r_c_])
                    dv(lambda e: e.tensor_tensor(out=bq, in0=bq, in1=cq_, op=ALU.add), reads=[r_b_, r_c_], writes=[r_b_])
                    dv(lambda e: e.tensor_scalar(out=bq, in0=bq, scalar1=-3.14159, scalar2=3.14159, op0=ALU.max, op1=ALU.min),
                       reads=[r_b_], writes=[r_b_])
                    if which == 0:
                        P.op("act", lambda e: e.activation(out=cq_, in_=bq, func=AF.Sin), reads=[r_b_], writes=[r_c_])
                    else:
                        P.op("act", lambda e: e.activation(out=cq_, in_=bq, func=AF.Sin, scale=invf_sb[:, 1:2]),
                             reads=[r_b_, r_const], writes=[r_c_])
                    P.op("sp", lambda e, which=which, c0=c0: e.dma_start(out=rope_d[which, :, c0:c0 + TB], in_=cq_),
                         reads=[r_c_], writes=[r_rope], dma=True)

        r_w = {}

        def conv_ops(l):
            ops = []

            def add(key, dst, src, n):
                r_w[key] = Res()
                for h0 in range(0, n, 2048):
                    h1 = min(n, h0 + 2048)
                    ops.append((key, dst[:, h0:h1], src[:, h0:h1]))
            for f in range(2):
                for c in range(NC_FF):
                    add(("gu", l, f, c), wgu_b[l, f, c], wgu[l, f, c], 2048)
                for fc in range(NFC):
                    add(("d", l, f, fc), wd_b[l, f, fc], wd[l, f, fc], DFF)
                if f == 0 and do_mixer:
                    for j in range(NWS):
                        add(("in", l, j), win_b[l, j], win[l, j], 2048)
                    for fc in range(NFC):
                        add(("out", l, fc), wout_b[l, fc], wout[l, fc], 1536)
            return ops

        def emit_conv(items):
            for key, dst, src in items:
                P.op("pool", lambda e, dst=dst, src=src: e.dma_start(out=dst, in_=src),
                     writes=[r_w[key]], dma=True)

        def bc_rstd(psrc, r_src, n, dst, r_dst):
            P.op("act", lambda e: e.activation(out=dst, in_=psrc, func=AF.Ln, bias=EPS, scale=1.0 / n),
                 reads=r_src, writes=[r_dst])
            P.op("act", lambda e: e.activation(out=dst, in_=dst, func=AF.Exp, scale=-0.5),
                 reads=[r_dst], writes=[r_dst])

        def sigmoid_chain(dst, r_dst, src, r_src, final_out=None, r_final=None):
            P.op("act", lambda e: e.activation(out=dst, in_=src, func=AF.Exp, scale=-1.0), reads=r_src, writes=[r_dst])
            P.op("act", lambda e: e.activation(out=dst, in_=dst, func=AF.Ln, bias=1.0, scale=1.0), reads=[r_dst], writes=[r_dst])
            if final_out is None:
                P.op("act", lambda e: e.activation(out=dst, in_=dst, func=AF.Exp, scale=-1.0), reads=[r_dst], writes=[r_dst])
            else:
                P.op("act", lambda e: e.activation(out=final_out, in_=dst, func=AF.Exp, scale=-1.0), reads=[r_dst], writes=r_final)

        def norm_stats():
            for fc in range(NFC):
                sq, r_sq = sq_ring.next()
                P.op("act", lambda e, sq=sq, fc=fc: e.activation(out=sq[:], in_=x_sb[:, fc, :], func=AF.Square),
                     reads=[r_x[fc]], writes=[r_sq])
                P.op("pe", lambda e, sq=sq, fc=fc: e.matmul(ps[6][:], lhsT=onesB[:], rhs=sq[:], start=(fc == 0), stop=(fc == NFC - 1)),
                     reads=[r_sq, r_const], writes=r_ps[6])
            bc_rstd(ps[6][:], r_ps[6], D, rstd[:], r_rstd)

        def norm_to_hT(l, gbase):
            norm_stats()
            for fc in range(NFC):
                P.op("dve", lambda e, fc=fc: e.scalar_tensor_tensor(
                    out=hT[:, fc, :], in0=x_sb[:, fc, :], scalar=gc[:, l, gbase + fc:gbase + fc + 1],
                    in1=rstd[:], op0=ALU.mult, op1=ALU.mult),
                    reads=[r_x[fc], r_rstd, r_const], writes=[r_h[fc]])

        gu_alt = [0]

        def ffn(l, f):
            norm_to_hT(l, G_FFN1 if f == 0 else G_FFN2)
            for c in range(NC_FF):
                slab, r_slab = big_ring.next()
                P.op("sp", lambda e, slab=slab, c=c: e.dma_start(out=slab[:], in_=wgu_b[l, f, c]),
                     reads=[r_w[("gu", l, f, c)]], writes=[r_slab], dma=True)
                k = gu_alt[0] % 2
                gu_alt[0] += 1
                G, U_ = ps[2 * k], ps[2 * k + 1]
                rG, rU = r_ps[2 * k], r_ps[2 * k + 1]
                for kc in range(NFC):
                    P.op("pe", lambda e, slab=slab, kc=kc, G=G: e.matmul(
                        G[:], lhsT=slab[:, kc * 128:(kc + 1) * 128], rhs=hT[:, kc, :], start=(kc == 0), stop=(kc == NFC - 1)),
                        reads=[r_slab, r_h[kc]], writes=rG)
                for kc in range(NFC):
                    P.op("pe", lambda e, slab=slab, kc=kc, U_=U_: e.matmul(
                        U_[:], lhsT=slab[:, (8 + kc) * 128:(9 + kc) * 128], rhs=hT[:, kc, :], start=(kc == 0), stop=(kc == NFC - 1)),
                        reads=[r_slab, r_h[kc]], writes=rU)
                e1, r_e1 = e_ring.next()
                t1, r_t1 = t_ring.next()
                sigmoid_chain(e1[:], r_e1, G[:], rG)
                P.op("dve", lambda e, e1=e1, t1=t1, G=G: e.tensor_tensor(out=t1[:], in0=G[:], in1=e1[:], op=ALU.mult),
                     reads=rG + [r_e1], writes=[r_t1])
                P.op("dve", lambda e, t1=t1, U_=U_, c=c: e.tensor_tensor(out=aT[:, c, :], in0=U_[:], in1=t1[:], op=ALU.mult),
                     reads=rU + [r_t1], writes=[r_a[c]])
            for fc in range(NFC):
                slab, r_slab = small_ring.next()
                P.op("sp", lambda e, slab=slab, fc=fc: e.dma_start(out=slab[:], in_=wd_b[l, f, fc]),
                     reads=[r_w[("d", l, f, fc)]], writes=[r_slab], dma=True)
                Y, rY = ps[4 + fc % 2], r_ps[4 + fc % 2]
                for c in range(NC_FF):
                    P.op("pe", lambda e, slab=slab, c=c, Y=Y: e.matmul(
                        Y[:], lhsT=slab[:, c * 128:(c + 1) * 128], rhs=aT[:, c, :], start=(c == 0), stop=(c == NC_FF - 1)),
                        reads=[r_slab, r_a[c]], writes=rY)
                P.op("dve", lambda e, Y=Y, fc=fc: e.scalar_tensor_tensor(
                    out=x_sb[:, fc, :], in0=Y[:], scalar=0.5, in1=x_sb[:, fc, :], op0=ALU.mult, op1=ALU.add),
                    reads=rY + [r_x[fc]], writes=[r_x[fc]])

        r_kc = [[Res() for _ in range(NB)] for _ in range(NSEQ)]
        r_vc = [[Res() for _ in range(NB)] for _ in range(NSEQ)]

        def mixer(l, s, b):
            t0 = s * S + b * TB
            dv = lambda fn, **k: P.op("dve", fn, **k)
            ac = lambda fn, **k: P.op("act", fn, **k)
            pe = lambda fn, **k: P.op("pe", fn, **k)
            pl = lambda fn, **k: P.op("pool", fn, **k)
            if s == 0 and b == 0:
                pl(lambda e: e.dma_start(out=uq_sb[:], in_=uq[l]), writes=[r_uq], dma=True)
                pl(lambda e: e.dma_start(out=ukv_sb[:], in_=ukv[l]), writes=[r_ukv], dma=True)
            norm_to_hT(l, G_MIX)
            P.op("sp", lambda e: e.dma_start(out=cs[:], in_=rope_d[:, :, t0:t0 + TB].rearrange("w p t -> p w t")),
                 reads=[r_rope], writes=[r_cs], dma=True)
            if b == 0:
                dv(lambda e: e.memset(U[:, :, 0:3], 0.0), writes=r_U)
                dv(lambda e: e.memset(Cst[:], 0.0), writes=r_Cst)
                dv(lambda e: e.memset(Cb[:], 0.0), writes=r_Cb)
                dv(lambda e: e.memset(nrepB[:], 0.0), writes=r_Cb)

            def proj(slab, off, out_ap, r_out, M=128):
                for kc in range(NFC):
                    pe(lambda e, kc=kc: e.matmul(out_ap, lhsT=slab[:, off + kc * 128: off + kc * 128 + M], rhs=hT[:, kc, :],
                                                 start=(kc == 0), stop=(kc == NFC - 1)),
                       reads=[cur_r_slab[0], r_h[kc]], writes=r_out)

            cur_r_slab = [None]
            for j in range(NWS):
                slab, r_slab = big_ring.next()
                cur_r_slab[0] = r_slab
                P.op("sp", lambda e, slab=slab, j=j: e.dma_start(out=slab[:], in_=win_b[l, j]),
                     reads=[r_w[("in", l, j)]], writes=[r_slab], dma=True)
                for tt in (2 * j, 2 * j + 1):
                    off = (tt % 2) * 1024
                    if tt in (0, 1):
                        proj(slab, off, ps[tt][:], r_ps[tt])
                        if tt == 1:
                            for q in range(2):
                                sq, r_sq = sq_ring.next()
                                ac(lambda e, sq=sq, q=q: e.activation(out=sq[:], in_=ps[q][:], func=AF.Square), reads=r_ps[q], writes=[r_sq])
                                pe(lambda e, sq=sq, q=q: e.matmul(ps[6][:], lhsT=onesB[:], rhs=sq[:], start=(q == 0), stop=(q == 1)),
                                   reads=[r_sq, r_const], writes=r_ps[6])
                            bc_rstd(ps[6][:], r_ps[6], 256, rstd[:], r_rstd)
                            for q in range(2):
                                dv(lambda e, q=q: e.scalar_tensor_tensor(out=cqn[:, q, :], in0=ps[q][:], scalar=gc[:, l, G_QL + q:G_QL + q + 1],
                                                                      in1=rstd[:], op0=ALU.mult, op1=ALU.mult),
                                   reads=r_ps[q] + [r_rstd, r_const], writes=[r_cqn])
                    elif tt == 2:
                        proj(slab, off, ps[2][:], r_ps[2])
                        sq, r_sq = sq_ring.next()
                        ac(lambda e, sq=sq: e.activation(out=sq[:], in_=ps[2][:], func=AF.Square), reads=r_ps[2], writes=[r_sq])
                        pe(lambda e, sq=sq: e.matmul(ps[7][:], lhsT=onesB[:], rhs=sq[:], start=True, stop=True),
                           reads=[r_sq, r_const], writes=r_ps[7])
                        e1, r_e1 = e_ring.next()
                        bc_rstd(ps[7][:], r_ps[7], 128, e1[:], r_e1)
                        dv(lambda e, e1=e1: e.scalar_tensor_tensor(out=ckvn[:], in0=ps[2][:], scalar=gc[:, l, G_KVL:G_KVL + 1],
                                                                in1=e1[:], op0=ALU.mult, op1=ALU.mult),
                           reads=r_ps[2] + [r_e1, r_const], writes=[r_ckvn])
                    elif tt in (3, 4):
                        proj(slab, off, ps[tt][0:96, :], r_ps[tt], M=96)
                        if tt == 4:
                            ta, r_ta = t_ring.next()
                            tb_, r_tb = t_ring.next()
                            dv(lambda e, ta=ta: e.tensor_tensor(out=ta[64:96, :], in0=ps[3][64:96, :], in1=cs[64:96, 0, :], op=ALU.mult),
                               reads=r_ps[3] + [r_cs], writes=[r_ta])
                            dv(lambda e, tb_=tb_: e.tensor_tensor(out=tb_[64:96, :], in0=ps[4][64:96, :], in1=cs[64:96, 1, :], op=ALU.mult),
                               reads=r_ps[4] + [r_cs], writes=[r_tb])
                            dv(lambda e, ta=ta, tb_=tb_: e.tensor_tensor(out=KT[64:96, 0, :], in0=ta[64:96, :], in1=tb_[64:96, :], op=ALU.add),
                               reads=[r_ta, r_tb], writes=[r_KT[0]])
                            for h in range(1, 8):
                                pl(lambda e, h=h: e.tensor_copy(out=KT[64:96, h, :], in_=KT[64:96, 0, :]), reads=[r_KT[0]], writes=[r_KT[h]])
                    elif 5 <= tt <= 8:
                        i = tt - 5
                        pk = ps[i % 4]
                        proj(slab, off, pk[:], r_ps[i % 4])
                        ac(lambda e, i=i, pk=pk: e.activation(out=U[:, i, 3:TB + 3], in_=pk[:], func=AF.Copy), reads=r_ps[i % 4], writes=[r_U[i]])
                    elif 9 <= tt <= 12:
                        h = tt - 9
                        pk, rk = ps[4 + h % 2], r_ps[4 + h % 2]
                        proj(slab, off, pk[:], rk)
                        e1, r_e1 = e_ring.next()
                        sigmoid_chain(e1[:], r_e1, pk[:], rk, final_out=so[:, h, :], r_final=[r_so[h]])
                    elif tt == 13:
                        for jj in range(4):
                            for kc in range(NFC):
                                pe(lambda e, kc=kc, jj=jj, slab=slab, off=off: e.matmul(
                                    ps[6][:, jj * 8:(jj + 1) * 8], lhsT=hT[:, kc, jj * 128:(jj + 1) * 128],
                                    rhs=slab[:, off + kc * 128: off + kc * 128 + 8], start=(kc == 0), stop=(kc == NFC - 1)),
                                   reads=[r_slab, r_h[kc]], writes=[r_ps[6][0]])
                        for jj in range(4):
                            dv(lambda e, jj=jj: e.tensor_tensor(out=gsb[:, jj, :], in0=ps[6][:, jj * 8:(jj + 1) * 8], in1=gb[:, l, :], op=ALU.add),
                               reads=[r_ps[6][0], r_const], writes=[r_gsb])
                    else:
                        hh = tt - 14
                        pk, rk = ps[hh % 2], r_ps[hh % 2]
                        for jj in range(4):
                            for kc in range(NFC):
                                pe(lambda e, kc=kc, jj=jj, slab=slab, off=off, pk=pk: e.matmul(
                                    pk[:, jj * 128:(jj + 1) * 128], lhsT=hT[:, kc, jj * 128:(jj + 1) * 128],
                                    rhs=slab[:, off + kc * 128: off + (kc + 1) * 128], start=(kc == 0), stop=(kc == NFC - 1)),
                                   reads=[r_slab, r_h[kc]], writes=rk)
                        ac(lambda e, hh=hh, pk=pk: e.activation(out=Vm[:, :, hh, 0:128], in_=pk[:].rearrange("p (j d) -> p j d", d=128), func=AF.Copy),
                           reads=rk, writes=[r_Vm[hh]])

            for h in range(8):
                pk, rk = ps[2 + h % 2], r_ps[2 + h % 2]
                pe(lambda e, h=h, pk=pk: e.matmul(pk[0:64, :], lhsT=ukv_sb[:, h, 0:64], rhs=ckvn[:], start=True, stop=True),
                   reads=[r_ukv, r_ckvn], writes=rk)
                ac(lambda e, h=h, pk=pk: e.activation(out=KT[0:64, h, :], in_=pk[0:64, :], func=AF.Copy), reads=rk, writes=[r_KT[h]])
            for jj in range(4):
                pk, rk = ps[4 + jj % 2], r_ps[4 + jj % 2]
                pe(lambda e, jj=jj, pk=pk: e.matmul(pk[:].rearrange("p (h d) -> p h d", d=64), lhsT=ckvn[:, jj * 128:(jj + 1) * 128],
                                                   rhs=ukv_sb[:, :, 64:128], start=True, stop=True),
                   reads=[r_ukv, r_ckvn], writes=rk)
                dv(lambda e, jj=jj, pk=pk: e.tensor_copy(out=VT[:, :, jj, 0:64], in_=pk[:].rearrange("p (h d) -> p h d", d=64)),
                   reads=rk, writes=[r_VT])
            P.op("sp", lambda e: e.dma_start(out=kc_d[s, :, :, t0 - s * S:t0 - s * S + TB].rearrange("h p t -> p h t"), in_=KT[:]),
                 reads=r_KT, writes=[r_kc[s][b]], dma=True)
            P.op("sp", lambda e: e.dma_start(out=vc_d[s, :, :, 4 * b:4 * b + 4, :].rearrange("h p c e -> p h c e"), in_=VT[:]),
                 reads=[r_VT], writes=[r_vc[s][b]], dma=True)

            for h in range(8):
                pq, rq = ps[2 * (h % 2)], r_ps[2 * (h % 2)]
                pr_, rr = ps[2 * (h % 2) + 1], r_ps[2 * (h % 2) + 1]
                for kc in range(2):
                    pe(lambda e, h=h, kc=kc, pq=pq: e.matmul(pq[0:96, :], lhsT=uq_sb[:, kc, h, 0:96], rhs=cqn[:, kc, :], start=(kc == 0), stop=(kc == 1)),
                       reads=[r_uq, r_cqn], writes=rq)
                for kc in range(2):
                    pe(lambda e, h=h, kc=kc, pr_=pr_: e.matmul(pr_[0:96, :], lhsT=uq_sb[:, kc, h, 96:192], rhs=cqn[:, kc, :], start=(kc == 0), stop=(kc == 1)),
                       reads=[r_uq, r_cqn], writes=rr)
                ac(lambda e, h=h, pq=pq: e.activation(out=QT[0:64, h, :], in_=pq[0:64, :], func=AF.Copy), reads=rq, writes=[r_QT[h]])
                ta, r_ta = t_ring.next()
                tb_, r_tb = t_ring.next()
                dv(lambda e, ta=ta, pq=pq: e.tensor_tensor(out=ta[64:96, :], in0=pq[64:96, :], in1=cs[64:96, 0, :], op=ALU.mult),
                   reads=rq + [r_cs], writes=[r_ta])
                dv(lambda e, tb_=tb_, pr_=pr_: e.tensor_tensor(out=tb_[64:96, :], in0=pr_[64:96, :], in1=cs[64:96, 1, :], op=ALU.mult),
                   reads=rr + [r_cs], writes=[r_tb])
                dv(lambda e, ta=ta, tb_=tb_, h=h: e.tensor_tensor(out=QT[64:96, h, :], in0=ta[64:96, :], in1=tb_[64:96, :], op=ALU.add),
                   reads=[r_ta, r_tb], writes=[r_QT[h]])

            if do_attn:
                s_alt = 0
                for h in range(8):
                    pO, rO = ps[3 + h % 2], r_ps[3 + h % 2]
                    nk = 4 * (b + 1)
                    for kb in range(b + 1):
                        kh, r_kh = KH.next()
                        vh, r_vh = VH.next()
                        P.op("sp", lambda e, kh=kh, kb=kb, h=h: e.dma_start(out=kh[:], in_=kc_d[s, h, :, kb * TB:(kb + 1) * TB]),
                             reads=[r_kc[s][kb]], writes=[r_kh], dma=True)
                        P.op("sp", lambda e, vh=vh, kb=kb, h=h: e.dma_start(out=vh[:], in_=vc_d[s, h, :, 4 * kb:4 * kb + 4, :]),
                             reads=[r_vc[s][kb]], writes=[r_vh], dma=True)
                        for kk in range(4):
                            kc = kb * 4 + kk
                            d = kc - 4 * b
                            col0 = 128 * d if d > 0 else 0
                            pS, rS = ps[s_alt % 3], r_ps[s_alt % 3]
                            s_alt += 1
                            pt, r_pt = PT.next()
                            pe(lambda e, kh=kh, kk=kk, h=h, col0=col0, pS=pS: e.matmul(
                                pS[:, col0:TB], lhsT=kh[:, kk * 128:(kk + 1) * 128], rhs=QT[:, h, col0:TB], start=True, stop=True),
                               reads=[r_kh, r_QT[h]], writes=rS)
                            ac(lambda e, pt=pt, pS=pS, col0=col0: e.activation(out=pt[:, col0:TB], in_=pS[:, col0:TB], func=AF.Exp, scale=ATT_SCALE),
                               reads=rS, writes=[r_pt])
                            if d >= 0:
                                dv(lambda e, pt=pt, col0=col0: e.tensor_tensor(out=pt[:, col0:col0 + 128], in0=pt[:, col0:col0 + 128], in1=TriB[:], op=ALU.mult),
                                   reads=[r_pt, r_const], writes=[r_pt])
                            pe(lambda e, vh=vh, kk=kk, pt=pt, col0=col0, kc=kc, nk=nk, pO=pO: e.matmul(
                                pO[0:65, col0:TB], lhsT=vh[:, kk, :], rhs=pt[:, col0:TB], start=(kc == 0), stop=(kc == nk - 1)),
                               reads=[r_vh, r_pt], writes=rO)
                    ac(lambda e, pO=pO: e.activation(out=sq65[:], in_=pO[0:65, :], func=AF.Square), reads=rO, writes=[r_sq65])
                    pe(lambda e: e.matmul(ps[5][0:64, :], lhsT=NormW[:], rhs=sq65[:], start=True, stop=True),
                       reads=[r_sq65, r_const], writes=r_ps[5])
                    e1, r_e1 = e_ring.next()
                    P.op("act", lambda e, e1=e1: e.activation(out=e1[0:64, :], in_=ps[5][0:64, :], func=AF.Ln), reads=r_ps[5], writes=[r_e1])
                    P.op("act", lambda e, e1=e1: e.activation(out=e1[0:64, :], in_=e1[0:64, :], func=AF.Exp, scale=-0.5), reads=[r_e1], writes=[r_e1])
                    dv(lambda e, e1=e1, h=h, pO=pO: e.scalar_tensor_tensor(out=yA[:, h, :], in0=pO[0:64, :], scalar=gc[0:64, l, G_AH + h:G_AH + h + 1],
                                                                    in1=e1[0:64, :], op0=ALU.mult, op1=ALU.mult),
                       reads=rO + [r_e1, r_const], writes=[r_yA[h]])

            if do_mlstm:
                for i in range(4):
                    acc, r_acc = t_ring.next()
                    cw = lambda jj, i=i: gc[:, l, G_CW + i * 4 + jj:G_CW + i * 4 + jj + 1]
                    dv(lambda e, acc=acc, i=i, cw=cw: e.tensor_scalar(out=acc[:], in0=U[:, i, 0:TB], scalar1=cw(0),
                                                                      scalar2=gc[:, l, G_CB + i:G_CB + i + 1], op0=ALU.mult, op1=ALU.add),
                       reads=[r_U[i], r_const], writes=[r_acc])
                    for jj in range(1, 4):
                        dv(lambda e, acc=acc, i=i, jj=jj, cw=cw: e.scalar_tensor_tensor(out=acc[:], in0=U[:, i, jj:jj + TB], scalar=cw(jj),
                                                                                  in1=acc[:], op0=ALU.mult, op1=ALU.add),
                           reads=[r_U[i], r_acc, r_const], writes=[r_acc])
                    e1, r_e1 = e_ring.next()
                    sigmoid_chain(e1[:], r_e1, acc[:], [r_acc])
                    if i < 2:
                        dv(lambda e, acc=acc, e1=e1, i=i: e.scalar_tensor_tensor(out=qcT[:, i, :], in0=acc[:], scalar=0.125, in1=e1[:], op0=ALU.mult, op1=ALU.mult),
                           reads=[r_acc, r_e1], writes=[r_qcT[i]])
                    else:
                        dv(lambda e, acc=acc, e1=e1, i=i: e.tensor_tensor(out=kTm[:, i - 2, :], in0=acc[:], in1=e1[:], op=ALU.mult),
                           reads=[r_acc, r_e1], writes=[r_kTm[i - 2]])
                    dv(lambda e, i=i: e.tensor_copy(out=U[:, i, 0:3], in_=U[:, i, TB:TB + 3]), reads=[r_U[i]], writes=[r_U[i]])

                for jj in range(4 if MSTAGE >= 2 else 0):
                    cols = slice(jj * 128, (jj + 1) * 128)
                    nl_, r_nl = nlf.next()
                    bs_, r_bs = bias_s.next()
                    wl_, r_wl = wl.next()
                    eb_, r_eb = ebend.next()
                    kt_, r_kt = ktok.next()
                    ac(lambda e, nl_=nl_, jj=jj: e.activation(out=nl_[:], in_=gsb[:, jj, 4:8], func=AF.Exp, scale=-1.0), reads=[r_gsb], writes=[r_nl])
                    ac(lambda e, nl_=nl_: e.activation(out=nl_[:], in_=nl_[:], func=AF.Ln, bias=1.0, scale=1.0), reads=[r_nl], writes=[r_nl])
                    rg0 = [r_ps[6][0]]
                    pe(lambda e, nl_=nl_: e.matmul(ps[6][:, 0:4], lhsT=TriF[:], rhs=nl_[:], start=True, stop=True), reads=[r_nl, r_const], writes=rg0)
                    pe(lambda e, nl_=nl_: e.matmul(ps[6][:, 4:8], lhsT=OnesF[:], rhs=nl_[:], start=True, stop=True), reads=[r_nl, r_const], writes=rg0)
                    dv(lambda e, bs_=bs_, jj=jj: e.tensor_tensor(out=bs_[:], in0=ps[6][:, 0:4], in1=gsb[:, jj, 0:4], op=ALU.add),
                       reads=rg0 + [r_gsb], writes=[r_bs])
                    dv(lambda e, bs_=bs_, wl_=wl_: e.tensor_tensor(out=wl_[:], in0=bs_[:], in1=ps[6][:, 4:8], op=ALU.subtract),
                       reads=rg0 + [r_bs], writes=[r_wl])
                    ac(lambda e, wl_=wl_: e.activation(out=wl_[:], in_=wl_[:], func=AF.Exp), reads=[r_wl], writes=[r_wl])
                    ac(lambda e, eb_=eb_: e.activation(out=eb_[:], in_=ps[6][:, 4:8], func=AF.Exp, scale=-1.0), reads=rg0, writes=[r_eb])
                    rg1 = r_ps[7]
                    ktv = ps[7][:, 0:256]
                    for i in range(2 if MSTAGE >= 3 else 0):
                        pe(lambda e, i=i, cols=cols, ktv=ktv: e.matmul(ktv[:, i * 128:(i + 1) * 128], lhsT=kTm[:, i, cols], rhs=IdentB[:], start=True, stop=True),
                           reads=[r_kTm[i], r_const], writes=rg1)
                    dv(lambda e, kt_=kt_, ktv=ktv: e.tensor_copy(out=kt_[:], in_=ktv), reads=rg1, writes=[r_kt])
                    for h in range(4 if MSTAGE >= 4 else 0):
                        po = (h % 2) * 64
                        prs = slice(po, po + 64)
                        ti = h // 2
                        pC, rC = ps[5], r_ps[5]
                        nr_, r_nr = nrep.next()
                        dt_, r_dt = DT.next()
                        ee_, r_ee = EE.next()
                        pt_, r_ptm = pTm.next()
                        qb_, r_qb = qb.next()
                        dn_, r_dn = dn.next()
                        kw_, r_kw = kw.next()
                        dv(lambda e, nr_=nr_, nl_=nl_, h=h: e.tensor_scalar(out=nr_[:], in0=OnesF[:], scalar1=nl_[:, h:h + 1], scalar2=-1.0, op0=ALU.mult, op1=ALU.mult),
                           reads=[r_nl, r_const], writes=[r_nr])
                        pe(lambda e, nr_=nr_: e.matmul(ps[1][:, 0:128], lhsT=nr_[:], rhs=TriF[:], start=True, stop=True), reads=[r_nr, r_const], writes=r_ps[1])
                        pe(lambda e, nr_=nr_: e.matmul(ps[0][:, 0:128], lhsT=nr_[:], rhs=TriF[:], start=True, stop=False), reads=[r_nr, r_const], writes=r_ps[0])
                        pe(lambda e: e.matmul(ps[0][:, 0:128], lhsT=IdentF[:], rhs=MaskNeg[:], start=False, stop=True), reads=[r_const], writes=r_ps[0])
                        pe(lambda e, prs=prs, ti=ti, cols=cols: e.matmul(ps[2][:, 0:128], lhsT=kTm[prs, ti, cols], rhs=qcT[prs, ti, cols], start=True, stop=True),
                           reads=[r_kTm[ti], r_qcT[ti]], writes=r_ps[2])
                        ac(lambda e, dt_=dt_, bs_=bs_, h=h: e.activation(out=dt_[:], in_=ps[0][:, 0:128], func=AF.Exp, bias=bs_[:, h:h + 1], scale=1.0),
                           reads=r_ps[0] + [r_bs], writes=[r_dt])
                        ac(lambda e, ee_=ee_: e.activation(out=ee_[:], in_=ps[1][:, 0:128], func=AF.Exp), reads=r_ps[1], writes=[r_ee])
                        dv(lambda e, pt_=pt_, dt_=dt_: e.tensor_tensor(out=pt_[:], in0=ps[2][:, 0:128], in1=dt_[:], op=ALU.mult),
                           reads=r_ps[2] + [r_dt], writes=[r_ptm])
                        dv(lambda e, qb_=qb_, ee_=ee_, prs=prs, ti=ti, cols=cols: e.tensor_tensor(out=qb_[prs, :], in0=qcT[prs, ti, cols], in1=ee_[prs, :], op=ALU.mult),
                           reads=[r_qcT[ti], r_ee], writes=[r_qb])
                        pe(lambda e, jj=jj, h=h, pt_=pt_: e.matmul(ps[3][:, 0:128], lhsT=Vm[:, jj, h, 0:128], rhs=pt_[:], start=True, stop=False),
                           reads=[r_Vm[h], r_ptm], writes=r_ps[3])
                        pe(lambda e, prs=prs, ti=ti, qb_=qb_: e.matmul(ps[3][:, 0:128], lhsT=Cb[prs, ti, :], rhs=qb_[prs, :], start=False, stop=True),
                           reads=[r_Cb[h], r_qb], writes=r_ps[3])
                        pe(lambda e, pt_=pt_: e.matmul(ps[4][:, 0:128], lhsT=onesB[:], rhs=pt_[:], start=True, stop=False),
                           reads=[r_const, r_ptm], writes=r_ps[4])
                        pe(lambda e, prs=prs, ti=ti, qb_=qb_: e.matmul(ps[4][:, 0:128], lhsT=nrepB[prs, ti, :], rhs=qb_[prs, :], start=False, stop=True),
                           reads=[r_Cb[h], r_qb], writes=r_ps[4])
                        ac(lambda e, dn_=dn_: e.activation(out=dn_[:], in_=ps[4][:, 0:128], func=AF.Abs), reads=r_ps[4], writes=[r_dn])
                        dv(lambda e, dn_=dn_: e.tensor_scalar(out=dn_[:], in0=dn_[:], scalar1=1.0, scalar2=None, op0=ALU.max),
                           reads=[r_dn], writes=[r_dn])
                        dv(lambda e, dn_=dn_: e.reciprocal(out=dn_[:], in_=dn_[:]), reads=[r_dn], writes=[r_dn])
                        dv(lambda e, dn_=dn_, h=h, cols=cols: e.tensor_tensor(out=hTm[:, h, cols], in0=ps[3][:, 0:128], in1=dn_[:], op=ALU.mult),
                           reads=r_ps[3] + [r_dn], writes=[r_hTm[h]])
                        if MSTAGE < 5:
                            continue
                        dv(lambda e, kw_=kw_, kt_=kt_, wl_=wl_, h=h: e.tensor_scalar(out=kw_[:], in0=kt_[:, h * 64:(h + 1) * 64], scalar1=wl_[:, h:h + 1], scalar2=None, op0=ALU.mult),
                           reads=[r_kt, r_wl], writes=[r_kw])
                        pe(lambda e, pC=pC, prs=prs, kw_=kw_, jj=jj, h=h: e.matmul(pC[prs, 0:129], lhsT=kw_[:], rhs=Vm[:, jj, h, :], start=True, stop=True),
                           reads=[r_kw, r_Vm[h]], writes=rC)
                        dv(lambda e, pC=pC, prs=prs, ti=ti, eb_=eb_, h=h: e.scalar_tensor_tensor(out=Cst[prs, ti, :], in0=Cst[prs, ti, :], scalar=eb_[prs, h:h + 1],
                                                                                     in1=pC[prs, 0:129], op0=ALU.mult, op1=ALU.add),
                           reads=rC + [r_eb, r_Cst[h]], writes=[r_Cst[h]])
                        dv(lambda e, prs=prs, ti=ti: e.tensor_copy(out=Cb[prs, ti, :], in_=Cst[prs, ti, 0:128]), reads=[r_Cst[h]], writes=[r_Cb[h]])
                        dv(lambda e, prs=prs, ti=ti: e.tensor_scalar(out=nrepB[prs, ti, :], in0=OnesF[prs, :], scalar1=Cst[prs, ti, 128:129], scalar2=None, op0=ALU.mult),
                           reads=[r_Cst[h], r_const], writes=[r_Cb[h]])
                for h in range(4):
                    sq, r_sq = sq_ring.next()
                    ac(lambda e, sq=sq, h=h: e.activation(out=sq[:], in_=hTm[:, h, :], func=AF.Square), reads=[r_hTm[h]], writes=[r_sq])
                    pe(lambda e, sq=sq: e.matmul(ps[7][:], lhsT=onesB[:], rhs=sq[:], start=True, stop=True), reads=[r_sq, r_const], writes=r_ps[7])
                    e1, r_e1 = e_ring.next()
                    bc_rstd(ps[7][:], r_ps[7], 128, e1[:], r_e1)
                    t1, r_t1 = t_ring.next()
                    dv(lambda e, t1=t1, e1=e1, h=h: e.scalar_tensor_tensor(out=t1[:], in0=hTm[:, h, :], scalar=gc[:, l, G_MH + h:G_MH + h + 1], in1=e1[:],
                                                                     op0=ALU.mult, op1=ALU.mult),
                       reads=[r_hTm[h], r_e1, r_const], writes=[r_t1])
                    dv(lambda e, t1=t1, h=h: e.tensor_tensor(out=yM[:, h, :], in0=t1[:], in1=so[:, h, :], op=ALU.mult),
                       reads=[r_t1, r_so[h]], writes=[r_yM[h]])

            for fc in range(NFC):
                slab, r_slab = small_ring.next()
                P.op("sp", lambda e, slab=slab, fc=fc: e.dma_start(out=slab[:, 0:1536], in_=wout_b[l, fc]),
                     reads=[r_w[("out", l, fc)]], writes=[r_slab], dma=True)
                Y, rY = ps[6 + fc % 2], r_ps[6 + fc % 2]
                for h in range(8):
                    pe(lambda e, slab=slab, h=h, Y=Y: e.matmul(Y[:], lhsT=slab[0:64, h * 128:(h + 1) * 128], rhs=yA[:, h, :], start=(h == 0), stop=False),
                       reads=[r_slab, r_yA[h]], writes=rY)
                for h in range(4):
                    pe(lambda e, slab=slab, h=h, Y=Y: e.matmul(Y[:], lhsT=slab[:, 1024 + h * 128:1024 + (h + 1) * 128], rhs=yM[:, h, :], start=False, stop=(h == 3)),
                       reads=[r_slab, r_yM[h]], writes=rY)
                dv(lambda e, Y=Y, fc=fc: e.tensor_tensor(out=x_sb[:, fc, :], in0=Y[:], in1=x_sb[:, fc, :], op=ALU.add),
                   reads=rY + [r_x[fc]], writes=[r_x[fc]])

        conv_all = [conv_ops(l) for l in range(NL)]
        emit_conv(conv_all[0])
        r_xs = [[[Res() for _ in range(NB)] for _ in range(NSEQ)] for _ in range(2)]
        finals = []
        for l in range(NL):
            nxt = conv_all[l + 1] if l + 1 < NL else []
            npass = NSEQ * NB
            per = (len(nxt) + npass - 1) // npass if nxt else 0
            ip = 0
            for s in range(NSEQ):
                for b in range(NB):
                    t0 = s * S + b * TB
                    src = xT if l == 0 else xs[(l - 1) % 2]
                    srcv = src.rearrange("(fc p) t -> p fc t", p=128)[:, :, t0:t0 + TB]
                    rd = [] if l == 0 else [r_xs[(l - 1) % 2][s][b]]
                    P.op("sp", lambda e, srcv=srcv: e.dma_start(out=x_sb[:], in_=srcv), reads=rd, writes=r_x, dma=True)
                    ffn(l, 0)
                    if do_mixer:
                        mixer(l, s, b)
                    ffn(l, 1)
                    if l == NL - 1 and final:
                        norm_stats()
                        for fc in range(NFC):
                            o_t, r_o = ostage.next()
                            P.op("dve", lambda e, fc=fc, o_t=o_t: e.scalar_tensor_tensor(
                                out=o_t[:], in0=x_sb[:, fc, :], scalar=gf[:, fc:fc + 1], in1=rstd[:], op0=ALU.mult, op1=ALU.mult),
                                reads=[r_x[fc], r_rstd, r_const], writes=[r_o])
                            finals.append(P.op("sp", lambda e, fc=fc, o_t=o_t, t0=t0: e.dma_start(
                                out=outT[fc * 128:(fc + 1) * 128, t0:t0 + TB], in_=o_t[:]), reads=[r_o], writes=[Res()], dma=True))
                    elif l == NL - 1:
                        dst = outT.rearrange("(fc p) t -> p fc t", p=128)[:, :, t0:t0 + TB]
                        finals.append(P.op("sp", lambda e, dst=dst: e.dma_start(out=dst, in_=x_sb[:]), reads=r_x, writes=[Res()], dma=True))
                    else:
                        dst = xs[l % 2].rearrange("(fc p) t -> p fc t", p=128)[:, :, t0:t0 + TB]
                        P.op("sp", lambda e, dst=dst: e.dma_start(out=dst, in_=x_sb[:]), reads=r_x, writes=[r_xs[l % 2][s][b]], dma=True)
                    if nxt:
                        emit_conv(nxt[ip * per:(ip + 1) * per])
                        ip += 1
        P.emit(final_waits=finals)
    return nc


def prep_weights(inp, L0, NL):
    f32 = np.float32
    w = {}
    inp = {k: (np.asarray(v)[L0:L0 + NL] if k not in ('x', 'positions', 'final_norm') else v) for k, v in inp.items()}
    wgu = np.zeros((NL, 2, NC_FF, 128, 2, NFC, 128), f32)
    wd = np.zeros((NL, 2, NFC, 128, NC_FF, 128), f32)
    ffn_w = ((inp["ffn1_w_gate"], inp["ffn1_w_up"], inp["ffn1_w_down"]),
             (inp["ffn2_w_gate"], inp["ffn2_w_up"], inp["ffn2_w_down"]))
    for f in range(2):
        for j in range(2):
            a = np.asarray(ffn_w[f][j], f32)[:NL]
            a = a.reshape(NL, NFC, 128, NC_FF, 128)
            wgu[:, f, :, :, j, :, :] = a.transpose(0, 3, 2, 1, 4)
        a = np.asarray(ffn_w[f][2], f32)[:NL]
        a = a.reshape(NL, NC_FF, 128, NFC, 128)
        wd[:, f] = a.transpose(0, 3, 2, 1, 4)
    w["wgu"] = wgu.reshape(NL, 2, NC_FF, 128, 2048)
    w["wd"] = wd.reshape(NL, 2, NFC, 128, DFF)
    Win = np.asarray(inp["w_in"], f32)[:NL].reshape(NL, NFC, 128, 1960)
    tiles = np.zeros((NL, NWT, 128, NFC, 128), f32)

    def put(t, c0, c1, src_cols):
        tiles[:, t, :, :, c0:c1] = Win[:, :, :, src_cols].transpose(0, 2, 1, 3)
    put(0, 0, 128, np.arange(0, 128)); put(1, 0, 128, np.arange(128, 256)); put(2, 0, 128, np.arange(256, 384))
    put(3, 64, 96, np.arange(384, 416))
    put(4, 64, 96, 384 + (np.arange(32) + 16) % 32)
    put(5, 0, 128, np.arange(416, 544)); put(6, 0, 128, np.arange(544, 672))
    put(7, 0, 128, np.arange(672, 800)); put(8, 0, 128, np.arange(800, 928))
    for h in range(4):
        put(9 + h, 0, 128, np.arange(1440 + 128 * h, 1568 + 128 * h))
        put(14 + h, 0, 128, np.arange(928 + 128 * h, 1056 + 128 * h))
    put(13, 0, 8, np.arange(1952, 1960))
    w["win"] = np.ascontiguousarray(tiles.reshape(NL, NWS, 2, 128, NFC * 128).transpose(0, 1, 3, 2, 4)).reshape(NL, NWS, 128, 2048)
    Wo = np.asarray(inp["w_out"], f32)[:NL]
    wout = np.zeros((NL, NFC, 128, 1536), f32)
    att = Wo[:, 0:512].reshape(NL, 8, 64, NFC, 128)
    wout[:, :, 0:64, 0:1024] = att.transpose(0, 3, 2, 1, 4).reshape(NL, NFC, 64, 1024)
    mem = Wo[:, 512:1024].reshape(NL, 4, 128, NFC, 128)
    wout[:, :, :, 1024:1536] = mem.transpose(0, 3, 2, 1, 4).reshape(NL, NFC, 128, 512)
    w["wout"] = wout
    Wq = np.asarray(inp["w_uq"], f32)[:NL].reshape(NL, 2, 128, 8, 96)
    uq = np.zeros((NL, 128, 2, 8, 192), f32)
    uq[:, :, :, :, 0:96] = Wq.transpose(0, 2, 1, 3, 4)
    uq[:, :, :, :, 160:192] = Wq.transpose(0, 2, 1, 3, 4)[..., 64 + (np.arange(32) + 16) % 32]
    w["uq"] = uq.reshape(NL, 128, 3072)
    w["ukv"] = np.ascontiguousarray(np.asarray(inp["w_ukv"], f32)[:NL])
    gc = np.zeros((NL, 128, NGC), f32)
    for base, nm in ((G_FFN1, "ffn1_norm"), (G_MIX, "mix_norm"), (G_FFN2, "ffn2_norm")):
        gc[:, :, base:base + 8] = np.asarray(inp[nm], f32)[:NL].reshape(NL, 8, 128).transpose(0, 2, 1)
    gc[:, :, G_QL:G_QL + 2] = np.asarray(inp["q_latent_norm"], f32)[:NL].reshape(NL, 2, 128).transpose(0, 2, 1)
    gc[:, :, G_KVL] = np.asarray(inp["kv_latent_norm"], f32)[:NL]
    cw = np.asarray(inp["conv_w"], f32)[:NL].reshape(NL, 4, 4, 128)
    gc[:, :, G_CW:G_CW + 16] = cw.transpose(0, 3, 2, 1).reshape(NL, 128, 16)
    gc[:, :, G_CB:G_CB + 4] = np.asarray(inp["conv_b"], f32)[:NL].reshape(NL, 4, 128).transpose(0, 2, 1)
    gc[:, 0:64, G_AH:G_AH + 8] = np.asarray(inp["attn_head_norm"], f32)[:NL].reshape(NL, 8, 64).transpose(0, 2, 1)
    gc[:, :, G_MH:G_MH + 4] = np.asarray(inp["mlstm_head_norm"], f32)[:NL].reshape(NL, 4, 128).transpose(0, 2, 1)
    w["gcols"] = gc
    w["gfin"] = np.ascontiguousarray(np.asarray(inp["final_norm"], f32).reshape(8, 128).T)
    w["gbias"] = np.concatenate([np.asarray(inp["b_igate"], f32)[:NL], np.asarray(inp["b_fgate"], f32)[:NL]], axis=1)
    inv = (10000.0 ** (-np.arange(0, 32, 2, dtype=np.float32) / 32)).astype(f32)
    invf = np.ones((96, 2), f32)
    invf[:, 0] = 0.0
    invf[64:80, 0] = inv
    invf[80:96, 0] = inv
    invf[64:80, 1] = -1.0
    w["invf"] = invf
    return w


_CACHE = {}


def launch(inp, xT_list, S, B, L0, NL, ncores, final, **kw):
    NSEQ = B // ncores
    key = (S, NSEQ, NL, final, tuple(sorted(kw.items())))
    if key not in _CACHE:
        _CACHE[key] = build(S, NSEQ, NL, final=final, **kw)
    nc = _CACHE[key]
    w = prep_weights(inp, L0, NL)
    posn = np.asarray(inp["positions"], np.int32)
    in_maps = []
    for c in range(ncores):
        m = dict(w)
        m["xT"] = xT_list[c]
        m["pos"] = np.ascontiguousarray(posn[c * NSEQ:(c + 1) * NSEQ].reshape(1, NSEQ * S))
        in_maps.append(m)
    res = run_bass_kernel_spmd(nc, in_maps, core_ids=list(range(ncores)))
    return [res.results[c]["outT"] for c in range(ncores)]


def run(inp, S, B, NL, ncores, per_launch=None, **kw):
    NSEQ = B // ncores
    x = np.asarray(inp["x"], np.float32)
    xT_list = [np.ascontiguousarray(x[c * NSEQ:(c + 1) * NSEQ].reshape(NSEQ * S, D).T) for c in range(ncores)]
    per = per_launch or NL
    for L0 in range(0, NL, per):
        n = min(per, NL - L0)
        xT_list = launch(inp, xT_list, S, B, L0, n, ncores, final=(L0 + n == NL), **kw)
    out = np.empty((B, S, D), np.float32)
    for c in range(ncores):
        out[c * NSEQ:(c + 1) * NSEQ] = xT_list[c].T.reshape(NSEQ, S, D)
    return out


def kernel(**inputs):
    return run(inputs, 4096, 16, 4, 8)
```

```python
import contextlib
import numpy as np
import concourse.bass as bass
import concourse.mybir as mybir
from concourse.bass_utils import run_bass_kernel_spmd

F32 = mybir.dt.float32
BF16 = mybir.dt.bfloat16
I32 = mybir.dt.int32
AF = mybir.ActivationFunctionType
ALU = mybir.AluOpType

D = 1024
DFF = 2816
NFC = 8
NC_FF = 22
TB = 512
EPS = 1e-6
ENGS = ("pe", "act", "dve", "pool", "sp")
NSLOT = 8

G_FFN1, G_MIX, G_FFN2, G_QL, G_KVL, G_CW, G_CB, G_AH, G_MH = 0, 8, 16, 24, 26, 27, 43, 47, 55
NGC = 59


class Res:
    __slots__ = ("w", "r", "excl")

    def __init__(self, excl=False):
        self.w = None
        self.r = []
        self.excl = excl


class Op:
    __slots__ = ("eng", "fn", "deps", "dma", "sig", "sem", "val", "slot")

    def __init__(self, eng, fn, dma):
        self.eng = eng
        self.fn = fn
        self.dma = dma
        self.deps = []
        self.sig = False
        self.sem = None
        self.val = 0
        self.slot = -1


class Prog:
    def __init__(self, nc):
        self.nc = nc
        self.ops = {e: [] for e in ENGS}
        self.dma_count = {e: 0 for e in ENGS}
        self.last_dma_in_slot = {}

    def op(self, eng, fn, reads=(), writes=(), dma=False):
        o = Op(eng, fn, dma)
        deps = []
        ex = [r for r in reads if r.excl]
        if ex:
            reads = [r for r in reads if not r.excl]
            writes = list(writes) + ex
        for r in reads:
            if r.w is not None:
                deps.append(r.w)
        for r in writes:
            if r.w is not None:
                deps.append(r.w)
            deps.extend(r.r)
        for d in deps:
            if d.eng == eng and not d.dma and not dma and eng == "pe":
                continue
            if d not in o.deps:
                o.deps.append(d)
                d.sig = True
        if dma:
            k = self.dma_count[eng]
            self.dma_count[eng] += 1
            o.slot = k % NSLOT
            prev = self.last_dma_in_slot.get((eng, o.slot))
            if prev is not None and prev not in o.deps:
                o.deps.append(prev)
            self.last_dma_in_slot[(eng, o.slot)] = o
            o.sig = True
        for r in reads:
            r.r.append(o)
        for r in writes:
            r.w = o
            r.r = []
        self.ops[eng].append(o)
        return o

    def emit(self, final_waits=()):
        nc = self.nc
        with contextlib.ExitStack() as st:
            sems = {e: st.enter_context(nc.semaphore("s_" + e)) for e in ENGS}
            dsems = {(e, s): st.enter_context(nc.semaphore(f"d_{e}_{s}"))
                     for e in ENGS if self.dma_count[e] > 0 for s in range(NSLOT)}
            cnt = {e: 0 for e in ENGS}
            dcnt = {}
            for e in ENGS:
                for o in self.ops[e]:
                    if o.dma:
                        key = (e, o.slot)
                        dcnt[key] = dcnt.get(key, 0) + 16
                        o.sem = dsems[key]
                        o.val = dcnt[key]
                    elif o.sig:
                        cnt[e] += 1
                        o.sem = sems[e]
                        o.val = cnt[e]
            block = st.enter_context(nc.Block())
            engobj = {"pe": nc.tensor, "act": nc.scalar, "dve": nc.vector,
                      "pool": nc.gpsimd, "sp": nc.sync}
            finals = list(final_waits)

            def run(e):
                eo = engobj[e]
                waited = {}
                for o in self.ops[e]:
                    need = {}
                    for d in o.deps:
                        key = id(d.sem)
                        if d.val > need.get(key, (0, None))[0]:
                            need[key] = (d.val, d.sem)
                    for key, (val, sem) in need.items():
                        if waited.get(key, 0) >= val:
                            continue
                        eo.wait_ge(sem, val)
                        waited[key] = val
                    ins = o.fn(eo)
                    if o.sig:
                        ins.then_inc(o.sem, 16 if o.dma else 1)
                if e == "sp":
                    for d in finals:
                        if waited.get(id(d.sem), 0) >= d.val:
                            continue
                        eo.wait_ge(d.sem, d.val)
                        waited[id(d.sem)] = d.val

            @block.tensor
            def _(eng):
                run("pe")

            @block.scalar
            def _(eng):
                run("act")

            @block.vector
            def _(eng):
                run("dve")

            @block.gpsimd
            def _(eng):
                run("pool")

            @block.sync
            def _(eng):
                run("sp")


class Ring:
    def __init__(self, tiles):
        self.tiles = tiles
        self.res = [Res() for _ in tiles]
        self.i = 0

    def next(self):
        k = self.i % len(self.tiles)
        self.i += 1
        return self.tiles[k], self.res[k]


NWT = 18
NWS = NWT // 2
PI = 3.14159265358979
TWO_PI = 6.28318530717959
ATT_SCALE = 96.0 ** -0.5
MSTAGE = 9


def build(S, NSEQ, NL, do_mixer=True, do_mlstm=True, do_attn=True, final=True):
    nc = bass.Bass("TRN2", target_bir_lowering=False)
    NT = S * NSEQ
    NB = S // TB
    NCH = S // 128

    def din(name, shape, dt=F32):
        return nc.dram_tensor(name, list(shape), dt, kind="ExternalInput").ap()

    def dscr(name, shape, dt):
        return nc.dram_tensor(name, list(shape), dt, kind="Internal").ap()

    xT = din("xT", [D, NT])
    pos = din("pos", [1, NT], I32)
    invf = din("invf", [96, 2])
    wgu = din("wgu", [NL, 2, NC_FF, 128, 2048])
    wd = din("wd", [NL, 2, NFC, 128, DFF])
    win = din("win", [NL, NWS, 128, 2048])
    wout = din("wout", [NL, NFC, 128, 1536])
    uq = din("uq", [NL, 128, 3072])
    ukv = din("ukv", [NL, 128, 1024])
    gcols = din("gcols", [NL, 128, NGC])
    gfin = din("gfin", [128, NFC])
    gbias = din("gbias", [NL, 8])
    outT = nc.dram_tensor("outT", [D, NT], F32, kind="ExternalOutput").ap()

    wgu_b = dscr("wgu_b", [NL, 2, NC_FF, 128, 2048], BF16)
    wd_b = dscr("wd_b", [NL, 2, NFC, 128, DFF], BF16)
    win_b = dscr("win_b", [NL, NWS, 128, 2048], BF16)
    wout_b = dscr("wout_b", [NL, NFC, 128, 1536], BF16)
    xs = [dscr("xs0", [D, NT], F32), dscr("xs1", [D, NT], F32)]
    rope_d = dscr("rope_d", [2, 96, NT], F32)
    kc_d = dscr("kc_d", [NSEQ, 8, 96, S], BF16)
    vc_d = dscr("vc_d", [NSEQ, 8, 128, NCH, 65], BF16)

    P = Prog(nc)
    st = contextlib.ExitStack()
    with st:
        def sb(name, shape, dt=F32):
            return st.enter_context(nc.sbuf_tensor(name, list(shape), dt))

        x_sb = sb("x_sb", [128, NFC, TB]); r_x = [Res() for _ in range(NFC)]
        hT = sb("hT", [128, NFC, TB], BF16); r_h = [Res() for _ in range(NFC)]
        aT = sb("aT", [128, NC_FF, TB], BF16); r_a = [Res() for _ in range(NC_FF)]
        rstd = sb("rstd", [128, TB]); r_rstd = Res()
        sq_ring = Ring([sb(f"sq{i}", [128, TB], BF16) for i in range(2)])
        e_ring = Ring([sb(f"e{i}", [128, TB]) for i in range(2)])
        t_ring = Ring([sb(f"t{i}", [128, TB]) for i in range(2)])
        big_ring = Ring([sb(f"wb{i}", [128, 2048], BF16) for i in range(4)])
        small_ring = Ring([sb(f"ws{i}", [128, DFF], BF16) for i in range(3)])
        onesB = sb("onesB", [128, 128], BF16); r_const = Res()
        gc = sb("gc", [128, NL, NGC]); gf = sb("gf", [128, NFC])
        ostage = Ring([sb(f"ost{i}", [128, TB]) for i in range(3)])

        ps = [st.enter_context(nc.psum_tensor(f"ps{i}", [128, TB], F32)) for i in range(8)]
        r_ps = [[Res(excl=True)] * 4 for _ in range(8)]

        P.op("pool", lambda e: e.memset(onesB[:], 1.0), writes=[r_const])
        P.op("sp", lambda e: e.dma_start(out=gc[:], in_=gcols.rearrange("l p c -> p l c")), writes=[r_const], dma=True)
        P.op("sp", lambda e: e.dma_start(out=gf[:], in_=gfin), writes=[r_const], dma=True)

        if do_mixer:
            cqn = sb("cqn", [128, 2, TB], BF16); r_cqn = Res()
            ckvn = sb("ckvn", [128, TB], BF16); r_ckvn = Res()
            cs = sb("cs", [96, 2, TB]); r_cs = Res()
            KT = sb("KT", [96, 8, TB], BF16); r_KT = [Res() for _ in range(8)]
            VT = sb("VT", [128, 8, 4, 65], BF16); r_VT = Res()
            QT = sb("QT", [96, 8, TB], BF16); r_QT = [Res() for _ in range(8)]
            KH = Ring([sb(f"KH{i}", [96, TB], BF16) for i in range(6)])
            VH = Ring([sb(f"VH{i}", [128, 4, 65], BF16) for i in range(6)])
            PT = Ring([sb(f"PT{i}", [128, TB], BF16) for i in range(5)])
            sq65 = sb("sq65", [65, TB], BF16); r_sq65 = Res()
            NormW = sb("NormW", [65, 64], BF16)
            yA = sb("yA", [64, 8, TB], BF16); r_yA = [Res() for _ in range(8)]
            yM = sb("yM", [128, 4, TB], BF16); r_yM = [Res() for _ in range(4)]
            uq_sb = sb("uq_sb", [128, 2, 8, 192], BF16); r_uq = Res()
            ukv_sb = sb("ukv_sb", [128, 8, 128], BF16); r_ukv = Res()
            gb = sb("gb", [128, NL, 8])
            invf_sb = sb("invf_sb", [96, 2])
            TriB = sb("TriB", [128, 128], BF16)
            U = sb("U", [128, 4, TB + 3]); r_U = [Res() for _ in range(4)]
            qcT = sb("qcT", [128, 2, TB], BF16); r_qcT = [Res() for _ in range(2)]
            kTm = sb("kTm", [128, 2, TB], BF16); r_kTm = [Res() for _ in range(2)]
            so = sb("so", [128, 4, TB], BF16); r_so = [Res() for _ in range(4)]
            hTm = sb("hTm", [128, 4, TB]); r_hTm = [Res() for _ in range(4)]
            Vm = sb("Vm", [128, 4, 4, 129], BF16); r_Vm = [Res() for _ in range(4)]
            gsb = sb("gsb", [128, 4, 8]); r_gsb = Res()
            TriF = sb("TriF", [128, 128]); OnesF = sb("OnesF", [128, 128]); IdentF = sb("IdentF", [128, 128])
            IdentB = sb("IdentB", [128, 128], BF16); MaskNeg = sb("MaskNeg", [128, 128])
            nlf = Ring([sb(f"nlf{i}", [128, 4]) for i in range(2)])
            bias_s = Ring([sb(f"bias_s{i}", [128, 4]) for i in range(2)])
            wl = Ring([sb(f"wl{i}", [128, 4]) for i in range(2)])
            ebend = Ring([sb(f"ebend{i}", [128, 4]) for i in range(2)])
            ktok = Ring([sb(f"ktok{i}", [128, 256], BF16) for i in range(2)])
            nrep = Ring([sb(f"nrep{i}", [128, 128]) for i in range(4)])
            DT = Ring([sb(f"DT{i}", [128, 128]) for i in range(4)])
            EE = Ring([sb(f"EE{i}", [128, 128]) for i in range(4)])
            pTm = Ring([sb(f"pTm{i}", [128, 128], BF16) for i in range(4)])
            qb = Ring([sb(f"qb{i}", [128, 128], BF16) for i in range(4)])
            dn = Ring([sb(f"dn{i}", [128, 128]) for i in range(4)])
            kw = Ring([sb(f"kw{i}", [128, 64], BF16) for i in range(4)])
            Cst = sb("Cst", [128, 2, 129]); r_Cst = [Res() for _ in range(4)]
            Cb = sb("Cb", [128, 2, 128], BF16); nrepB = sb("nrepB", [128, 2, 128], BF16)
            r_Cb = [Res() for _ in range(4)]

            pl = lambda fn, **k: P.op("pool", fn, **k)
            pl(lambda e: e.memset(NormW[0:64, :], 1.0 / 64), writes=[r_const])
            pl(lambda e: e.memset(NormW[64:65, :], EPS), writes=[r_const])
            pl(lambda e: e.memset(OnesF[:], 1.0), writes=[r_const])
            pl(lambda e: e.memset(MaskNeg[:], 0.0), writes=[r_const])
            pl(lambda e: e.memset(VT[:], 1.0), writes=[r_VT])
            pl(lambda e: e.memset(Vm[:], 1.0), writes=r_Vm)
            pl(lambda e: e.memset(yA[:], 0.0), writes=r_yA)
            pl(lambda e: e.memset(yM[:], 0.0), writes=r_yM)
            pl(lambda e: e.affine_select(out=TriF[:], in_=OnesF[:], pattern=[[1, 128]], compare_op=ALU.is_ge,
                                         fill=0.0, base=0, channel_multiplier=-1), reads=[r_const], writes=[r_const])
            pl(lambda e: e.affine_select(out=MaskNeg[:], in_=MaskNeg[:], pattern=[[1, 128]], compare_op=ALU.is_ge,
                                         fill=-30000.0, base=0, channel_multiplier=-1), reads=[r_const], writes=[r_const])
            pl(lambda e: e.affine_select(out=IdentF[:], in_=OnesF[:], pattern=[[1, 128]], compare_op=ALU.is_equal,
                                         fill=0.0, base=0, channel_multiplier=-1), reads=[r_const], writes=[r_const])
            pl(lambda e: e.tensor_copy(out=TriB[:], in_=TriF[:]), reads=[r_const], writes=[r_const])
            pl(lambda e: e.tensor_copy(out=IdentB[:], in_=IdentF[:]), reads=[r_const], writes=[r_const])
            P.op("sp", lambda e: e.dma_start(out=invf_sb[:], in_=invf), writes=[r_const], dma=True)
            for l in range(NL):
                P.op("sp", lambda e, l=l: e.dma_start(out=gb[:, l, :], in_=gbias[l].partition_broadcast(128)),
                     writes=[r_const], dma=True)

            r_rope = Res()
            a, bq, cq_ = e_ring.tiles[0][0:96, :], e_ring.tiles[1][0:96, :], t_ring.tiles[0][0:96, :]
            rpi = t_ring.tiles[1][0:96, :].bitcast(I32)
            r_a_, r_b_, r_c_, r_i_ = e_ring.res[0], e_ring.res[1], t_ring.res[0], t_ring.res[1]
            for ci in range(NT // TB):
                c0 = ci * TB
                P.op("sp", lambda e, c0=c0: e.dma_start(out=rpi, in_=pos[0, c0:c0 + TB].partition_broadcast(96)),
                     writes=[r_i_], dma=True)
                dv = lambda fn, **k: P.op("dve", fn, **k)
                dv(lambda e: e.tensor_copy(out=a, in_=rpi), reads=[r_i_], writes=[r_a_])
                dv(lambda e: e.tensor_scalar(out=a, in0=a, scalar1=invf_sb[:, 0:1], scalar2=None, op0=ALU.mult),
                   reads=[r_a_, r_const], writes=[r_a_])
                for which in range(2):
                    sh = PI / 2 if which == 0 else 0.0
                    dv(lambda e, sh=sh: e.tensor_scalar(out=bq, in0=a, scalar1=sh, scalar2=1.0 / TWO_PI, op0=ALU.add, op1=ALU.mult),
                       reads=[r_a_], writes=[r_b_])
                    dv(lambda e: e.tensor_copy(out=rpi, in_=bq), reads=[r_b_], writes=[r_i_])
                    dv(lambda e: e.tensor_copy(out=bq, in_=rpi), reads=[r_i_], writes=[r_b_])
                    dv(lambda e: e.scalar_tensor_tensor(out=bq, in0=bq, scalar=-TWO_PI, in1=a, op0=ALU.mult, op1=ALU.add),
                       reads=[r_b_, r_a_], writes=[r_b_])
                    if which == 0:
                        dv(lambda e, sh=sh: e.tensor_scalar(out=bq, in0=bq, scalar1=sh, scalar2=None, op0=ALU.add),
                           reads=[r_b_], writes=[r_b_])
                    dv(lambda e: e.tensor_scalar(out=cq_, in0=bq, scalar1=PI, scalar2=-TWO_PI, op0=ALU.is_gt, op1=ALU.mult),
                       reads=[r_b_], writes=[r_c_])
                    dv(lambda e: e.tensor_tensor(out=bq, in0=bq, in1=cq_, op=ALU.add), reads=[r_b_, r_c_], writes=[r_b_])
                    dv(lambda e: e.tensor_scalar(out=cq_, in0=bq, scalar1=-PI, scalar2=TWO_PI, op0=ALU.is_lt, op1=ALU.mult),
                       reads=[r_b_], writes=[r_c_])
                    dv(lambda e: e.tensor_tensor(out=bq, in0=bq, in1=cq_, op=ALU.add), reads=[r_b_, r_c_], writes=[r_b_])
                    dv(lambda e: e.tensor_scalar(out=bq, in0=bq, scalar1=-3.14159, scalar2=3.14159, op0=ALU.max, op1=ALU.min),
                       reads=[r_b_], writes=[r_b_])
                    if which == 0:
                        P.op("act", lambda e: e.activation(out=cq_, in_=bq, func=AF.Sin), reads=[r_b_], writes=[r_c_])
                    else:
                        P.op("act", lambda e: e.activation(out=cq_, in_=bq, func=AF.Sin, scale=invf_sb[:, 1:2]),
                             reads=[r_b_, r_const], writes=[r_c_])
                    P.op("sp", lambda e, which=which, c0=c0: e.dma_start(out=rope_d[which, :, c0:c0 + TB], in_=cq_),
                         reads=[r_c_], writes=[r_rope], dma=True)

        r_w = {}

        def conv_ops(l):
            ops = []

            def add(key, dst, src, n):
                r_w[key] = Res()
                for h0 in range(0, n, 2048):
                    h1 = min(n, h0 + 2048)
                    ops.append((key, dst[:, h0:h1], src[:, h0:h1]))
            for f in range(2):
                for c in range(NC_FF):
                    add(("gu", l, f, c), wgu_b[l, f, c], wgu[l, f, c], 2048)
                for fc in range(NFC):
                    add(("d", l, f, fc), wd_b[l, f, fc], wd[l, f, fc], DFF)
                if f == 0 and do_mixer:
                    for j in range(NWS):
                        add(("in", l, j), win_b[l, j], win[l, j], 2048)
                    for fc in range(NFC):
                        add(("out", l, fc), wout_b[l, fc], wout[l, fc], 1536)
            return ops

        def emit_conv(items):
            for key, dst, src in items:
                P.op("pool", lambda e, dst=dst, src=src: e.dma_start(out=dst, in_=src),
                     writes=[r_w[key]], dma=True)

        def bc_rstd(psrc, r_src, n, dst, r_dst):
            P.op("act", lambda e: e.activation(out=dst, in_=psrc, func=AF.Ln, bias=EPS, scale=1.0 / n),
                 reads=r_src, writes=[r_dst])
            P.op("act", lambda e: e.activation(out=dst, in_=dst, func=AF.Exp, scale=-0.5),
                 reads=[r_dst], writes=[r_dst])

        def sigmoid_chain(dst, r_dst, src, r_src, final_out=None, r_final=None):
            P.op("act", lambda e: e.activation(out=dst, in_=src, func=AF.Exp, scale=-1.0), reads=r_src, writes=[r_dst])
            P.op("act", lambda e: e.activation(out=dst, in_=dst, func=AF.Ln, bias=1.0, scale=1.0), reads=[r_dst], writes=[r_dst])
            if final_out is None:
                P.op("act", lambda e: e.activation(out=dst, in_=dst, func=AF.Exp, scale=-1.0), reads=[r_dst], writes=[r_dst])
            else:
                P.op("act", lambda e: e.activation(out=final_out, in_=dst, func=AF.Exp, scale=-1.0), reads=[r_dst], writes=r_final)

        def norm_stats():
            for fc in range(NFC):
                sq, r_sq = sq_ring.next()
                P.op("act", lambda e, sq=sq, fc=fc: e.activation(out=sq[:], in_=x_sb[:, fc, :], func=AF.Square),
                     reads=[r_x[fc]], writes=[r_sq])
                P.op("pe", lambda e, sq=sq, fc=fc: e.matmul(ps[6][:], lhsT=onesB[:], rhs=sq[:], start=(fc == 0), stop=(fc == NFC - 1)),
                     reads=[r_sq, r_const], writes=r_ps[6])
            bc_rstd(ps[6][:], r_ps[6], D, rstd[:], r_rstd)

        def norm_to_hT(l, gbase):
            norm_stats()
            for fc in range(NFC):
                P.op("dve", lambda e, fc=fc: e.scalar_tensor_tensor(
                    out=hT[:, fc, :], in0=x_sb[:, fc, :], scalar=gc[:, l, gbase + fc:gbase + fc + 1],
                    in1=rstd[:], op0=ALU.mult, op1=ALU.mult),
                    reads=[r_x[fc], r_rstd, r_const], writes=[r_h[fc]])

        gu_alt = [0]

        def ffn(l, f, stream_out=None):
            norm_to_hT(l, G_FFN1 if f == 0 else G_FFN2)
            for c in range(NC_FF):
                slab, r_slab = big_ring.next()
                P.op("sp", lambda e, slab=slab, c=c: e.dma_start(out=slab[:], in_=wgu_b[l, f, c]),
                     reads=[r_w[("gu", l, f, c)]], writes=[r_slab], dma=True)
                k = gu_alt[0] % 2
                gu_alt[0] += 1
                G, U_ = ps[2 * k], ps[2 * k + 1]
                rG, rU = r_ps[2 * k], r_ps[2 * k + 1]
                for kc in range(NFC):
                    P.op("pe", lambda e, slab=slab, kc=kc, G=G: e.matmul(
                        G[:], lhsT=slab[:, kc * 128:(kc + 1) * 128], rhs=hT[:, kc, :], start=(kc == 0), stop=(kc == NFC - 1)),
                        reads=[r_slab, r_h[kc]], writes=rG)
                for kc in range(NFC):
                    P.op("pe", lambda e, slab=slab, kc=kc, U_=U_: e.matmul(
                        U_[:], lhsT=slab[:, (8 + kc) * 128:(9 + kc) * 128], rhs=hT[:, kc, :], start=(kc == 0), stop=(kc == NFC - 1)),
                        reads=[r_slab, r_h[kc]], writes=rU)
                e1, r_e1 = e_ring.next()
                t1, r_t1 = t_ring.next()
                sigmoid_chain(e1[:], r_e1, G[:], rG)
                P.op("dve", lambda e, e1=e1, t1=t1, G=G: e.tensor_tensor(out=t1[:], in0=G[:], in1=e1[:], op=ALU.mult),
                     reads=rG + [r_e1], writes=[r_t1])
                P.op("dve", lambda e, t1=t1, U_=U_, c=c: e.tensor_tensor(out=aT[:, c, :], in0=U_[:], in1=t1[:], op=ALU.mult),
                     reads=rU + [r_t1], writes=[r_a[c]])
            for fc in range(NFC):
                slab, r_slab = small_ring.next()
                P.op("sp", lambda e, slab=slab, fc=fc: e.dma_start(out=slab[:], in_=wd_b[l, f, fc]),
                     reads=[r_w[("d", l, f, fc)]], writes=[r_slab], dma=True)
                Y, rY = ps[4 + fc % 2], r_ps[4 + fc % 2]
                for c in range(NC_FF):
                    P.op("pe", lambda e, slab=slab, c=c, Y=Y: e.matmul(
                        Y[:], lhsT=slab[:, c * 128:(c + 1) * 128], rhs=aT[:, c, :], start=(c == 0), stop=(c == NC_FF - 1)),
                        reads=[r_slab, r_a[c]], writes=rY)
                if stream_out is None:
                    P.op("dve", lambda e, Y=Y, fc=fc: e.scalar_tensor_tensor(
                        out=x_sb[:, fc, :], in0=Y[:], scalar=0.5, in1=x_sb[:, fc, :], op0=ALU.mult, op1=ALU.add),
                        reads=rY + [r_x[fc]], writes=[r_x[fc]])
                else:
                    o_t, r_o = ostage.next()
                    P.op("dve", lambda e, Y=Y, fc=fc, o_t=o_t: e.scalar_tensor_tensor(
                        out=o_t[:], in0=Y[:], scalar=0.5, in1=x_sb[:, fc, :], op0=ALU.mult, op1=ALU.add),
                        reads=rY + [r_x[fc]], writes=[r_o])
                    stream_out(fc, o_t, r_o)

        r_kc = [[Res() for _ in range(NB)] for _ in range(NSEQ)]
        r_vc = [[Res() for _ in range(NB)] for _ in range(NSEQ)]

        def mixer(l, s, b):
            t0 = s * S + b * TB
            dv = lambda fn, **k: P.op("dve", fn, **k)
            ac = lambda fn, **k: P.op("act", fn, **k)
            pe = lambda fn, **k: P.op("pe", fn, **k)
            pl = lambda fn, **k: P.op("pool", fn, **k)
            if s == 0 and b == 0:
                pl(lambda e: e.dma_start(out=uq_sb[:], in_=uq[l]), writes=[r_uq], dma=True)
                pl(lambda e: e.dma_start(out=ukv_sb[:], in_=ukv[l]), writes=[r_ukv], dma=True)
            norm_to_hT(l, G_MIX)
            P.op("sp", lambda e: e.dma_start(out=cs[:], in_=rope_d[:, :, t0:t0 + TB].rearrange("w p t -> p w t")),
                 reads=[r_rope], writes=[r_cs], dma=True)
            if b == 0:
                dv(lambda e: e.memset(U[:, :, 0:3], 0.0), writes=r_U)
                dv(lambda e: e.memset(Cst[:], 0.0), writes=r_Cst)
                dv(lambda e: e.memset(Cb[:], 0.0), writes=r_Cb)
                dv(lambda e: e.memset(nrepB[:], 0.0), writes=r_Cb)

            def proj(slab, off, out_ap, r_out, M=128):
                for kc in range(NFC):
                    pe(lambda e, kc=kc: e.matmul(out_ap, lhsT=slab[:, off + kc * 128: off + kc * 128 + M], rhs=hT[:, kc, :],
                                                 start=(kc == 0), stop=(kc == NFC - 1)),
                       reads=[cur_r_slab[0], r_h[kc]], writes=r_out)

            cur_r_slab = [None]
            for j in range(NWS):
                slab, r_slab = big_ring.next()
                cur_r_slab[0] = r_slab
                P.op("sp", lambda e, slab=slab, j=j: e.dma_start(out=slab[:], in_=win_b[l, j]),
                     reads=[r_w[("in", l, j)]], writes=[r_slab], dma=True)
                for tt in (2 * j, 2 * j + 1):
                    off = (tt % 2) * 1024
                    if tt in (0, 1):
                        proj(slab, off, ps[tt][:], r_ps[tt])
                        if tt == 1:
                            for q in range(2):
                                sq, r_sq = sq_ring.next()
                                ac(lambda e, sq=sq, q=q: e.activation(out=sq[:], in_=ps[q][:], func=AF.Square), reads=r_ps[q], writes=[r_sq])
                                pe(lambda e, sq=sq, q=q: e.matmul(ps[6][:], lhsT=onesB[:], rhs=sq[:], start=(q == 0), stop=(q == 1)),
                                   reads=[r_sq, r_const], writes=r_ps[6])
                            bc_rstd(ps[6][:], r_ps[6], 256, rstd[:], r_rstd)
                            for q in range(2):
                                dv(lambda e, q=q: e.scalar_tensor_tensor(out=cqn[:, q, :], in0=ps[q][:], scalar=gc[:, l, G_QL + q:G_QL + q + 1],
                                                                      in1=rstd[:], op0=ALU.mult, op1=ALU.mult),
                                   reads=r_ps[q] + [r_rstd, r_const], writes=[r_cqn])
                    elif tt == 2:
                        proj(slab, off, ps[2][:], r_ps[2])
                        sq, r_sq = sq_ring.next()
                        ac(lambda e, sq=sq: e.activation(out=sq[:], in_=ps[2][:], func=AF.Square), reads=r_ps[2], writes=[r_sq])
                        pe(lambda e, sq=sq: e.matmul(ps[7][:], lhsT=onesB[:], rhs=sq[:], start=True, stop=True),
                           reads=[r_sq, r_const], writes=r_ps[7])
                        e1, r_e1 = e_ring.next()
                        bc_rstd(ps[7][:], r_ps[7], 128, e1[:], r_e1)
                        dv(lambda e, e1=e1: e.scalar_tensor_tensor(out=ckvn[:], in0=ps[2][:], scalar=gc[:, l, G_KVL:G_KVL + 1],
                                                                in1=e1[:], op0=ALU.mult, op1=ALU.mult),
                           reads=r_ps[2] + [r_e1, r_const], writes=[r_ckvn])
                    elif tt in (3, 4):
                        proj(slab, off, ps[tt][0:96, :], r_ps[tt], M=96)
                        if tt == 4:
                            ta, r_ta = t_ring.next()
                            tb_, r_tb = t_ring.next()
                            dv(lambda e, ta=ta: e.tensor_tensor(out=ta[64:96, :], in0=ps[3][64:96, :], in1=cs[64:96, 0, :], op=ALU.mult),
                               reads=r_ps[3] + [r_cs], writes=[r_ta])
                            dv(lambda e, tb_=tb_: e.tensor_tensor(out=tb_[64:96, :], in0=ps[4][64:96, :], in1=cs[64:96, 1, :], op=ALU.mult),
                               reads=r_ps[4] + [r_cs], writes=[r_tb])
                            dv(lambda e, ta=ta, tb_=tb_: e.tensor_tensor(out=KT[64:96, 0, :], in0=ta[64:96, :], in1=tb_[64:96, :], op=ALU.add),
                               reads=[r_ta, r_tb], writes=[r_KT[0]])
                            for h in range(1, 8):
                                pl(lambda e, h=h: e.tensor_copy(out=KT[64:96, h, :], in_=KT[64:96, 0, :]), reads=[r_KT[0]], writes=[r_KT[h]])
                    elif 5 <= tt <= 8:
                        i = tt - 5
                        pk = ps[i % 4]
                        proj(slab, off, pk[:], r_ps[i % 4])
                        ac(lambda e, i=i, pk=pk: e.activation(out=U[:, i, 3:TB + 3], in_=pk[:], func=AF.Copy), reads=r_ps[i % 4], writes=[r_U[i]])
                    elif 9 <= tt <= 12:
                        h = tt - 9
                        pk, rk = ps[4 + h % 2], r_ps[4 + h % 2]
                        proj(slab, off, pk[:], rk)
                        e1, r_e1 = e_ring.next()
                        sigmoid_chain(e1[:], r_e1, pk[:], rk, final_out=so[:, h, :], r_final=[r_so[h]])
                    elif tt == 13:
                        for jj in range(4):
                            for kc in range(NFC):
                                pe(lambda e, kc=kc, jj=jj, slab=slab, off=off: e.matmul(
                                    ps[6][:, jj * 8:(jj + 1) * 8], lhsT=hT[:, kc, jj * 128:(jj + 1) * 128],
                                    rhs=slab[:, off + kc * 128: off + kc * 128 + 8], start=(kc == 0), stop=(kc == NFC - 1)),
                                   reads=[r_slab, r_h[kc]], writes=[r_ps[6][0]])
                        for jj in range(4):
                            dv(lambda e, jj=jj: e.tensor_tensor(out=gsb[:, jj, :], in0=ps[6][:, jj * 8:(jj + 1) * 8], in1=gb[:, l, :], op=ALU.add),
                               reads=[r_ps[6][0], r_const], writes=[r_gsb])
                    else:
                        hh = tt - 14
                        pk, rk = ps[hh % 2], r_ps[hh % 2]
                        for jj in range(4):
                            for kc in range(NFC):
                                pe(lambda e, kc=kc, jj=jj, slab=slab, off=off, pk=pk: e.matmul(
                                    pk[:, jj * 128:(jj + 1) * 128], lhsT=hT[:, kc, jj * 128:(jj + 1) * 128],
                                    rhs=slab[:, off + kc * 128: off + (kc + 1) * 128], start=(kc == 0), stop=(kc == NFC - 1)),
                                   reads=[r_slab, r_h[kc]], writes=rk)
                        ac(lambda e, hh=hh, pk=pk: e.activation(out=Vm[:, :, hh, 0:128], in_=pk[:].rearrange("p (j d) -> p j d", d=128), func=AF.Copy),
                           reads=rk, writes=[r_Vm[hh]])

            for h in range(8):
                pk, rk = ps[2 + h % 2], r_ps[2 + h % 2]
                pe(lambda e, h=h, pk=pk: e.matmul(pk[0:64, :], lhsT=ukv_sb[:, h, 0:64], rhs=ckvn[:], start=True, stop=True),
                   reads=[r_ukv, r_ckvn], writes=rk)
                ac(lambda e, h=h, pk=pk: e.activation(out=KT[0:64, h, :], in_=pk[0:64, :], func=AF.Copy), reads=rk, writes=[r_KT[h]])
            for jj in range(4):
                pk, rk = ps[4 + jj % 2], r_ps[4 + jj % 2]
                pe(lambda e, jj=jj, pk=pk: e.matmul(pk[:].rearrange("p (h d) -> p h d", d=64), lhsT=ckvn[:, jj * 128:(jj + 1) * 128],
                                                   rhs=ukv_sb[:, :, 64:128], start=True, stop=True),
                   reads=[r_ukv, r_ckvn], writes=rk)
                dv(lambda e, jj=jj, pk=pk: e.tensor_copy(out=VT[:, :, jj, 0:64], in_=pk[:].rearrange("p (h d) -> p h d", d=64)),
                   reads=rk, writes=[r_VT])
            P.op("sp", lambda e: e.dma_start(out=kc_d[s, :, :, t0 - s * S:t0 - s * S + TB].rearrange("h p t -> p h t"), in_=KT[:]),
                 reads=r_KT, writes=[r_kc[s][b]], dma=True)
            P.op("sp", lambda e: e.dma_start(out=vc_d[s, :, :, 4 * b:4 * b + 4, :].rearrange("h p c e -> p h c e"), in_=VT[:]),
                 reads=[r_VT], writes=[r_vc[s][b]], dma=True)

            for h in range(8):
                pq, rq = ps[2 * (h % 2)], r_ps[2 * (h % 2)]
                pr_, rr = ps[2 * (h % 2) + 1], r_ps[2 * (h % 2) + 1]
                for kc in range(2):
                    pe(lambda e, h=h, kc=kc, pq=pq: e.matmul(pq[0:96, :], lhsT=uq_sb[:, kc, h, 0:96], rhs=cqn[:, kc, :], start=(kc == 0), stop=(kc == 1)),
                       reads=[r_uq, r_cqn], writes=rq)
                for kc in range(2):
                    pe(lambda e, h=h, kc=kc, pr_=pr_: e.matmul(pr_[0:96, :], lhsT=uq_sb[:, kc, h, 96:192], rhs=cqn[:, kc, :], start=(kc == 0), stop=(kc == 1)),
                       reads=[r_uq, r_cqn], writes=rr)
                ac(lambda e, h=h, pq=pq: e.activation(out=QT[0:64, h, :], in_=pq[0:64, :], func=AF.Copy), reads=rq, writes=[r_QT[h]])
                ta, r_ta = t_ring.next()
                tb_, r_tb = t_ring.next()
                dv(lambda e, ta=ta, pq=pq: e.tensor_tensor(out=ta[64:96, :], in0=pq[64:96, :], in1=cs[64:96, 0, :], op=ALU.mult),
                   reads=rq + [r_cs], writes=[r_ta])
                dv(lambda e, tb_=tb_, pr_=pr_: e.tensor_tensor(out=tb_[64:96, :], in0=pr_[64:96, :], in1=cs[64:96, 1, :], op=ALU.mult),
                   reads=rr + [r_cs], writes=[r_tb])
                dv(lambda e, ta=ta, tb_=tb_, h=h: e.tensor_tensor(out=QT[64:96, h, :], in0=ta[64:96, :], in1=tb_[64:96, :], op=ALU.add),
                   reads=[r_ta, r_tb], writes=[r_QT[h]])

            if do_attn:
                def head_norm(h, pO, rO):
                        ac(lambda e, pO=pO: e.activation(out=sq65[:], in_=pO[0:65, :], func=AF.Square), reads=rO, writes=[r_sq65])
                        pe(lambda e: e.matmul(ps[5][0:64, :], lhsT=NormW[:], rhs=sq65[:], start=True, stop=True),
                           reads=[r_sq65, r_const], writes=r_ps[5])
                        e1, r_e1 = e_ring.next()
                        P.op("act", lambda e, e1=e1: e.activation(out=e1[0:64, :], in_=ps[5][0:64, :], func=AF.Ln), reads=r_ps[5], writes=[r_e1])
                        P.op("act", lambda e, e1=e1: e.activation(out=e1[0:64, :], in_=e1[0:64, :], func=AF.Exp, scale=-0.5), reads=[r_e1], writes=[r_e1])
                        dv(lambda e, e1=e1, h=h, pO=pO: e.scalar_tensor_tensor(out=yA[:, h, :], in0=pO[0:64, :], scalar=gc[0:64, l, G_AH + h:G_AH + h + 1],
                                                                        in1=e1[0:64, :], op0=ALU.mult, op1=ALU.mult),
                           reads=rO + [r_e1, r_const], writes=[r_yA[h]])

                units = []
                for h in range(8):
                    for kb in range(b + 1):
                        for kk in range(4):
                            units.append((h, kb, kk))
                nk = 4 * (b + 1)
                pend = []
                cur = {}

                def emit_pv(u):
                    h, kc, col0, vh, r_vh, kk, pt, r_pt = u
                    pO, rO = ps[3 + h % 2], r_ps[3 + h % 2]
                    pe(lambda e: e.matmul(pO[0:65, col0:TB], lhsT=vh[:, kk, :], rhs=pt[:, col0:TB], start=(kc == 0), stop=(kc == nk - 1)),
                       reads=[r_vh, r_pt], writes=rO)
                    if kc == nk - 1:
                        head_norm(h, pO, rO)

                s_alt = 0
                for (h, kb, kk) in units:
                    if kk == 0:
                        kh, r_kh = KH.next()
                        vh, r_vh = VH.next()
                        P.op("sp", lambda e, kh=kh, kb=kb, h=h: e.dma_start(out=kh[:], in_=kc_d[s, h, :, kb * TB:(kb + 1) * TB]),
                             reads=[r_kc[s][kb]], writes=[r_kh], dma=True)
                        P.op("sp", lambda e, vh=vh, kb=kb, h=h: e.dma_start(out=vh[:], in_=vc_d[s, h, :, 4 * kb:4 * kb + 4, :]),
                             reads=[r_vc[s][kb]], writes=[r_vh], dma=True)
                        cur = dict(kh=kh, r_kh=r_kh, vh=vh, r_vh=r_vh)
                    kh, r_kh, vh, r_vh = cur["kh"], cur["r_kh"], cur["vh"], cur["r_vh"]
                    kc = kb * 4 + kk
                    d = kc - 4 * b
                    col0 = 128 * d if d > 0 else 0
                    pS, rS = ps[s_alt % 3], r_ps[s_alt % 3]
                    s_alt += 1
                    pt, r_pt = PT.next()
                    pe(lambda e, kh=kh, kk=kk, h=h, col0=col0, pS=pS: e.matmul(
                        pS[:, col0:TB], lhsT=kh[:, kk * 128:(kk + 1) * 128], rhs=QT[:, h, col0:TB], start=True, stop=True),
                       reads=[r_kh, r_QT[h]], writes=rS)
                    ac(lambda e, pt=pt, pS=pS, col0=col0: e.activation(out=pt[:, col0:TB], in_=pS[:, col0:TB], func=AF.Exp, scale=ATT_SCALE),
                       reads=rS, writes=[r_pt])
                    if d >= 0:
                        dv(lambda e, pt=pt, col0=col0: e.tensor_tensor(out=pt[:, col0:col0 + 128], in0=pt[:, col0:col0 + 128], in1=TriB[:], op=ALU.mult),
                           reads=[r_pt, r_const], writes=[r_pt])
                    pend.append((h, kc, col0, vh, r_vh, kk, pt, r_pt))
                    if len(pend) > 2:
                        emit_pv(pend.pop(0))
                while pend:
                    emit_pv(pend.pop(0))

            if do_mlstm:
                for i in range(4):
                    acc, r_acc = t_ring.next()
                    cw = lambda jj, i=i: gc[:, l, G_CW + i * 4 + jj:G_CW + i * 4 + jj + 1]
                    dv(lambda e, acc=acc, i=i, cw=cw: e.tensor_scalar(out=acc[:], in0=U[:, i, 0:TB], scalar1=cw(0),
                                                                      scalar2=gc[:, l, G_CB + i:G_CB + i + 1], op0=ALU.mult, op1=ALU.add),
                       reads=[r_U[i], r_const], writes=[r_acc])
                    for jj in range(1, 4):
                        dv(lambda e, acc=acc, i=i, jj=jj, cw=cw: e.scalar_tensor_tensor(out=acc[:], in0=U[:, i, jj:jj + TB], scalar=cw(jj),
                                                                                  in1=acc[:], op0=ALU.mult, op1=ALU.add),
                           reads=[r_U[i], r_acc, r_const], writes=[r_acc])
                    e1, r_e1 = e_ring.next()
                    sigmoid_chain(e1[:], r_e1, acc[:], [r_acc])
                    if i < 2:
                        dv(lambda e, acc=acc, e1=e1, i=i: e.scalar_tensor_tensor(out=qcT[:, i, :], in0=acc[:], scalar=0.125, in1=e1[:], op0=ALU.mult, op1=ALU.mult),
                           reads=[r_acc, r_e1], writes=[r_qcT[i]])
                    else:
                        dv(lambda e, acc=acc, e1=e1, i=i: e.tensor_tensor(out=kTm[:, i - 2, :], in0=acc[:], in1=e1[:], op=ALU.mult),
                           reads=[r_acc, r_e1], writes=[r_kTm[i - 2]])
                    dv(lambda e, i=i: e.tensor_copy(out=U[:, i, 0:3], in_=U[:, i, TB:TB + 3]), reads=[r_U[i]], writes=[r_U[i]])

                for jj in range(4 if MSTAGE >= 2 else 0):
                    cols = slice(jj * 128, (jj + 1) * 128)
                    nl_, r_nl = nlf.next()
                    bs_, r_bs = bias_s.next()
                    wl_, r_wl = wl.next()
                    eb_, r_eb = ebend.next()
                    kt_, r_kt = ktok.next()
                    ac(lambda e, nl_=nl_, jj=jj: e.activation(out=nl_[:], in_=gsb[:, jj, 4:8], func=AF.Exp, scale=-1.0), reads=[r_gsb], writes=[r_nl])
                    ac(lambda e, nl_=nl_: e.activation(out=nl_[:], in_=nl_[:], func=AF.Ln, bias=1.0, scale=1.0), reads=[r_nl], writes=[r_nl])
                    rg0 = [r_ps[6][0]]
                    pe(lambda e, nl_=nl_: e.matmul(ps[6][:, 0:4], lhsT=TriF[:], rhs=nl_[:], start=True, stop=True), reads=[r_nl, r_const], writes=rg0)
                    pe(lambda e, nl_=nl_: e.matmul(ps[6][:, 4:8], lhsT=OnesF[:], rhs=nl_[:], start=True, stop=True), reads=[r_nl, r_const], writes=rg0)
                    dv(lambda e, bs_=bs_, jj=jj: e.tensor_tensor(out=bs_[:], in0=ps[6][:, 0:4], in1=gsb[:, jj, 0:4], op=ALU.add),
                       reads=rg0 + [r_gsb], writes=[r_bs])
                    dv(lambda e, bs_=bs_, wl_=wl_: e.tensor_tensor(out=wl_[:], in0=bs_[:], in1=ps[6][:, 4:8], op=ALU.subtract),
                       reads=rg0 + [r_bs], writes=[r_wl])
                    ac(lambda e, wl_=wl_: e.activation(out=wl_[:], in_=wl_[:], func=AF.Exp), reads=[r_wl], writes=[r_wl])
                    ac(lambda e, eb_=eb_: e.activation(out=eb_[:], in_=ps[6][:, 4:8], func=AF.Exp, scale=-1.0), reads=rg0, writes=[r_eb])
                    rg1 = r_ps[7]
                    ktv = ps[7][:, 0:256]
                    for i in range(2 if MSTAGE >= 3 else 0):
                        pe(lambda e, i=i, cols=cols, ktv=ktv: e.matmul(ktv[:, i * 128:(i + 1) * 128], lhsT=kTm[:, i, cols], rhs=IdentB[:], start=True, stop=True),
                           reads=[r_kTm[i], r_const], writes=rg1)
                    dv(lambda e, kt_=kt_, ktv=ktv: e.tensor_copy(out=kt_[:], in_=ktv), reads=rg1, writes=[r_kt])
                    H = []
                    for h in range(4):
                        po = (h % 2) * 64
                        H.append(dict(h=h, prs=slice(po, po + 64), ti=h // 2, A=ps[2 * h], rA=r_ps[2 * h], B=ps[2 * h + 1], rB=r_ps[2 * h + 1],
                                      nr=nrep.next(), dt=DT.next(), ee=EE.next(), pt=pTm.next(), qb=qb.next(), dn=dn.next(), kw=kw.next()))
                    for c in H:
                        dv(lambda e, c=c, nl_=nl_: e.tensor_scalar(out=c["nr"][0][:], in0=OnesF[:], scalar1=nl_[:, c["h"]:c["h"] + 1], scalar2=-1.0, op0=ALU.mult, op1=ALU.mult),
                           reads=[r_nl, r_const], writes=[c["nr"][1]])
                    for c in H:
                        A, rA, nr_, r_nr, prs, ti = c["A"], c["rA"], c["nr"][0], c["nr"][1], c["prs"], c["ti"]
                        pe(lambda e, A=A, nr_=nr_: e.matmul(A[:, 128:256], lhsT=nr_[:], rhs=TriF[:], start=True, stop=True), reads=[r_nr, r_const], writes=rA)
                        pe(lambda e, A=A, nr_=nr_: e.matmul(A[:, 0:128], lhsT=nr_[:], rhs=TriF[:], start=True, stop=False), reads=[r_nr, r_const], writes=rA)
                        pe(lambda e, A=A: e.matmul(A[:, 0:128], lhsT=IdentF[:], rhs=MaskNeg[:], start=False, stop=True), reads=[r_const], writes=rA)
                        pe(lambda e, A=A, prs=prs, ti=ti, cols=cols: e.matmul(A[:, 256:384], lhsT=kTm[prs, ti, cols], rhs=qcT[prs, ti, cols], start=True, stop=True),
                           reads=[r_kTm[ti], r_qcT[ti]], writes=rA)
                    for c in H:
                        A, rA, h = c["A"], c["rA"], c["h"]
                        dt_, r_dt = c["dt"]
                        ee_, r_ee = c["ee"]
                        ac(lambda e, dt_=dt_, A=A, bs_=bs_, h=h: e.activation(out=dt_[:], in_=A[:, 0:128], func=AF.Exp, bias=bs_[:, h:h + 1], scale=1.0),
                           reads=rA + [r_bs], writes=[r_dt])
                        ac(lambda e, ee_=ee_, A=A: e.activation(out=ee_[:], in_=A[:, 128:256], func=AF.Exp), reads=rA, writes=[r_ee])
                    for c in H:
                        A, rA, prs, ti = c["A"], c["rA"], c["prs"], c["ti"]
                        dt_, r_dt = c["dt"]
                        ee_, r_ee = c["ee"]
                        pt_, r_ptm = c["pt"]
                        qb_, r_qb = c["qb"]
                        dv(lambda e, pt_=pt_, dt_=dt_, A=A: e.tensor_tensor(out=pt_[:], in0=A[:, 256:384], in1=dt_[:], op=ALU.mult),
                           reads=rA + [r_dt], writes=[r_ptm])
                        dv(lambda e, qb_=qb_, ee_=ee_, prs=prs, ti=ti, cols=cols: e.tensor_tensor(out=qb_[prs, :], in0=qcT[prs, ti, cols], in1=ee_[prs, :], op=ALU.mult),
                           reads=[r_qcT[ti], r_ee], writes=[r_qb])
                    for c in H:
                        B, rB, prs, ti, h = c["B"], c["rB"], c["prs"], c["ti"], c["h"]
                        pt_, r_ptm = c["pt"]
                        qb_, r_qb = c["qb"]
                        pe(lambda e, B=B, jj=jj, h=h, pt_=pt_: e.matmul(B[:, 0:128], lhsT=Vm[:, jj, h, 0:128], rhs=pt_[:], start=True, stop=False),
                           reads=[r_Vm[h], r_ptm], writes=rB)
                        pe(lambda e, B=B, prs=prs, ti=ti, qb_=qb_: e.matmul(B[:, 0:128], lhsT=Cb[prs, ti, :], rhs=qb_[prs, :], start=False, stop=True),
                           reads=[r_Cb[h], r_qb], writes=rB)
                        pe(lambda e, B=B, pt_=pt_: e.matmul(B[:, 128:256], lhsT=onesB[:], rhs=pt_[:], start=True, stop=False),
                           reads=[r_const, r_ptm], writes=rB)
                        pe(lambda e, B=B, prs=prs, ti=ti, qb_=qb_: e.matmul(B[:, 128:256], lhsT=nrepB[prs, ti, :], rhs=qb_[prs, :], start=False, stop=True),
                           reads=[r_Cb[h], r_qb], writes=rB)
                    for c in H:
                        B, rB = c["B"], c["rB"]
                        dn_, r_dn = c["dn"]
                        ac(lambda e, dn_=dn_, B=B: e.activation(out=dn_[:], in_=B[:, 128:256], func=AF.Abs), reads=rB, writes=[r_dn])
                    for c in H:
                        B, rB, h = c["B"], c["rB"], c["h"]
                        dn_, r_dn = c["dn"]
                        kw_, r_kw = c["kw"]
                        dv(lambda e, dn_=dn_: e.tensor_scalar(out=dn_[:], in0=dn_[:], scalar1=1.0, scalar2=None, op0=ALU.max),
                           reads=[r_dn], writes=[r_dn])
                        dv(lambda e, dn_=dn_: e.reciprocal(out=dn_[:], in_=dn_[:]), reads=[r_dn], writes=[r_dn])
                        dv(lambda e, dn_=dn_, h=h, cols=cols, B=B: e.tensor_tensor(out=hTm[:, h, cols], in0=B[:, 0:128], in1=dn_[:], op=ALU.mult),
                           reads=rB + [r_dn], writes=[r_hTm[h]])
                        dv(lambda e, kw_=kw_, kt_=kt_, wl_=wl_, h=h: e.tensor_scalar(out=kw_[:], in0=kt_[:, h * 64:(h + 1) * 64], scalar1=wl_[:, h:h + 1], scalar2=None, op0=ALU.mult),
                           reads=[r_kt, r_wl], writes=[r_kw])
                    for c in H:
                        B, rB, prs, h = c["B"], c["rB"], c["prs"], c["h"]
                        kw_, r_kw = c["kw"]
                        pe(lambda e, B=B, prs=prs, kw_=kw_, jj=jj, h=h: e.matmul(B[prs, 256:385], lhsT=kw_[:], rhs=Vm[:, jj, h, :], start=True, stop=True),
                           reads=[r_kw, r_Vm[h]], writes=rB)
                    for c in H:
                        B, rB, prs, ti, h = c["B"], c["rB"], c["prs"], c["ti"], c["h"]
                        dv(lambda e, B=B, prs=prs, ti=ti, eb_=eb_, h=h: e.scalar_tensor_tensor(out=Cst[prs, ti, :], in0=Cst[prs, ti, :], scalar=eb_[prs, h:h + 1],
                                                                                    in1=B[prs, 256:385], op0=ALU.mult, op1=ALU.add),
                           reads=rB + [r_eb, r_Cst[h]], writes=[r_Cst[h]])
                        dv(lambda e, prs=prs, ti=ti: e.tensor_copy(out=Cb[prs, ti, :], in_=Cst[prs, ti, 0:128]), reads=[r_Cst[h]], writes=[r_Cb[h]])
                        dv(lambda e, prs=prs, ti=ti: e.tensor_scalar(out=nrepB[prs, ti, :], in0=OnesF[prs, :], scalar1=Cst[prs, ti, 128:129], scalar2=None, op0=ALU.mult),
                           reads=[r_Cst[h], r_const], writes=[r_Cb[h]])
                for h in range(4):
                    sq, r_sq = sq_ring.next()
                    ac(lambda e, sq=sq, h=h: e.activation(out=sq[:], in_=hTm[:, h, :], func=AF.Square), reads=[r_hTm[h]], writes=[r_sq])
                    pe(lambda e, sq=sq: e.matmul(ps[7][:], lhsT=onesB[:], rhs=sq[:], start=True, stop=True), reads=[r_sq, r_const], writes=r_ps[7])
                    e1, r_e1 = e_ring.next()
                    bc_rstd(ps[7][:], r_ps[7], 128, e1[:], r_e1)
                    t1, r_t1 = t_ring.next()
                    dv(lambda e, t1=t1, e1=e1, h=h: e.scalar_tensor_tensor(out=t1[:], in0=hTm[:, h, :], scalar=gc[:, l, G_MH + h:G_MH + h + 1], in1=e1[:],
                                                                     op0=ALU.mult, op1=ALU.mult),
                       reads=[r_hTm[h], r_e1, r_const], writes=[r_t1])
                    dv(lambda e, t1=t1, h=h: e.tensor_tensor(out=yM[:, h, :], in0=t1[:], in1=so[:, h, :], op=ALU.mult),
                       reads=[r_t1, r_so[h]], writes=[r_yM[h]])

            for fc in range(NFC):
                slab, r_slab = small_ring.next()
                P.op("sp", lambda e, slab=slab, fc=fc: e.dma_start(out=slab[:, 0:1536], in_=wout_b[l, fc]),
                     reads=[r_w[("out", l, fc)]], writes=[r_slab], dma=True)
                Y, rY = ps[6 + fc % 2], r_ps[6 + fc % 2]
                for h in range(8):
                    pe(lambda e, slab=slab, h=h, Y=Y: e.matmul(Y[:], lhsT=slab[0:64, h * 128:(h + 1) * 128], rhs=yA[:, h, :], start=(h == 0), stop=False),
                       reads=[r_slab, r_yA[h]], writes=rY)
                for h in range(4):
                    pe(lambda e, slab=slab, h=h, Y=Y: e.matmul(Y[:], lhsT=slab[:, 1024 + h * 128:1024 + (h + 1) * 128], rhs=yM[:, h, :], start=False, stop=(h == 3)),
                       reads=[r_slab, r_yM[h]], writes=rY)
                dv(lambda e, Y=Y, fc=fc: e.tensor_tensor(out=x_sb[:, fc, :], in0=Y[:], in1=x_sb[:, fc, :], op=ALU.add),
                   reads=rY + [r_x[fc]], writes=[r_x[fc]])

        conv_all = [conv_ops(l) for l in range(NL)]
        emit_conv(conv_all[0])
        r_xs = [[[[Res() for _ in range(NFC)] for _ in range(NB)] for _ in range(NSEQ)] for _ in range(2)]
        finals = []
        for l in range(NL):
            nxt = conv_all[l + 1] if l + 1 < NL else []
            npass = NSEQ * NB
            per = (len(nxt) + npass - 1) // npass if nxt else 0
            ip = 0
            for s in range(NSEQ):
                for b in range(NB):
                    t0 = s * S + b * TB
                    src = xT if l == 0 else xs[(l - 1) % 2]
                    for fc in range(NFC):
                        rd = [] if l == 0 else [r_xs[(l - 1) % 2][s][b][fc]]
                        P.op("sp", lambda e, src=src, fc=fc, t0=t0: e.dma_start(out=x_sb[:, fc, :], in_=src[fc * 128:(fc + 1) * 128, t0:t0 + TB]),
                             reads=rd, writes=[r_x[fc]], dma=True)
                    ffn(l, 0)
                    if do_mixer:
                        mixer(l, s, b)
                    if l == NL - 1 and final:
                        ffn(l, 1)
                        norm_stats()
                        for fc in range(NFC):
                            o_t, r_o = ostage.next()
                            P.op("dve", lambda e, fc=fc, o_t=o_t: e.scalar_tensor_tensor(
                                out=o_t[:], in0=x_sb[:, fc, :], scalar=gf[:, fc:fc + 1], in1=rstd[:], op0=ALU.mult, op1=ALU.mult),
                                reads=[r_x[fc], r_rstd, r_const], writes=[r_o])
                            finals.append(P.op("sp", lambda e, fc=fc, o_t=o_t, t0=t0: e.dma_start(
                                out=outT[fc * 128:(fc + 1) * 128, t0:t0 + TB], in_=o_t[:]), reads=[r_o], writes=[Res()], dma=True))
                    else:
                        last = (l == NL - 1)
                        dstT = outT if last else xs[l % 2]

                        def stream_out(fc, o_t, r_o, dstT=dstT, t0=t0, last=last, l=l, s=s, b=b):
                            wr = Res() if last else r_xs[l % 2][s][b][fc]
                            o = P.op("sp", lambda e: e.dma_start(out=dstT[fc * 128:(fc + 1) * 128, t0:t0 + TB], in_=o_t[:]),
                                     reads=[r_o], writes=[wr], dma=True)
                            if last:
                                finals.append(o)
                        ffn(l, 1, stream_out=stream_out)
                    if nxt:
                        emit_conv(nxt[ip * per:(ip + 1) * per])
                        ip += 1
        P.emit(final_waits=finals)
    return nc


def prep_weights(inp, L0, NL):
    f32 = np.float32
    w = {}
    inp = {k: (np.asarray(v)[L0:L0 + NL] if k not in ('x', 'positions', 'final_norm') else v) for k, v in inp.items()}
    wgu = np.zeros((NL, 2, NC_FF, 128, 2, NFC, 128), f32)
    wd = np.zeros((NL, 2, NFC, 128, NC_FF, 128), f32)
    ffn_w = ((inp["ffn1_w_gate"], inp["ffn1_w_up"], inp["ffn1_w_down"]),
             (inp["ffn2_w_gate"], inp["ffn2_w_up"], inp["ffn2_w_down"]))
    for f in range(2):
        for j in range(2):
            a = np.asarray(ffn_w[f][j], f32)[:NL]
            a = a.reshape(NL, NFC, 128, NC_FF, 128)
            wgu[:, f, :, :, j, :, :] = a.transpose(0, 3, 2, 1, 4)
        a = np.asarray(ffn_w[f][2], f32)[:NL]
        a = a.reshape(NL, NC_FF, 128, NFC, 128)
        wd[:, f] = a.transpose(0, 3, 2, 1, 4)
    w["wgu"] = wgu.reshape(NL, 2, NC_FF, 128, 2048)
    w["wd"] = wd.reshape(NL, 2, NFC, 128, DFF)
    Win = np.asarray(inp["w_in"], f32)[:NL].reshape(NL, NFC, 128, 1960)
    tiles = np.zeros((NL, NWT, 128, NFC, 128), f32)

    def put(t, c0, c1, src_cols):
        tiles[:, t, :, :, c0:c1] = Win[:, :, :, src_cols].transpose(0, 2, 1, 3)
    put(0, 0, 128, np.arange(0, 128)); put(1, 0, 128, np.arange(128, 256)); put(2, 0, 128, np.arange(256, 384))
    put(3, 64, 96, np.arange(384, 416))
    put(4, 64, 96, 384 + (np.arange(32) + 16) % 32)
    put(5, 0, 128, np.arange(416, 544)); put(6, 0, 128, np.arange(544, 672))
    put(7, 0, 128, np.arange(672, 800)); put(8, 0, 128, np.arange(800, 928))
    for h in range(4):
        put(9 + h, 0, 128, np.arange(1440 + 128 * h, 1568 + 128 * h))
        put(14 + h, 0, 128, np.arange(928 + 128 * h, 1056 + 128 * h))
    put(13, 0, 8, np.arange(1952, 1960))
    w["win"] = np.ascontiguousarray(tiles.reshape(NL, NWS, 2, 128, NFC * 128).transpose(0, 1, 3, 2, 4)).reshape(NL, NWS, 128, 2048)
    Wo = np.asarray(inp["w_out"], f32)[:NL]
    wout = np.zeros((NL, NFC, 128, 1536), f32)
    att = Wo[:, 0:512].reshape(NL, 8, 64, NFC, 128)
    wout[:, :, 0:64, 0:1024] = att.transpose(0, 3, 2, 1, 4).reshape(NL, NFC, 64, 1024)
    mem = Wo[:, 512:1024].reshape(NL, 4, 128, NFC, 128)
    wout[:, :, :, 1024:1536] = mem.transpose(0, 3, 2, 1, 4).reshape(NL, NFC, 128, 512)
    w["wout"] = wout
    Wq = np.asarray(inp["w_uq"], f32)[:NL].reshape(NL, 2, 128, 8, 96)
    uq = np.zeros((NL, 128, 2, 8, 192), f32)
    uq[:, :, :, :, 0:96] = Wq.transpose(0, 2, 1, 3, 4)
    uq[:, :, :, :, 160:192] = Wq.transpose(0, 2, 1, 3, 4)[..., 64 + (np.arange(32) + 16) % 32]
    w["uq"] = uq.reshape(NL, 128, 3072)
    w["ukv"] = np.ascontiguousarray(np.asarray(inp["w_ukv"], f32)[:NL])
    gc = np.zeros((NL, 128, NGC), f32)
    for base, nm in ((G_FFN1, "ffn1_norm"), (G_MIX, "mix_norm"), (G_FFN2, "ffn2_norm")):
        gc[:, :, base:base + 8] = np.asarray(inp[nm], f32)[:NL].reshape(NL, 8, 128).transpose(0, 2, 1)
    gc[:, :, G_QL:G_QL + 2] = np.asarray(inp["q_latent_norm"], f32)[:NL].reshape(NL, 2, 128).transpose(0, 2, 1)
    gc[:, :, G_KVL] = np.asarray(inp["kv_latent_norm"], f32)[:NL]
    cw = np.asarray(inp["conv_w"], f32)[:NL].reshape(NL, 4, 4, 128)
    gc[:, :, G_CW:G_CW + 16] = cw.transpose(0, 3, 2, 1).reshape(NL, 128, 16)
    gc[:, :, G_CB:G_CB + 4] = np.asarray(inp["conv_b"], f32)[:NL].reshape(NL, 4, 128).transpose(0, 2, 1)
    gc[:, 0:64, G_AH:G_AH + 8] = np.asarray(inp["attn_head_norm"], f32)[:NL].reshape(NL, 8, 64).transpose(0, 2, 1)
    gc[:, :, G_MH:G_MH + 4] = np.asarray(inp["mlstm_head_norm"], f32)[:NL].reshape(NL, 4, 128).transpose(0, 2, 1)
    w["gcols"] = gc
    w["gfin"] = np.ascontiguousarray(np.asarray(inp["final_norm"], f32).reshape(8, 128).T)
    w["gbias"] = np.concatenate([np.asarray(inp["b_igate"], f32)[:NL], np.asarray(inp["b_fgate"], f32)[:NL]], axis=1)
    inv = (10000.0 ** (-np.arange(0, 32, 2, dtype=np.float32) / 32)).astype(f32)
    invf = np.ones((96, 2), f32)
    invf[:, 0] = 0.0
    invf[64:80, 0] = inv
    invf[80:96, 0] = inv
    invf[64:80, 1] = -1.0
    w["invf"] = invf
    return w


_CACHE = {}


def launch(inp, xT_list, S, B, L0, NL, ncores, final, **kw):
    NSEQ = B // ncores
    key = (S, NSEQ, NL, final, tuple(sorted(kw.items())))
    if key not in _CACHE:
        _CACHE[key] = build(S, NSEQ, NL, final=final, **kw)
    nc = _CACHE[key]
    w = prep_weights(inp, L0, NL)
    posn = np.asarray(inp["positions"], np.int32)
    in_maps = []
    for c in range(ncores):
        m = dict(w)
        m["xT"] = xT_list[c]
        m["pos"] = np.ascontiguousarray(posn[c * NSEQ:(c + 1) * NSEQ].reshape(1, NSEQ * S))
        in_maps.append(m)
    res = run_bass_kernel_spmd(nc, in_maps, core_ids=list(range(ncores)))
    return [res.results[c]["outT"] for c in range(ncores)]


def run(inp, S, B, NL, ncores, per_launch=None, **kw):
    NSEQ = B // ncores
    x = np.asarray(inp["x"], np.float32)
    xT_list = [np.ascontiguousarray(x[c * NSEQ:(c + 1) * NSEQ].reshape(NSEQ * S, D).T) for c in range(ncores)]
    per = per_launch or NL
    for L0 in range(0, NL, per):
        n = min(per, NL - L0)
        xT_list = launch(inp, xT_list, S, B, L0, n, ncores, final=(L0 + n == NL), **kw)
    out = np.empty((B, S, D), np.float32)
    for c in range(ncores):
        out[c * NSEQ:(c + 1) * NSEQ] = xT_list[c].T.reshape(NSEQ, S, D)
    return out


def kernel(**inputs):
    return run(inputs, 4096, 16, 4, 8)
```

```python
import contextlib
import numpy as np
import concourse.bass as bass
import concourse.mybir as mybir
from concourse.bass_utils import run_bass_kernel_spmd

F32 = mybir.dt.float32
BF16 = mybir.dt.bfloat16
I32 = mybir.dt.int32
AF = mybir.ActivationFunctionType
ALU = mybir.AluOpType

D = 1024
DFF = 2816
NFC = 8
NC_FF = 22
TB = 512
EPS = 1e-6
ENGS = ("pe", "act", "dve", "pool", "sp")
NSLOT = 8

G_FFN1, G_MIX, G_FFN2, G_QL, G_KVL, G_CW, G_CB, G_AH, G_MH = 0, 8, 16, 24, 26, 27, 43, 47, 55
NGC = 59


class Res:
    __slots__ = ("w", "r", "excl")

    def __init__(self, excl=False):
        self.w = None
        self.r = []
        self.excl = excl


class Op:
    __slots__ = ("eng", "fn", "deps", "dma", "sig", "sem", "val", "slot")

    def __init__(self, eng, fn, dma):
        self.eng = eng
        self.fn = fn
        self.dma = dma
        self.deps = []
        self.sig = False
        self.sem = None
        self.val = 0
        self.slot = -1


class Prog:
    def __init__(self, nc):
        self.nc = nc
        self.ops = {e: [] for e in ENGS}
        self.dma_count = {e: 0 for e in ENGS}
        self.last_dma_in_slot = {}

    def op(self, eng, fn, reads=(), writes=(), dma=False):
        o = Op(eng, fn, dma)
        deps = []
        ex = [r for r in reads if r.excl]
        if ex:
            reads = [r for r in reads if not r.excl]
            writes = list(writes) + ex
        for r in reads:
            if r.w is not None:
                deps.append(r.w)
        for r in writes:
            if r.w is not None:
                deps.append(r.w)
            deps.extend(r.r)
        for d in deps:
            if d.eng == eng and not d.dma and not dma and eng == "pe":
                continue
            if d not in o.deps:
                o.deps.append(d)
                d.sig = True
        if dma:
            k = self.dma_count[eng]
            self.dma_count[eng] += 1
            o.slot = k % NSLOT
            prev = self.last_dma_in_slot.get((eng, o.slot))
            if prev is not None and prev not in o.deps:
                o.deps.append(prev)
            self.last_dma_in_slot[(eng, o.slot)] = o
            o.sig = True
        for r in reads:
            r.r.append(o)
        for r in writes:
            r.w = o
            r.r = []
        self.ops[eng].append(o)
        return o

    def emit(self, final_waits=()):
        nc = self.nc
        with contextlib.ExitStack() as st:
            sems = {e: st.enter_context(nc.semaphore("s_" + e)) for e in ENGS}
            dsems = {(e, s): st.enter_context(nc.semaphore(f"d_{e}_{s}"))
                     for e in ENGS if self.dma_count[e] > 0 for s in range(NSLOT)}
            cnt = {e: 0 for e in ENGS}
            dcnt = {}
            for e in ENGS:
                for o in self.ops[e]:
                    if o.dma:
                        key = (e, o.slot)
                        dcnt[key] = dcnt.get(key, 0) + 16
                        o.sem = dsems[key]
                        o.val = dcnt[key]
                    elif o.sig:
                        cnt[e] += 1
                        o.sem = sems[e]
                        o.val = cnt[e]
            block = st.enter_context(nc.Block())
            engobj = {"pe": nc.tensor, "act": nc.scalar, "dve": nc.vector,
                      "pool": nc.gpsimd, "sp": nc.sync}
            finals = list(final_waits)

            def run(e):
                eo = engobj[e]
                waited = {}
                for o in self.ops[e]:
                    need = {}
                    for d in o.deps:
                        key = id(d.sem)
                        if d.val > need.get(key, (0, None))[0]:
                            need[key] = (d.val, d.sem)
                    for key, (val, sem) in need.items():
                        if waited.get(key, 0) >= val:
                            continue
                        eo.wait_ge(sem, val)
                        waited[key] = val
                    ins = o.fn(eo)
                    if o.sig:
                        ins.then_inc(o.sem, 16 if o.dma else 1)
                if e == "sp":
                    for d in finals:
                        if waited.get(id(d.sem), 0) >= d.val:
                            continue
                        eo.wait_ge(d.sem, d.val)
                        waited[id(d.sem)] = d.val

            @block.tensor
            def _(eng):
                run("pe")

            @block.scalar
            def _(eng):
                run("act")

            @block.vector
            def _(eng):
                run("dve")

            @block.gpsimd
            def _(eng):
                run("pool")

            @block.sync
            def _(eng):
                run("sp")


class Ring:
    def __init__(self, tiles):
        self.tiles = tiles
        self.res = [Res() for _ in tiles]
        self.i = 0

    def next(self):
        k = self.i % len(self.tiles)
        self.i += 1
        return self.tiles[k], self.res[k]


NWT = 18
NWS = NWT // 2
PI = 3.14159265358979
TWO_PI = 6.28318530717959
ATT_SCALE = 96.0 ** -0.5
MSTAGE = 9


def build(S, NSEQ, NL, do_mixer=True, do_mlstm=True, do_attn=True, final=True):
    nc = bass.Bass("TRN2", target_bir_lowering=False)
    NT = S * NSEQ
    NB = S // TB
    NCH = S // 128

    def din(name, shape, dt=F32):
        return nc.dram_tensor(name, list(shape), dt, kind="ExternalInput").ap()

    def dscr(name, shape, dt):
        return nc.dram_tensor(name, list(shape), dt, kind="Internal").ap()

    xT = din("xT", [D, NT])
    pos = din("pos", [1, NT], I32)
    invf = din("invf", [96, 2])
    wgu = din("wgu", [NL, 2, NC_FF, 128, 2048])
    wd = din("wd", [NL, 2, NFC, 128, DFF])
    win = din("win", [NL, NWS, 128, 2048])
    wout = din("wout", [NL, NFC, 128, 1536])
    uq = din("uq", [NL, 128, 3072])
    ukv = din("ukv", [NL, 128, 1024])
    gcols = din("gcols", [NL, 128, NGC])
    gfin = din("gfin", [128, NFC])
    gbias = din("gbias", [NL, 8])
    outT = nc.dram_tensor("outT", [D, NT], F32, kind="ExternalOutput").ap()

    wgu_b = dscr("wgu_b", [NL, 2, NC_FF, 128, 2048], BF16)
    wd_b = dscr("wd_b", [NL, 2, NFC, 128, DFF], BF16)
    win_b = dscr("win_b", [NL, NWS, 128, 2048], BF16)
    wout_b = dscr("wout_b", [NL, NFC, 128, 1536], BF16)
    xs = [dscr("xs0", [D, NT], F32), dscr("xs1", [D, NT], F32)]
    rope_d = dscr("rope_d", [2, 96, NT], F32)
    kc_d = dscr("kc_d", [NSEQ, 8, 96, S], BF16)
    vc_d = dscr("vc_d", [NSEQ, 8, 128, NCH, 65], BF16)

    P = Prog(nc)
    st = contextlib.ExitStack()
    with st:
        def sb(name, shape, dt=F32):
            return st.enter_context(nc.sbuf_tensor(name, list(shape), dt))

        x_sb = sb("x_sb", [128, NFC, TB]); r_x = [Res() for _ in range(NFC)]
        hT = sb("hT", [128, NFC, TB], BF16); r_h = [Res() for _ in range(NFC)]
        aT = sb("aT", [128, NC_FF, TB], BF16); r_a = [Res() for _ in range(NC_FF)]
        rstd = sb("rstd", [128, TB]); r_rstd = Res()
        sq_ring = Ring([sb(f"sq{i}", [128, TB], BF16) for i in range(2)])
        e_ring = Ring([sb(f"e{i}", [128, TB]) for i in range(2)])
        t_ring = Ring([sb(f"t{i}", [128, TB]) for i in range(2)])
        big_ring = Ring([sb(f"wb{i}", [128, 2048], BF16) for i in range(4)])
        small_ring = Ring([sb(f"ws{i}", [128, DFF], BF16) for i in range(3)])
        onesB = sb("onesB", [128, 128], BF16); r_const = Res()
        gc = sb("gc", [128, NL, NGC]); gf = sb("gf", [128, NFC])
        ostage = Ring([sb(f"ost{i}", [128, TB]) for i in range(3)])

        ps = [st.enter_context(nc.psum_tensor(f"ps{i}", [128, TB], F32)) for i in range(8)]
        r_ps = [[Res(excl=True)] * 4 for _ in range(8)]

        P.op("pool", lambda e: e.memset(onesB[:], 1.0), writes=[r_const])
        P.op("sp", lambda e: e.dma_start(out=gc[:], in_=gcols.rearrange("l p c -> p l c")), writes=[r_const], dma=True)
        P.op("sp", lambda e: e.dma_start(out=gf[:], in_=gfin), writes=[r_const], dma=True)

        if do_mixer:
            cqn = sb("cqn", [128, 2, TB], BF16); r_cqn = Res()
            ckvn = sb("ckvn", [128, TB], BF16); r_ckvn = Res()
            cs = sb("cs", [96, 2, TB]); r_cs = Res()
            KT = sb("KT", [96, 8, TB], BF16); r_KT = [Res() for _ in range(8)]
            VT = sb("VT", [128, 8, 4, 65], BF16); r_VT = Res()
            QT = sb("QT", [96, 8, TB], BF16); r_QT = [Res() for _ in range(8)]
            KH = Ring([sb(f"KH{i}", [96, TB], BF16) for i in range(6)])
            VH = Ring([sb(f"VH{i}", [128, 4, 65], BF16) for i in range(6)])
            PT = Ring([sb(f"PT{i}", [128, TB], BF16) for i in range(5)])
            sq65 = sb("sq65", [65, TB], BF16); r_sq65 = Res()
            NormW = sb("NormW", [65, 64], BF16)
            yA = sb("yA", [64, 8, TB], BF16); r_yA = [Res() for _ in range(8)]
            yM = sb("yM", [128, 4, TB], BF16); r_yM = [Res() for _ in range(4)]
            uq_sb = sb("uq_sb", [128, 2, 8, 192], BF16); r_uq = Res()
            ukv_sb = sb("ukv_sb", [128, 8, 128], BF16); r_ukv = Res()
            gb = sb("gb", [128, NL, 8])
            invf_sb = sb("invf_sb", [96, 2])
            TriB = sb("TriB", [128, 128], BF16)
            U = sb("U", [128, 4, TB + 3]); r_U = [Res() for _ in range(4)]
            qcT = sb("qcT", [128, 2, TB], BF16); r_qcT = [Res() for _ in range(2)]
            kTm = sb("kTm", [128, 2, TB], BF16); r_kTm = [Res() for _ in range(2)]
            so = sb("so", [128, 4, TB], BF16); r_so = [Res() for _ in range(4)]
            hTm = sb("hTm", [128, 4, TB]); r_hTm = [Res() for _ in range(4)]
            Vm = sb("Vm", [128, 4, 4, 129], BF16); r_Vm = [Res() for _ in range(4)]
            gsb = sb("gsb", [128, 4, 8]); r_gsb = Res()
            TriF = sb("TriF", [128, 128]); OnesF = sb("OnesF", [128, 128]); IdentF = sb("IdentF", [128, 128])
            IdentB = sb("IdentB", [128, 128], BF16); MaskNeg = sb("MaskNeg", [128, 128])
            nlf = Ring([sb(f"nlf{i}", [128, 4]) for i in range(2)])
            bias_s = Ring([sb(f"bias_s{i}", [128, 4]) for i in range(2)])
            wl = Ring([sb(f"wl{i}", [128, 4]) for i in range(2)])
            ebend = Ring([sb(f"ebend{i}", [128, 4]) for i in range(2)])
            ktok = Ring([sb(f"ktok{i}", [128, 256], BF16) for i in range(2)])
            nrep = Ring([sb(f"nrep{i}", [128, 128]) for i in range(4)])
            DT = Ring([sb(f"DT{i}", [128, 128]) for i in range(4)])
            EE = Ring([sb(f"EE{i}", [128, 128]) for i in range(4)])
            pTm = Ring([sb(f"pTm{i}", [128, 128], BF16) for i in range(4)])
            qb = Ring([sb(f"qb{i}", [128, 128], BF16) for i in range(4)])
            dn = Ring([sb(f"dn{i}", [128, 128]) for i in range(4)])
            kw = Ring([sb(f"kw{i}", [128, 64], BF16) for i in range(4)])
            Cst = sb("Cst", [128, 2, 129]); r_Cst = [Res() for _ in range(4)]
            Cb = sb("Cb", [128, 2, 128], BF16); nrepB = sb("nrepB", [128, 2, 128], BF16)
            r_Cb = [Res() for _ in range(4)]

            pl = lambda fn, **k: P.op("pool", fn, **k)
            pl(lambda e: e.memset(NormW[0:64, :], 1.0 / 64), writes=[r_const])
            pl(lambda e: e.memset(NormW[64:65, :], EPS), writes=[r_const])
            pl(lambda e: e.memset(OnesF[:], 1.0), writes=[r_const])
            pl(lambda e: e.memset(MaskNeg[:], 0.0), writes=[r_const])
            pl(lambda e: e.memset(VT[:], 1.0), writes=[r_VT])
            pl(lambda e: e.memset(Vm[:], 1.0), writes=r_Vm)
            pl(lambda e: e.memset(yA[:], 0.0), writes=r_yA)
            pl(lambda e: e.memset(yM[:], 0.0), writes=r_yM)
            pl(lambda e: e.affine_select(out=TriF[:], in_=OnesF[:], pattern=[[1, 128]], compare_op=ALU.is_ge,
                                         fill=0.0, base=0, channel_multiplier=-1), reads=[r_const], writes=[r_const])
            pl(lambda e: e.affine_select(out=MaskNeg[:], in_=MaskNeg[:], pattern=[[1, 128]], compare_op=ALU.is_ge,
                                         fill=-30000.0, base=0, channel_multiplier=-1), reads=[r_const], writes=[r_const])
            pl(lambda e: e.affine_select(out=IdentF[:], in_=OnesF[:], pattern=[[1, 128]], compare_op=ALU.is_equal,
                                         fill=0.0, base=0, channel_multiplier=-1), reads=[r_const], writes=[r_const])
            pl(lambda e: e.tensor_copy(out=TriB[:], in_=TriF[:]), reads=[r_const], writes=[r_const])
            pl(lambda e: e.tensor_copy(out=IdentB[:], in_=IdentF[:]), reads=[r_const], writes=[r_const])
            P.op("sp", lambda e: e.dma_start(out=invf_sb[:], in_=invf), writes=[r_const], dma=True)
            for l in range(NL):
                P.op("sp", lambda e, l=l: e.dma_start(out=gb[:, l, :], in_=gbias[l].partition_broadcast(128)),
                     writes=[r_const], dma=True)

            r_rope = Res()
            a, bq, cq_ = e_ring.tiles[0][0:96, :], e_ring.tiles[1][0:96, :], t_ring.tiles[0][0:96, :]
            rpi = t_ring.tiles[1][0:96, :].bitcast(I32)
            r_a_, r_b_, r_c_, r_i_ = e_ring.res[0], e_ring.res[1], t_ring.res[0], t_ring.res[1]
            for ci in range(NT // TB):
                c0 = ci * TB
                P.op("sp", lambda e, c0=c0: e.dma_start(out=rpi, in_=pos[0, c0:c0 + TB].partition_broadcast(96)),
                     writes=[r_i_], dma=True)
                dv = lambda fn, **k: P.op("dve", fn, **k)
                dv(lambda e: e.tensor_copy(out=a, in_=rpi), reads=[r_i_], writes=[r_a_])
                dv(lambda e: e.tensor_scalar(out=a, in0=a, scalar1=invf_sb[:, 0:1], scalar2=None, op0=ALU.mult),
                   reads=[r_a_, r_const], writes=[r_a_])
                for which in range(2):
                    sh = PI / 2 if which == 0 else 0.0
                    dv(lambda e, sh=sh: e.tensor_scalar(out=bq, in0=a, scalar1=sh, scalar2=1.0 / TWO_PI, op0=ALU.add, op1=ALU.mult),
                       reads=[r_a_], writes=[r_b_])
                    dv(lambda e: e.tensor_copy(out=rpi, in_=bq), reads=[r_b_], writes=[r_i_])
                    dv(lambda e: e.tensor_copy(out=bq, in_=rpi), reads=[r_i_], writes=[r_b_])
                    dv(lambda e: e.scalar_tensor_tensor(out=bq, in0=bq, scalar=-TWO_PI, in1=a, op0=ALU.mult, op1=ALU.add),
                       reads=[r_b_, r_a_], writes=[r_b_])
                    if which == 0:
                        dv(lambda e, sh=sh: e.tensor_scalar(out=bq, in0=bq, scalar1=sh, scalar2=None, op0=ALU.add),
                           reads=[r_b_], writes=[r_b_])
                    dv(lambda e: e.tensor_scalar(out=cq_, in0=bq, scalar1=PI, scalar2=-TWO_PI, op0=ALU.is_gt, op1=ALU.mult),
                       reads=[r_b_], writes=[r_c_])
                    dv(lambda e: e.tensor_tensor(out=bq, in0=bq, in1=cq_, op=ALU.add), reads=[r_b_, r_c_], writes=[r_b_])
                    dv(lambda e: e.tensor_scalar(out=cq_, in0=bq, scalar1=-PI, scalar2=TWO_PI, op0=ALU.is_lt, op1=ALU.mult),
                       reads=[r_b_], writes=[r_c_])
                    dv(lambda e: e.tensor_tensor(out=bq, in0=bq, in1=cq_, op=ALU.add), reads=[r_b_, r_c_], writes=[r_b_])
                    dv(lambda e: e.tensor_scalar(out=bq, in0=bq, scalar1=-3.14159, scalar2=3.14159, op0=ALU.max, op1=ALU.min),
                       reads=[r_b_], writes=[r_b_])
                    if which == 0:
                        P.op("act", lambda e: e.activation(out=cq_, in_=bq, func=AF.Sin), reads=[r_b_], writes=[r_c_])
                    else:
                        P.op("act", lambda e: e.activation(out=cq_, in_=bq, func=AF.Sin, scale=invf_sb[:, 1:2]),
                             reads=[r_b_, r_const], writes=[r_c_])
                    P.op("sp", lambda e, which=which, c0=c0: e.dma_start(out=rope_d[which, :, c0:c0 + TB], in_=cq_),
                         reads=[r_c_], writes=[r_rope], dma=True)

        r_w = {}

        def conv_ops(l):
            ops = []

            def add(key, dst, src, n):
                r_w[key] = Res()
                for h0 in range(0, n, 2048):
                    h1 = min(n, h0 + 2048)
                    ops.append((key, dst[:, h0:h1], src[:, h0:h1]))
            for f in range(2):
                for c in range(NC_FF):
                    add(("gu", l, f, c), wgu_b[l, f, c], wgu[l, f, c], 2048)
                for fc in range(NFC):
                    add(("d", l, f, fc), wd_b[l, f, fc], wd[l, f, fc], DFF)
                if f == 0 and do_mixer:
                    for j in range(NWS):
                        add(("in", l, j), win_b[l, j], win[l, j], 2048)
                    for fc in range(NFC):
                        add(("out", l, fc), wout_b[l, fc], wout[l, fc], 1536)
            return ops

        def emit_conv(items):
            for key, dst, src in items:
                P.op("pool", lambda e, dst=dst, src=src: e.dma_start(out=dst, in_=src),
                     writes=[r_w[key]], dma=True)

        def bc_rstd(psrc, r_src, n, dst, r_dst):
            P.op("act", lambda e: e.activation(out=dst, in_=psrc, func=AF.Ln, bias=EPS, scale=1.0 / n),
                 reads=r_src, writes=[r_dst])
            P.op("act", lambda e: e.activation(out=dst, in_=dst, func=AF.Exp, scale=-0.5),
                 reads=[r_dst], writes=[r_dst])

        def sigmoid_chain(dst, r_dst, src, r_src, final_out=None, r_final=None):
            P.op("act", lambda e: e.activation(out=dst, in_=src, func=AF.Exp, scale=-1.0), reads=r_src, writes=[r_dst])
            P.op("act", lambda e: e.activation(out=dst, in_=dst, func=AF.Ln, bias=1.0, scale=1.0), reads=[r_dst], writes=[r_dst])
            if final_out is None:
                P.op("act", lambda e: e.activation(out=dst, in_=dst, func=AF.Exp, scale=-1.0), reads=[r_dst], writes=[r_dst])
            else:
                P.op("act", lambda e: e.activation(out=final_out, in_=dst, func=AF.Exp, scale=-1.0), reads=[r_dst], writes=r_final)

        def norm_stats():
            for fc in range(NFC):
                sq, r_sq = sq_ring.next()
                P.op("act", lambda e, sq=sq, fc=fc: e.activation(out=sq[:], in_=x_sb[:, fc, :], func=AF.Square),
                     reads=[r_x[fc]], writes=[r_sq])
                P.op("pe", lambda e, sq=sq, fc=fc: e.matmul(ps[6][:], lhsT=onesB[:], rhs=sq[:], start=(fc == 0), stop=(fc == NFC - 1)),
                     reads=[r_sq, r_const], writes=r_ps[6])
            bc_rstd(ps[6][:], r_ps[6], D, rstd[:], r_rstd)

        def norm_to_hT(l, gbase):
            norm_stats()
            for fc in range(NFC):
                P.op("dve", lambda e, fc=fc: e.scalar_tensor_tensor(
                    out=hT[:, fc, :], in0=x_sb[:, fc, :], scalar=gc[:, l, gbase + fc:gbase + fc + 1],
                    in1=rstd[:], op0=ALU.mult, op1=ALU.mult),
                    reads=[r_x[fc], r_rstd, r_const], writes=[r_h[fc]])

        gu_alt = [0]

        def ffn(l, f, stream_out=None):
            norm_to_hT(l, G_FFN1 if f == 0 else G_FFN2)
            for c in range(NC_FF):
                slab, r_slab = big_ring.next()
                P.op("sp", lambda e, slab=slab, c=c: e.dma_start(out=slab[:], in_=wgu_b[l, f, c]),
                     reads=[r_w[("gu", l, f, c)]], writes=[r_slab], dma=True)
                k = gu_alt[0] % 2
                gu_alt[0] += 1
                G, U_ = ps[2 * k], ps[2 * k + 1]
                rG, rU = r_ps[2 * k], r_ps[2 * k + 1]
                for kc in range(NFC):
                    P.op("pe", lambda e, slab=slab, kc=kc, G=G: e.matmul(
                        G[:], lhsT=slab[:, kc * 128:(kc + 1) * 128], rhs=hT[:, kc, :], start=(kc == 0), stop=(kc == NFC - 1)),
                        reads=[r_slab, r_h[kc]], writes=rG)
                for kc in range(NFC):
                    P.op("pe", lambda e, slab=slab, kc=kc, U_=U_: e.matmul(
                        U_[:], lhsT=slab[:, (8 + kc) * 128:(9 + kc) * 128], rhs=hT[:, kc, :], start=(kc == 0), stop=(kc == NFC - 1)),
                        reads=[r_slab, r_h[kc]], writes=rU)
                e1, r_e1 = e_ring.next()
                t1, r_t1 = t_ring.next()
                sigmoid_chain(e1[:], r_e1, G[:], rG)
                P.op("dve", lambda e, e1=e1, t1=t1, G=G: e.tensor_tensor(out=t1[:], in0=G[:], in1=e1[:], op=ALU.mult),
                     reads=rG + [r_e1], writes=[r_t1])
                P.op("dve", lambda e, t1=t1, U_=U_, c=c: e.tensor_tensor(out=aT[:, c, :], in0=U_[:], in1=t1[:], op=ALU.mult),
                     reads=rU + [r_t1], writes=[r_a[c]])
            for fc in range(NFC):
                slab, r_slab = small_ring.next()
                P.op("sp", lambda e, slab=slab, fc=fc: e.dma_start(out=slab[:], in_=wd_b[l, f, fc]),
                     reads=[r_w[("d", l, f, fc)]], writes=[r_slab], dma=True)
                Y, rY = ps[4 + fc % 2], r_ps[4 + fc % 2]
                for c in range(NC_FF):
                    P.op("pe", lambda e, slab=slab, c=c, Y=Y: e.matmul(
                        Y[:], lhsT=slab[:, c * 128:(c + 1) * 128], rhs=aT[:, c, :], start=(c == 0), stop=(c == NC_FF - 1)),
                        reads=[r_slab, r_a[c]], writes=rY)
                if stream_out is None:
                    P.op("dve", lambda e, Y=Y, fc=fc: e.scalar_tensor_tensor(
                        out=x_sb[:, fc, :], in0=Y[:], scalar=0.5, in1=x_sb[:, fc, :], op0=ALU.mult, op1=ALU.add),
                        reads=rY + [r_x[fc]], writes=[r_x[fc]])
                else:
                    o_t, r_o = ostage.next()
                    P.op("dve", lambda e, Y=Y, fc=fc, o_t=o_t: e.scalar_tensor_tensor(
                        out=o_t[:], in0=Y[:], scalar=0.5, in1=x_sb[:, fc, :], op0=ALU.mult, op1=ALU.add),
                        reads=rY + [r_x[fc]], writes=[r_o])
                    stream_out(fc, o_t, r_o)

        r_kc = [[Res() for _ in range(NB)] for _ in range(NSEQ)]
        r_vc = [[Res() for _ in range(NB)] for _ in range(NSEQ)]

        def mixer(l, s, b):
            t0 = s * S + b * TB
            dv = lambda fn, **k: P.op("dve", fn, **k)
            ac = lambda fn, **k: P.op("act", fn, **k)
            pe = lambda fn, **k: P.op("pe", fn, **k)
            pl = lambda fn, **k: P.op("pool", fn, **k)
            if s == 0 and b == 0:
                pl(lambda e: e.dma_start(out=uq_sb[:], in_=uq[l]), writes=[r_uq], dma=True)
                pl(lambda e: e.dma_start(out=ukv_sb[:], in_=ukv[l]), writes=[r_ukv], dma=True)
            norm_to_hT(l, G_MIX)
            P.op("sp", lambda e: e.dma_start(out=cs[:], in_=rope_d[:, :, t0:t0 + TB].rearrange("w p t -> p w t")),
                 reads=[r_rope], writes=[r_cs], dma=True)
            if b == 0:
                dv(lambda e: e.memset(U[:, :, 0:3], 0.0), writes=r_U)
                dv(lambda e: e.memset(Cst[:], 0.0), writes=r_Cst)
                dv(lambda e: e.memset(Cb[:], 0.0), writes=r_Cb)
                dv(lambda e: e.memset(nrepB[:], 0.0), writes=r_Cb)

            def proj(slab, off, out_ap, r_out, M=128):
                for kc in range(NFC):
                    pe(lambda e, kc=kc: e.matmul(out_ap, lhsT=slab[:, off + kc * 128: off + kc * 128 + M], rhs=hT[:, kc, :],
                                                 start=(kc == 0), stop=(kc == NFC - 1)),
                       reads=[cur_r_slab[0], r_h[kc]], writes=r_out)

            cur_r_slab = [None]
            for j in range(NWS):
                slab, r_slab = big_ring.next()
                cur_r_slab[0] = r_slab
                P.op("sp", lambda e, slab=slab, j=j: e.dma_start(out=slab[:], in_=win_b[l, j]),
                     reads=[r_w[("in", l, j)]], writes=[r_slab], dma=True)
                for tt in (2 * j, 2 * j + 1):
                    off = (tt % 2) * 1024
                    if tt in (0, 1):
                        proj(slab, off, ps[tt][:], r_ps[tt])
                        if tt == 1:
                            for q in range(2):
                                sq, r_sq = sq_ring.next()
                                ac(lambda e, sq=sq, q=q: e.activation(out=sq[:], in_=ps[q][:], func=AF.Square), reads=r_ps[q], writes=[r_sq])
                                pe(lambda e, sq=sq, q=q: e.matmul(ps[6][:], lhsT=onesB[:], rhs=sq[:], start=(q == 0), stop=(q == 1)),
                                   reads=[r_sq, r_const], writes=r_ps[6])
                            bc_rstd(ps[6][:], r_ps[6], 256, rstd[:], r_rstd)
                            for q in range(2):
                                dv(lambda e, q=q: e.scalar_tensor_tensor(out=cqn[:, q, :], in0=ps[q][:], scalar=gc[:, l, G_QL + q:G_QL + q + 1],
                                                                      in1=rstd[:], op0=ALU.mult, op1=ALU.mult),
                                   reads=r_ps[q] + [r_rstd, r_const], writes=[r_cqn])
                    elif tt == 2:
                        proj(slab, off, ps[2][:], r_ps[2])
                        sq, r_sq = sq_ring.next()
                        ac(lambda e, sq=sq: e.activation(out=sq[:], in_=ps[2][:], func=AF.Square), reads=r_ps[2], writes=[r_sq])
                        pe(lambda e, sq=sq: e.matmul(ps[7][:], lhsT=onesB[:], rhs=sq[:], start=True, stop=True),
                           reads=[r_sq, r_const], writes=r_ps[7])
                        e1, r_e1 = e_ring.next()
                        bc_rstd(ps[7][:], r_ps[7], 128, e1[:], r_e1)
                        dv(lambda e, e1=e1: e.scalar_tensor_tensor(out=ckvn[:], in0=ps[2][:], scalar=gc[:, l, G_KVL:G_KVL + 1],
                                                                in1=e1[:], op0=ALU.mult, op1=ALU.mult),
                           reads=r_ps[2] + [r_e1, r_const], writes=[r_ckvn])
                    elif tt in (3, 4):
                        proj(slab, off, ps[tt][0:96, :], r_ps[tt], M=96)
                        if tt == 4:
                            ta, r_ta = t_ring.next()
                            tb_, r_tb = t_ring.next()
                            dv(lambda e, ta=ta: e.tensor_tensor(out=ta[64:96, :], in0=ps[3][64:96, :], in1=cs[64:96, 0, :], op=ALU.mult),
                               reads=r_ps[3] + [r_cs], writes=[r_ta])
                            dv(lambda e, tb_=tb_: e.tensor_tensor(out=tb_[64:96, :], in0=ps[4][64:96, :], in1=cs[64:96, 1, :], op=ALU.mult),
                               reads=r_ps[4] + [r_cs], writes=[r_tb])
                            dv(lambda e, ta=ta, tb_=tb_: e.tensor_tensor(out=KT[64:96, 0, :], in0=ta[64:96, :], in1=tb_[64:96, :], op=ALU.add),
                               reads=[r_ta, r_tb], writes=[r_KT[0]])
                            for h in range(1, 8):
                                pl(lambda e, h=h: e.tensor_copy(out=KT[64:96, h, :], in_=KT[64:96, 0, :]), reads=[r_KT[0]], writes=[r_KT[h]])
                    elif 5 <= tt <= 8:
                        i = tt - 5
                        pk = ps[i % 4]
                        proj(slab, off, pk[:], r_ps[i % 4])
                        ac(lambda e, i=i, pk=pk: e.activation(out=U[:, i, 3:TB + 3], in_=pk[:], func=AF.Copy), reads=r_ps[i % 4], writes=[r_U[i]])
                    elif 9 <= tt <= 12:
                        h = tt - 9
                        pk, rk = ps[4 + h % 2], r_ps[4 + h % 2]
                        proj(slab, off, pk[:], rk)
                        e1, r_e1 = e_ring.next()
                        sigmoid_chain(e1[:], r_e1, pk[:], rk, final_out=so[:, h, :], r_final=[r_so[h]])
                    elif tt == 13:
                        for jj in range(4):
                            for kc in range(NFC):
                                pe(lambda e, kc=kc, jj=jj, slab=slab, off=off: e.matmul(
                                    ps[6][:, jj * 8:(jj + 1) * 8], lhsT=hT[:, kc, jj * 128:(jj + 1) * 128],
                                    rhs=slab[:, off + kc * 128: off + kc * 128 + 8], start=(kc == 0), stop=(kc == NFC - 1)),
                                   reads=[r_slab, r_h[kc]], writes=[r_ps[6][0]])
                        for jj in range(4):
                            dv(lambda e, jj=jj: e.tensor_tensor(out=gsb[:, jj, :], in0=ps[6][:, jj * 8:(jj + 1) * 8], in1=gb[:, l, :], op=ALU.add),
                               reads=[r_ps[6][0], r_const], writes=[r_gsb])
                    else:
                        hh = tt - 14
                        pk, rk = ps[hh % 2], r_ps[hh % 2]
                        for jj in range(4):
                            for kc in range(NFC):
                                pe(lambda e, kc=kc, jj=jj, slab=slab, off=off, pk=pk: e.matmul(
                                    pk[:, jj * 128:(jj + 1) * 128], lhsT=hT[:, kc, jj * 128:(jj + 1) * 128],
                                    rhs=slab[:, off + kc * 128: off + (kc + 1) * 128], start=(kc == 0), stop=(kc == NFC - 1)),
                                   reads=[r_slab, r_h[kc]], writes=rk)
                        ac(lambda e, hh=hh, pk=pk: e.activation(out=Vm[:, :, hh, 0:128], in_=pk[:].rearrange("p (j d) -> p j d", d=128), func=AF.Copy),
                           reads=rk, writes=[r_Vm[hh]])

            for h in range(8):
                pk, rk = ps[2 + h % 2], r_ps[2 + h % 2]
                pe(lambda e, h=h, pk=pk: e.matmul(pk[0:64, :], lhsT=ukv_sb[:, h, 0:64], rhs=ckvn[:], start=True, stop=True),
                   reads=[r_ukv, r_ckvn], writes=rk)
                ac(lambda e, h=h, pk=pk: e.activation(out=KT[0:64, h, :], in_=pk[0:64, :], func=AF.Copy), reads=rk, writes=[r_KT[h]])
            for jj in range(4):
                pk, rk = ps[4 + jj % 2], r_ps[4 + jj % 2]
                pe(lambda e, jj=jj, pk=pk: e.matmul(pk[:].rearrange("p (h d) -> p h d", d=64), lhsT=ckvn[:, jj * 128:(jj + 1) * 128],
                                                   rhs=ukv_sb[:, :, 64:128], start=True, stop=True),
                   reads=[r_ukv, r_ckvn], writes=rk)
                dv(lambda e, jj=jj, pk=pk: e.tensor_copy(out=VT[:, :, jj, 0:64], in_=pk[:].rearrange("p (h d) -> p h d", d=64)),
                   reads=rk, writes=[r_VT])
            P.op("sp", lambda e: e.dma_start(out=kc_d[s, :, :, t0 - s * S:t0 - s * S + TB].rearrange("h p t -> p h t"), in_=KT[:]),
                 reads=r_KT, writes=[r_kc[s][b]], dma=True)
            P.op("sp", lambda e: e.dma_start(out=vc_d[s, :, :, 4 * b:4 * b + 4, :].rearrange("h p c e -> p h c e"), in_=VT[:]),
                 reads=[r_VT], writes=[r_vc[s][b]], dma=True)

            for h in range(8):
                pq, rq = ps[2 * (h % 2)], r_ps[2 * (h % 2)]
                pr_, rr = ps[2 * (h % 2) + 1], r_ps[2 * (h % 2) + 1]
                for kc in range(2):
                    pe(lambda e, h=h, kc=kc, pq=pq: e.matmul(pq[0:96, :], lhsT=uq_sb[:, kc, h, 0:96], rhs=cqn[:, kc, :], start=(kc == 0), stop=(kc == 1)),
                       reads=[r_uq, r_cqn], writes=rq)
                for kc in range(2):
                    pe(lambda e, h=h, kc=kc, pr_=pr_: e.matmul(pr_[0:96, :], lhsT=uq_sb[:, kc, h, 96:192], rhs=cqn[:, kc, :], start=(kc == 0), stop=(kc == 1)),
                       reads=[r_uq, r_cqn], writes=rr)
                ac(lambda e, h=h, pq=pq: e.activation(out=QT[0:64, h, :], in_=pq[0:64, :], func=AF.Copy), reads=rq, writes=[r_QT[h]])
                ta, r_ta = t_ring.next()
                tb_, r_tb = t_ring.next()
                dv(lambda e, ta=ta, pq=pq: e.tensor_tensor(out=ta[64:96, :], in0=pq[64:96, :], in1=cs[64:96, 0, :], op=ALU.mult),
                   reads=rq + [r_cs], writes=[r_ta])
                dv(lambda e, tb_=tb_, pr_=pr_: e.tensor_tensor(out=tb_[64:96, :], in0=pr_[64:96, :], in1=cs[64:96, 1, :], op=ALU.mult),
                   reads=rr + [r_cs], writes=[r_tb])
                dv(lambda e, ta=ta, tb_=tb_, h=h: e.tensor_tensor(out=QT[64:96, h, :], in0=ta[64:96, :], in1=tb_[64:96, :], op=ALU.add),
                   reads=[r_ta, r_tb], writes=[r_QT[h]])

            if do_attn:
                def head_norm(h, pO, rO):
                        ac(lambda e, pO=pO: e.activation(out=sq65[:], in_=pO[0:65, :], func=AF.Square), reads=rO, writes=[r_sq65])
                        pe(lambda e: e.matmul(ps[5][0:64, :], lhsT=NormW[:], rhs=sq65[:], start=True, stop=True),
                           reads=[r_sq65, r_const], writes=r_ps[5])
                        e1, r_e1 = e_ring.next()
                        P.op("act", lambda e, e1=e1: e.activation(out=e1[0:64, :], in_=ps[5][0:64, :], func=AF.Ln), reads=r_ps[5], writes=[r_e1])
                        P.op("act", lambda e, e1=e1: e.activation(out=e1[0:64, :], in_=e1[0:64, :], func=AF.Exp, scale=-0.5), reads=[r_e1], writes=[r_e1])
                        dv(lambda e, e1=e1, h=h, pO=pO: e.scalar_tensor_tensor(out=yA[:, h, :], in0=pO[0:64, :], scalar=gc[0:64, l, G_AH + h:G_AH + h + 1],
                                                                        in1=e1[0:64, :], op0=ALU.mult, op1=ALU.mult),
                           reads=rO + [r_e1, r_const], writes=[r_yA[h]])

                units = []
                for h in range(8):
                    for kb in range(b + 1):
                        for kk in range(4):
                            units.append((h, kb, kk))
                nk = 4 * (b + 1)
                pend = []
                cur = {}

                def emit_pv(u):
                    h, kc, col0, vh, r_vh, kk, pt, r_pt = u
                    pO, rO = ps[3 + h % 2], r_ps[3 + h % 2]
                    pe(lambda e: e.matmul(pO[0:65, col0:TB], lhsT=vh[:, kk, :], rhs=pt[:, col0:TB], start=(kc == 0), stop=(kc == nk - 1)),
                       reads=[r_vh, r_pt], writes=rO)
                    if kc == nk - 1:
                        head_norm(h, pO, rO)

                s_alt = 0
                for (h, kb, kk) in units:
                    if kk == 0:
                        kh, r_kh = KH.next()
                        vh, r_vh = VH.next()
                        P.op("sp", lambda e, kh=kh, kb=kb, h=h: e.dma_start(out=kh[:], in_=kc_d[s, h, :, kb * TB:(kb + 1) * TB]),
                             reads=[r_kc[s][kb]], writes=[r_kh], dma=True)
                        P.op("sp", lambda e, vh=vh, kb=kb, h=h: e.dma_start(out=vh[:], in_=vc_d[s, h, :, 4 * kb:4 * kb + 4, :]),
                             reads=[r_vc[s][kb]], writes=[r_vh], dma=True)
                        cur = dict(kh=kh, r_kh=r_kh, vh=vh, r_vh=r_vh)
                    kh, r_kh, vh, r_vh = cur["kh"], cur["r_kh"], cur["vh"], cur["r_vh"]
                    kc = kb * 4 + kk
                    d = kc - 4 * b
                    col0 = 128 * d if d > 0 else 0
                    pS, rS = ps[s_alt % 3], r_ps[s_alt % 3]
                    s_alt += 1
                    pt, r_pt = PT.next()
                    pe(lambda e, kh=kh, kk=kk, h=h, col0=col0, pS=pS: e.matmul(
                        pS[:, col0:TB], lhsT=kh[:, kk * 128:(kk + 1) * 128], rhs=QT[:, h, col0:TB], start=True, stop=True),
                       reads=[r_kh, r_QT[h]], writes=rS)
                    ac(lambda e, pt=pt, pS=pS, col0=col0: e.activation(out=pt[:, col0:TB], in_=pS[:, col0:TB], func=AF.Exp, scale=ATT_SCALE),
                       reads=rS, writes=[r_pt])
                    if d >= 0:
                        dv(lambda e, pt=pt, col0=col0: e.tensor_tensor(out=pt[:, col0:col0 + 128], in0=pt[:, col0:col0 + 128], in1=TriB[:], op=ALU.mult),
                           reads=[r_pt, r_const], writes=[r_pt])
                    pend.append((h, kc, col0, vh, r_vh, kk, pt, r_pt))
                    if len(pend) > 2:
                        emit_pv(pend.pop(0))
                while pend:
                    emit_pv(pend.pop(0))

            if do_mlstm:
                for i in range(4):
                    acc, r_acc = t_ring.next()
                    cw = lambda jj, i=i: gc[:, l, G_CW + i * 4 + jj:G_CW + i * 4 + jj + 1]
                    dv(lambda e, acc=acc, i=i, cw=cw: e.tensor_scalar(out=acc[:], in0=U[:, i, 0:TB], scalar1=cw(0),
                                                                      scalar2=gc[:, l, G_CB + i:G_CB + i + 1], op0=ALU.mult, op1=ALU.add),
                       reads=[r_U[i], r_const], writes=[r_acc])
                    for jj in range(1, 4):
                        dv(lambda e, acc=acc, i=i, jj=jj, cw=cw: e.scalar_tensor_tensor(out=acc[:], in0=U[:, i, jj:jj + TB], scalar=cw(jj),
                                                                                  in1=acc[:], op0=ALU.mult, op1=ALU.add),
                           reads=[r_U[i], r_acc, r_const], writes=[r_acc])
                    e1, r_e1 = e_ring.next()
                    sigmoid_chain(e1[:], r_e1, acc[:], [r_acc])
                    if i < 2:
                        dv(lambda e, acc=acc, e1=e1, i=i: e.scalar_tensor_tensor(out=qcT[:, i, :], in0=acc[:], scalar=0.125, in1=e1[:], op0=ALU.mult, op1=ALU.mult),
                           reads=[r_acc, r_e1], writes=[r_qcT[i]])
                    else:
                        dv(lambda e, acc=acc, e1=e1, i=i: e.tensor_tensor(out=kTm[:, i - 2, :], in0=acc[:], in1=e1[:], op=ALU.mult),
                           reads=[r_acc, r_e1], writes=[r_kTm[i - 2]])
                    dv(lambda e, i=i: e.tensor_copy(out=U[:, i, 0:3], in_=U[:, i, TB:TB + 3]), reads=[r_U[i]], writes=[r_U[i]])

                for jj in range(4 if MSTAGE >= 2 else 0):
                    cols = slice(jj * 128, (jj + 1) * 128)
                    nl_, r_nl = nlf.next()
                    bs_, r_bs = bias_s.next()
                    wl_, r_wl = wl.next()
                    eb_, r_eb = ebend.next()
                    kt_, r_kt = ktok.next()
                    ac(lambda e, nl_=nl_, jj=jj: e.activation(out=nl_[:], in_=gsb[:, jj, 4:8], func=AF.Exp, scale=-1.0), reads=[r_gsb], writes=[r_nl])
                    ac(lambda e, nl_=nl_: e.activation(out=nl_[:], in_=nl_[:], func=AF.Ln, bias=1.0, scale=1.0), reads=[r_nl], writes=[r_nl])
                    rg0 = [r_ps[6][0]]
                    pe(lambda e, nl_=nl_: e.matmul(ps[6][:, 0:4], lhsT=TriF[:], rhs=nl_[:], start=True, stop=True), reads=[r_nl, r_const], writes=rg0)
                    pe(lambda e, nl_=nl_: e.matmul(ps[6][:, 4:8], lhsT=OnesF[:], rhs=nl_[:], start=True, stop=True), reads=[r_nl, r_const], writes=rg0)
                    dv(lambda e, bs_=bs_, jj=jj: e.tensor_tensor(out=bs_[:], in0=ps[6][:, 0:4], in1=gsb[:, jj, 0:4], op=ALU.add),
                       reads=rg0 + [r_gsb], writes=[r_bs])
                    dv(lambda e, bs_=bs_, wl_=wl_: e.tensor_tensor(out=wl_[:], in0=bs_[:], in1=ps[6][:, 4:8], op=ALU.subtract),
                       reads=rg0 + [r_bs], writes=[r_wl])
                    ac(lambda e, wl_=wl_: e.activation(out=wl_[:], in_=wl_[:], func=AF.Exp), reads=[r_wl], writes=[r_wl])
                    ac(lambda e, eb_=eb_: e.activation(out=eb_[:], in_=ps[6][:, 4:8], func=AF.Exp, scale=-1.0), reads=rg0, writes=[r_eb])
                    rg1 = r_ps[7]
                    ktv = ps[7][:, 0:256]
                    for i in range(2 if MSTAGE >= 3 else 0):
                        pe(lambda e, i=i, cols=cols, ktv=ktv: e.matmul(ktv[:, i * 128:(i + 1) * 128], lhsT=kTm[:, i, cols], rhs=IdentB[:], start=True, stop=True),
                           reads=[r_kTm[i], r_const], writes=rg1)
                    dv(lambda e, kt_=kt_, ktv=ktv: e.tensor_copy(out=kt_[:], in_=ktv), reads=rg1, writes=[r_kt])
                    H = []
                    for h in range(4):
                        po = (h % 2) * 64
                        H.append(dict(h=h, prs=slice(po, po + 64), ti=h // 2, A=ps[2 * h], rA=r_ps[2 * h], B=ps[2 * h + 1], rB=r_ps[2 * h + 1],
                                      nr=nrep.next(), dt=DT.next(), ee=EE.next(), pt=pTm.next(), qb=qb.next(), dn=dn.next(), kw=kw.next()))
                    for c in H:
                        dv(lambda e, c=c, nl_=nl_: e.tensor_scalar(out=c["nr"][0][:], in0=OnesF[:], scalar1=nl_[:, c["h"]:c["h"] + 1], scalar2=-1.0, op0=ALU.mult, op1=ALU.mult),
                           reads=[r_nl, r_const], writes=[c["nr"][1]])
                    for c in H:
                        A, rA, nr_, r_nr, prs, ti = c["A"], c["rA"], c["nr"][0], c["nr"][1], c["prs"], c["ti"]
                        pe(lambda e, A=A, nr_=nr_: e.matmul(A[:, 128:256], lhsT=nr_[:], rhs=TriF[:], start=True, stop=True), reads=[r_nr, r_const], writes=rA)
                        pe(lambda e, A=A, nr_=nr_: e.matmul(A[:, 0:128], lhsT=nr_[:], rhs=TriF[:], start=True, stop=False), reads=[r_nr, r_const], writes=rA)
                        pe(lambda e, A=A: e.matmul(A[:, 0:128], lhsT=IdentF[:], rhs=MaskNeg[:], start=False, stop=True), reads=[r_const], writes=rA)
                        pe(lambda e, A=A, prs=prs, ti=ti, cols=cols: e.matmul(A[:, 256:384], lhsT=kTm[prs, ti, cols], rhs=qcT[prs, ti, cols], start=True, stop=True),
                           reads=[r_kTm[ti], r_qcT[ti]], writes=rA)
                    for c in H:
                        A, rA, h = c["A"], c["rA"], c["h"]
                        dt_, r_dt = c["dt"]
                        ee_, r_ee = c["ee"]
                        ac(lambda e, dt_=dt_, A=A, bs_=bs_, h=h: e.activation(out=dt_[:], in_=A[:, 0:128], func=AF.Exp, bias=bs_[:, h:h + 1], scale=1.0),
                           reads=rA + [r_bs], writes=[r_dt])
                        ac(lambda e, ee_=ee_, A=A: e.activation(out=ee_[:], in_=A[:, 128:256], func=AF.Exp), reads=rA, writes=[r_ee])
                    for c in H:
                        A, rA, prs, ti = c["A"], c["rA"], c["prs"], c["ti"]
                        dt_, r_dt = c["dt"]
                        ee_, r_ee = c["ee"]
                        pt_, r_ptm = c["pt"]
                        qb_, r_qb = c["qb"]
                        dv(lambda e, pt_=pt_, dt_=dt_, A=A: e.tensor_tensor(out=pt_[:], in0=A[:, 256:384], in1=dt_[:], op=ALU.mult),
                           reads=rA + [r_dt], writes=[r_ptm])
                        dv(lambda e, qb_=qb_, ee_=ee_, prs=prs, ti=ti, cols=cols: e.tensor_tensor(out=qb_[prs, :], in0=qcT[prs, ti, cols], in1=ee_[prs, :], op=ALU.mult),
                           reads=[r_qcT[ti], r_ee], writes=[r_qb])
                    for c in H:
                        B, rB, prs, ti, h = c["B"], c["rB"], c["prs"], c["ti"], c["h"]
                        pt_, r_ptm = c["pt"]
                        qb_, r_qb = c["qb"]
                        pe(lambda e, B=B, jj=jj, h=h, pt_=pt_: e.matmul(B[:, 0:128], lhsT=Vm[:, jj, h, 0:128], rhs=pt_[:], start=True, stop=False),
                           reads=[r_Vm[h], r_ptm], writes=rB)
                        pe(lambda e, B=B, prs=prs, ti=ti, qb_=qb_: e.matmul(B[:, 0:128], lhsT=Cb[prs, ti, :], rhs=qb_[prs, :], start=False, stop=True),
                           reads=[r_Cb[h], r_qb], writes=rB)
                        pe(lambda e, B=B, pt_=pt_: e.matmul(B[:, 128:256], lhsT=onesB[:], rhs=pt_[:], start=True, stop=False),
                           reads=[r_const, r_ptm], writes=rB)
                        pe(lambda e, B=B, prs=prs, ti=ti, qb_=qb_: e.matmul(B[:, 128:256], lhsT=nrepB[prs, ti, :], rhs=qb_[prs, :], start=False, stop=True),
                           reads=[r_Cb[h], r_qb], writes=rB)
                    for c in H:
                        B, rB = c["B"], c["rB"]
                        dn_, r_dn = c["dn"]
                        ac(lambda e, dn_=dn_, B=B: e.activation(out=dn_[:], in_=B[:, 128:256], func=AF.Abs), reads=rB, writes=[r_dn])
                    for c in H:
                        B, rB, h = c["B"], c["rB"], c["h"]
                        dn_, r_dn = c["dn"]
                        kw_, r_kw = c["kw"]
                        dv(lambda e, dn_=dn_: e.tensor_scalar(out=dn_[:], in0=dn_[:], scalar1=1.0, scalar2=None, op0=ALU.max),
                           reads=[r_dn], writes=[r_dn])
                        dv(lambda e, dn_=dn_: e.reciprocal(out=dn_[:], in_=dn_[:]), reads=[r_dn], writes=[r_dn])
                        dv(lambda e, dn_=dn_, h=h, cols=cols, B=B: e.tensor_tensor(out=hTm[:, h, cols], in0=B[:, 0:128], in1=dn_[:], op=ALU.mult),
                           reads=rB + [r_dn], writes=[r_hTm[h]])
                        dv(lambda e, kw_=kw_, kt_=kt_, wl_=wl_, h=h: e.tensor_scalar(out=kw_[:], in0=kt_[:, h * 64:(h + 1) * 64], scalar1=wl_[:, h:h + 1], scalar2=None, op0=ALU.mult),
                           reads=[r_kt, r_wl], writes=[r_kw])
                    for c in H:
                        B, rB, prs, h = c["B"], c["rB"], c["prs"], c["h"]
                        kw_, r_kw = c["kw"]
                        pe(lambda e, B=B, prs=prs, kw_=kw_, jj=jj, h=h: e.matmul(B[prs, 256:385], lhsT=kw_[:], rhs=Vm[:, jj, h, :], start=True, stop=True),
                           reads=[r_kw, r_Vm[h]], writes=rB)
                    for c in H:
                        B, rB, prs, ti, h = c["B"], c["rB"], c["prs"], c["ti"], c["h"]
                        dv(lambda e, B=B, prs=prs, ti=ti, eb_=eb_, h=h: e.scalar_tensor_tensor(out=Cst[prs, ti, :], in0=Cst[prs, ti, :], scalar=eb_[prs, h:h + 1],
                                                                                    in1=B[prs, 256:385], op0=ALU.mult, op1=ALU.add),
                           reads=rB + [r_eb, r_Cst[h]], writes=[r_Cst[h]])
                        dv(lambda e, prs=prs, ti=ti: e.tensor_copy(out=Cb[prs, ti, :], in_=Cst[prs, ti, 0:128]), reads=[r_Cst[h]], writes=[r_Cb[h]])
                        dv(lambda e, prs=prs, ti=ti: e.tensor_scalar(out=nrepB[prs, ti, :], in0=OnesF[prs, :], scalar1=Cst[prs, ti, 128:129], scalar2=None, op0=ALU.mult),
                           reads=[r_Cst[h], r_const], writes=[r_Cb[h]])
                for h in range(4):
                    sq, r_sq = sq_ring.next()
                    ac(lambda e, sq=sq, h=h: e.activation(out=sq[:], in_=hTm[:, h, :], func=AF.Square), reads=[r_hTm[h]], writes=[r_sq])
                    pe(lambda e, sq=sq: e.matmul(ps[7][:], lhsT=onesB[:], rhs=sq[:], start=True, stop=True), reads=[r_sq, r_const], writes=r_ps[7])
                    e1, r_e1 = e_ring.next()
                    bc_rstd(ps[7][:], r_ps[7], 128, e1[:], r_e1)
                    t1, r_t1 = t_ring.next()
                    dv(lambda e, t1=t1, e1=e1, h=h: e.scalar_tensor_tensor(out=t1[:], in0=hTm[:, h, :], scalar=gc[:, l, G_MH + h:G_MH + h + 1], in1=e1[:],
                                                                     op0=ALU.mult, op1=ALU.mult),
                       reads=[r_hTm[h], r_e1, r_const], writes=[r_t1])
                    dv(lambda e, t1=t1, h=h: e.tensor_tensor(out=yM[:, h, :], in0=t1[:], in1=so[:, h, :], op=ALU.mult),
                       reads=[r_t1, r_so[h]], writes=[r_yM[h]])

            for fc in range(NFC):
                slab, r_slab = small_ring.next()
                P.op("sp", lambda e, slab=slab, fc=fc: e.dma_start(out=slab[:, 0:1536], in_=wout_b[l, fc]),
                     reads=[r_w[("out", l, fc)]], writes=[r_slab], dma=True)
                Y, rY = ps[6 + fc % 2], r_ps[6 + fc % 2]
                for h in range(8):
                    pe(lambda e, slab=slab, h=h, Y=Y: e.matmul(Y[:], lhsT=slab[0:64, h * 128:(h + 1) * 128], rhs=yA[:, h, :], start=(h == 0), stop=False),
                       reads=[r_slab, r_yA[h]], writes=rY)
                for h in range(4):
                    pe(lambda e, slab=slab, h=h, Y=Y: e.matmul(Y[:], lhsT=slab[:, 1024 + h * 128:1024 + (h + 1) * 128], rhs=yM[:, h, :], start=False, stop=(h == 3)),
                       reads=[r_slab, r_yM[h]], writes=rY)
                dv(lambda e, Y=Y, fc=fc: e.tensor_tensor(out=x_sb[:, fc, :], in0=Y[:], in1=x_sb[:, fc, :], op=ALU.add),
                   reads=rY + [r_x[fc]], writes=[r_x[fc]])

        conv_all = [conv_ops(l) for l in range(NL)]
        emit_conv(conv_all[0])
        r_xs = [[[[Res() for _ in range(NFC)] for _ in range(NB)] for _ in range(NSEQ)] for _ in range(2)]
        finals = []
        for l in range(NL):
            nxt = conv_all[l + 1] if l + 1 < NL else []
            npass = NSEQ * NB
            per = (len(nxt) + npass - 1) // npass if nxt else 0
            ip = 0
            for s in range(NSEQ):
                for b in range(NB):
                    t0 = s * S + b * TB
                    src = xT if l == 0 else xs[(l - 1) % 2]
                    for fc in range(NFC):
                        rd = [] if l == 0 else [r_xs[(l - 1) % 2][s][b][fc]]
                        P.op("sp", lambda e, src=src, fc=fc, t0=t0: e.dma_start(out=x_sb[:, fc, :], in_=src[fc * 128:(fc + 1) * 128, t0:t0 + TB]),
                             reads=rd, writes=[r_x[fc]], dma=True)
                    ffn(l, 0)
                    if do_mixer:
                        mixer(l, s, b)
                    if l == NL - 1 and final:
                        ffn(l, 1)
                        norm_stats()
                        for fc in range(NFC):
                            o_t, r_o = ostage.next()
                            P.op("dve", lambda e, fc=fc, o_t=o_t: e.scalar_tensor_tensor(
                                out=o_t[:], in0=x_sb[:, fc, :], scalar=gf[:, fc:fc + 1], in1=rstd[:], op0=ALU.mult, op1=ALU.mult),
                                reads=[r_x[fc], r_rstd, r_const], writes=[r_o])
                            finals.append(P.op("sp", lambda e, fc=fc, o_t=o_t, t0=t0: e.dma_start(
                                out=outT[fc * 128:(fc + 1) * 128, t0:t0 + TB], in_=o_t[:]), reads=[r_o], writes=[Res()], dma=True))
                    else:
                        last = (l == NL - 1)
                        dstT = outT if last else xs[l % 2]

                        def stream_out(fc, o_t, r_o, dstT=dstT, t0=t0, last=last, l=l, s=s, b=b):
                            wr = Res() if last else r_xs[l % 2][s][b][fc]
                            o = P.op("pool", lambda e: e.dma_start(out=dstT[fc * 128:(fc + 1) * 128, t0:t0 + TB], in_=o_t[:]),
                                     reads=[r_o], writes=[wr], dma=True)
                            if last:
                                finals.append(o)
                        ffn(l, 1, stream_out=stream_out)
                    if nxt:
                        emit_conv(nxt[ip * per:(ip + 1) * per])
                        ip += 1
        P.emit(final_waits=finals)
    return nc


def prep_weights(inp, L0, NL):
    f32 = np.float32
    w = {}
    inp = {k: (np.asarray(v)[L0:L0 + NL] if k not in ('x', 'positions', 'final_norm') else v) for k, v in inp.items()}
    wgu = np.zeros((NL, 2, NC_FF, 128, 2, NFC, 128), f32)
    wd = np.zeros((NL, 2, NFC, 128, NC_FF, 128), f32)
    ffn_w = ((inp["ffn1_w_gate"], inp["ffn1_w_up"], inp["ffn1_w_down"]),
             (inp["ffn2_w_gate"], inp["ffn2_w_up"], inp["ffn2_w_down"]))
    for f in range(2):
        for j in range(2):
            a = np.asarray(ffn_w[f][j], f32)[:NL]
            a = a.reshape(NL, NFC, 128, NC_FF, 128)
            wgu[:, f, :, :, j, :, :] = a.transpose(0, 3, 2, 1, 4)
        a = np.asarray(ffn_w[f][2], f32)[:NL]
        a = a.reshape(NL, NC_FF, 128, NFC, 128)
        wd[:, f] = a.transpose(0, 3, 2, 1, 4)
    w["wgu"] = wgu.reshape(NL, 2, NC_FF, 128, 2048)
    w["wd"] = wd.reshape(NL, 2, NFC, 128, DFF)
    Win = np.asarray(inp["w_in"], f32)[:NL].reshape(NL, NFC, 128, 1960)
    tiles = np.zeros((NL, NWT, 128, NFC, 128), f32)

    def put(t, c0, c1, src_cols):
        tiles[:, t, :, :, c0:c1] = Win[:, :, :, src_cols].transpose(0, 2, 1, 3)
    put(0, 0, 128, np.arange(0, 128)); put(1, 0, 128, np.arange(128, 256)); put(2, 0, 128, np.arange(256, 384))
    put(3, 64, 96, np.arange(384, 416))
    put(4, 64, 96, 384 + (np.arange(32) + 16) % 32)
    put(5, 0, 128, np.arange(416, 544)); put(6, 0, 128, np.arange(544, 672))
    put(7, 0, 128, np.arange(672, 800)); put(8, 0, 128, np.arange(800, 928))
    for h in range(4):
        put(9 + h, 0, 128, np.arange(1440 + 128 * h, 1568 + 128 * h))
        put(14 + h, 0, 128, np.arange(928 + 128 * h, 1056 + 128 * h))
    put(13, 0, 8, np.arange(1952, 1960))
    w["win"] = np.ascontiguousarray(tiles.reshape(NL, NWS, 2, 128, NFC * 128).transpose(0, 1, 3, 2, 4)).reshape(NL, NWS, 128, 2048)
    Wo = np.asarray(inp["w_out"], f32)[:NL]
    wout = np.zeros((NL, NFC, 128, 1536), f32)
    att = Wo[:, 0:512].reshape(NL, 8, 64, NFC, 128)
    wout[:, :, 0:64, 0:1024] = att.transpose(0, 3, 2, 1, 4).reshape(NL, NFC, 64, 1024)
    mem = Wo[:, 512:1024].reshape(NL, 4, 128, NFC, 128)
    wout[:, :, :, 1024:1536] = mem.transpose(0, 3, 2, 1, 4).reshape(NL, NFC, 128, 512)
    w["wout"] = wout
    Wq = np.asarray(inp["w_uq"], f32)[:NL].reshape(NL, 2, 128, 8, 96)
    uq = np.zeros((NL, 128, 2, 8, 192), f32)
    uq[:, :, :, :, 0:96] = Wq.transpose(0, 2, 1, 3, 4)
    uq[:, :, :, :, 160:192] = Wq.transpose(0, 2, 1, 3, 4)[..., 64 + (np.arange(32) + 16) % 32]
    w["uq"] = uq.reshape(NL, 128, 3072)
    w["ukv"] = np.ascontiguousarray(np.asarray(inp["w_ukv"], f32)[:NL])
    gc = np.zeros((NL, 128, NGC), f32)
    for base, nm in ((G_FFN1, "ffn1_norm"), (G_MIX, "mix_norm"), (G_FFN2, "ffn2_norm")):
        gc[:, :, base:base + 8] = np.asarray(inp[nm], f32)[:NL].reshape(NL, 8, 128).transpose(0, 2, 1)
    gc[:, :, G_QL:G_QL + 2] = np.asarray(inp["q_latent_norm"], f32)[:NL].reshape(NL, 2, 128).transpose(0, 2, 1)
    gc[:, :, G_KVL] = np.asarray(inp["kv_latent_norm"], f32)[:NL]
    cw = np.asarray(inp["conv_w"], f32)[:NL].reshape(NL, 4, 4, 128)
    gc[:, :, G_CW:G_CW + 16] = cw.transpose(0, 3, 2, 1).reshape(NL, 128, 16)
    gc[:, :, G_CB:G_CB + 4] = np.asarray(inp["conv_b"], f32)[:NL].reshape(NL, 4, 128).transpose(0, 2, 1)
    gc[:, 0:64, G_AH:G_AH + 8] = np.asarray(inp["attn_head_norm"], f32)[:NL].reshape(NL, 8, 64).transpose(0, 2, 1)
    gc[:, :, G_MH:G_MH + 4] = np.asarray(inp["mlstm_head_norm"], f32)[:NL].reshape(NL, 4, 128).transpose(0, 2, 1)
    w["gcols"] = gc
    w["gfin"] = np.ascontiguousarray(np.asarray(inp["final_norm"], f32).reshape(8, 128).T)
    w["gbias"] = np.concatenate([np.asarray(inp["b_igate"], f32)[:NL], np.asarray(inp["b_fgate"], f32)[:NL]], axis=1)
    inv = (10000.0 ** (-np.arange(0, 32, 2, dtype=np.float32) / 32)).astype(f32)
    invf = np.ones((96, 2), f32)
    invf[:, 0] = 0.0
    invf[64:80, 0] = inv
    invf[80:96, 0] = inv
    invf[64:80, 1] = -1.0
    w["invf"] = invf
    return w


_CACHE = {}


def launch(inp, xT_list, S, B, L0, NL, ncores, final, **kw):
    NSEQ = B // ncores
    key = (S, NSEQ, NL, final, tuple(sorted(kw.items())))
    if key not in _CACHE:
        _CACHE[key] = build(S, NSEQ, NL, final=final, **kw)
    nc = _CACHE[key]
    w = prep_weights(inp, L0, NL)
    posn = np.asarray(inp["positions"], np.int32)
    in_maps = []
    for c in range(ncores):
        m = dict(w)
        m["xT"] = xT_list[c]
        m["pos"] = np.ascontiguousarray(posn[c * NSEQ:(c + 1) * NSEQ].reshape(1, NSEQ * S))
        in_maps.append(m)
    res = run_bass_kernel_spmd(nc, in_maps, core_ids=list(range(ncores)))
    return [res.results[c]["outT"] for c in range(ncores)]


def run(inp, S, B, NL, ncores, per_launch=None, **kw):
    NSEQ = B // ncores
    x = np.asarray(inp["x"], np.float32)
    xT_list = [np.ascontiguousarray(x[c * NSEQ:(c + 1) * NSEQ].reshape(NSEQ * S, D).T) for c in range(ncores)]
    per = per_launch or NL
    for L0 in range(0, NL, per):
        n = min(per, NL - L0)
        xT_list = launch(inp, xT_list, S, B, L0, n, ncores, final=(L0 + n == NL), **kw)
    out = np.empty((B, S, D), np.float32)
    for c in range(ncores):
        out[c * NSEQ:(c + 1) * NSEQ] = xT_list[c].T.reshape(NSEQ, S, D)
    return out


def kernel(**inputs):
    return run(inputs, 4096, 16, 4, 8)
```
